# Optimizing a Trainium2 kernel written in Bass

```python
import functools
import jax, jax.numpy as jnp
from jax import lax
import numpy as np

D_MODEL = 1024
BATCH = 2
SEQ = 8192
DEPTH = 1
DEC_BATCH = 128
DEC_SEQ = 4
PAST_LEN = 2048
PAGE_SIZE = 128

N_HEADS = 8
HEAD_DIM = 64
D_ATTN = N_HEADS * HEAD_DIM
Q_BLOCK = 128
FORGET_BIAS = 3.0
D_CONV = D_MODEL // 2
CONV_W = 3
N_EXPERTS = 32
TOP_K = 4
D_EXPERT = D_MODEL
SWIGLU_LIMIT = 7.0
SWIGLU_ALPHA = 1.702
MOE_BLOCK = 128
D_PLE = 256
LN_EPS = 1e-5
DEEPNORM_ALPHA = (2 * DEPTH) ** 0.25
DEEPNORM_BETA = (8 * DEPTH) ** -0.25
D_IN = 3 * D_CONV + 3 * D_ATTN + N_HEADS + 2 * D_MODEL

kernel_name = 'hybrid_conv_fox_moe_decoder_step'


def mixer_split_sizes():
    return [D_CONV, D_CONV, D_CONV, D_ATTN, D_ATTN, D_ATTN, N_HEADS, D_MODEL, D_MODEL]


def layer_norm(x, g, b):
    xf = x.astype(jnp.float32)
    mu = jnp.mean(xf, axis=-1, keepdims=True)
    var = jnp.mean(jnp.square(xf - mu), axis=-1, keepdims=True)
    y = (xf - mu) * lax.rsqrt(var + LN_EPS) * g.astype(jnp.float32) + b.astype(jnp.float32)
    return y.astype(x.dtype)


def short_conv(u, prev, conv_w):
    seq = u.shape[1]
    ext = jnp.concatenate([prev.astype(u.dtype), u], axis=1)
    y = ext[:, 0:seq] * conv_w[0]
    for j in range(1, CONV_W):
        y = y + ext[:, j:j + seq] * conv_w[j]
    return y, ext[:, seq:]


def prompt_attention(q, k, v, logf):
    b, s = q.shape[0], q.shape[1]
    nb = s // Q_BLOCK
    scale = HEAD_DIM ** -0.5
    c = jnp.cumsum(logf, axis=1)
    ck = jnp.transpose(c, (0, 2, 1))[:, :, None, :]
    kpos = jnp.arange(s)
    qb = jnp.transpose(q.reshape(b, nb, Q_BLOCK, N_HEADS, HEAD_DIM), (1, 0, 2, 3, 4))
    cqb = jnp.transpose(c.reshape(b, nb, Q_BLOCK, N_HEADS), (1, 0, 3, 2))

    def one_block(args):
        qi, cqi, i = args
        sc = jnp.einsum('bqhd,bkhd->bhqk', qi, k, preferred_element_type=jnp.float32) * scale
        sc = sc + (cqi[..., None] - ck)
        qpos = i * Q_BLOCK + jnp.arange(Q_BLOCK)
        sc = jnp.where(kpos[None, :] <= qpos[:, None], sc, -jnp.inf)
        p = jax.nn.softmax(sc, axis=-1)
        return jnp.einsum('bhqk,bkhd->bqhd', p.astype(v.dtype), v)

    out = lax.map(one_block, (qb, cqb, jnp.arange(nb)))
    return jnp.transpose(out, (1, 0, 2, 3, 4)).reshape(b, s, D_ATTN)


def sample_attention(q, k, v, logf, k_past, v_past, logf_past):
    n, t = q.shape[0], q.shape[1]
    past = k_past.shape[1]
    scale = HEAD_DIM ** -0.5
    k_all = jnp.concatenate([k_past.astype(k.dtype), k], axis=1)
    v_all = jnp.concatenate([v_past.astype(v.dtype), v], axis=1)
    c = jnp.cumsum(jnp.concatenate([logf_past.astype(jnp.float32), logf], axis=1), axis=1)
    cq = jnp.transpose(c[:, past:], (0, 2, 1))[..., None]
    ck = jnp.transpose(c, (0, 2, 1))[:, :, None, :]
    sc = jnp.einsum('bqhd,bkhd->bhqk', q, k_all, preferred_element_type=jnp.float32) * scale
    sc = sc + (cq - ck)
    qpos = past + jnp.arange(t)
    kpos = jnp.arange(past + t)
    sc = jnp.where(kpos[None, :] <= qpos[:, None], sc, -jnp.inf)
    p = jax.nn.softmax(sc, axis=-1)
    out = jnp.einsum('bhqk,bkhd->bqhd', p.astype(v_all.dtype), v_all)
    return out.reshape(n, t, D_ATTN)


def token_mixer(x, conv_prev, attend, w_in, b_f, conv_w, w_br_conv, w_br_attn, w_o):
    n, s = x.shape[0], x.shape[1]
    z = jnp.einsum('nsd,de->nse', x, w_in)
    points = np.cumsum(mixer_split_sizes())[:-1].tolist()
    c_b, c_c, c_h, q, k, v, f_logit, g_conv, g_attn = jnp.split(z, points, axis=-1)
    y_conv, conv_new = short_conv(c_c * c_h, conv_prev, conv_w)
    y_conv = c_b * y_conv
    q = q.reshape(n, s, N_HEADS, HEAD_DIM)
    k = k.reshape(n, s, N_HEADS, HEAD_DIM)
    v = v.reshape(n, s, N_HEADS, HEAD_DIM)
    logf = jax.nn.log_sigmoid(f_logit.astype(jnp.float32) + b_f.astype(jnp.float32))
    y_attn = attend(q, k, v, logf)
    merged = (jax.nn.sigmoid(g_conv) * (y_conv @ w_br_conv)
              + jax.nn.sigmoid(g_attn) * (y_attn @ w_br_attn))
    return merged @ w_o, (k, v, logf, conv_new)


def moe(x, w_router, b_router, w_gate, b_gate, w_up, b_up, w_down, b_down):
    shape = x.shape
    xt = x.reshape(-1, shape[-1])
    n_tok = xt.shape[0]
    n_assign = n_tok * TOP_K
    n_blocks = -(-n_assign // MOE_BLOCK) + N_EXPERTS
    logits = (jnp.einsum('td,de->te', xt, w_router, preferred_element_type=jnp.float32)
              + b_router.astype(jnp.float32))
    top_val, top_idx = lax.top_k(logits, TOP_K)
    probs = jax.nn.softmax(top_val, axis=-1)
    flat_e = top_idx.reshape(-1)
    order = jnp.argsort(flat_e)
    e_sorted = flat_e[order]
    counts = jnp.bincount(flat_e, length=N_EXPERTS).astype(jnp.int32)
    start = jnp.cumsum(counts) - counts
    padded = (counts + MOE_BLOCK - 1) // MOE_BLOCK * MOE_BLOCK
    pad_end = jnp.cumsum(padded)
    pad_start = pad_end - padded
    dest = pad_start[e_sorted] + (jnp.arange(n_assign, dtype=jnp.int32) - start[e_sorted])
    buf = jnp.zeros((n_blocks * MOE_BLOCK, shape[-1]), xt.dtype).at[dest].set(xt[order // TOP_K])
    block_e = jnp.minimum(
        jnp.searchsorted(pad_end, jnp.arange(n_blocks, dtype=jnp.int32) * MOE_BLOCK, side='right'),
        N_EXPERTS - 1)

    def expert_block(args):
        xb, e = args
        g = xb @ w_gate[e] + b_gate[e]
        u = xb @ w_up[e] + b_up[e]
        g = jnp.minimum(g, SWIGLU_LIMIT)
        u = jnp.clip(u, -SWIGLU_LIMIT, SWIGLU_LIMIT)
        h = (u + 1.0) * (g * jax.nn.sigmoid(SWIGLU_ALPHA * g))
        return h @ w_down[e] + b_down[e]

    out = lax.map(expert_block, (buf.reshape(n_blocks, MOE_BLOCK, shape[-1]), block_e))
    out_sorted = out.reshape(-1, shape[-1])[dest]
    out_assign = jnp.zeros_like(out_sorted).at[order].set(out_sorted).reshape(n_tok, TOP_K, shape[-1])
    y = jnp.einsum('tkd,tk->td', out_assign, probs.astype(out_assign.dtype))
    return y.reshape(shape)


def setup_inputs(seed: int = 0) -> dict:
    key = jax.random.key(seed)
    ks = jax.random.split(key, 32)
    f32 = jnp.float32
    n_pages = PAST_LEN // PAGE_SIZE
    n_used = DEC_BATCH * n_pages
    n_pool = n_used + (n_used + 3) // 4
    beta = DEEPNORM_BETA

    def nrm(k, shape, scale=1.0):
        return jax.random.normal(k, shape, f32) * scale

    return dict(
        x_prompt=nrm(ks[0], (BATCH, SEQ, D_MODEL)),
        x_sample=nrm(ks[1], (DEC_BATCH, DEC_SEQ, D_MODEL)),
        cache_k=nrm(ks[2], (DEPTH, n_pool, PAGE_SIZE, N_HEADS, HEAD_DIM)),
        cache_v=nrm(ks[3], (DEPTH, n_pool, PAGE_SIZE, N_HEADS, HEAD_DIM)),
        cache_logf=jax.nn.log_sigmoid(FORGET_BIAS + nrm(ks[4], (DEPTH, n_pool, PAGE_SIZE, N_HEADS))),
        state_conv=nrm(ks[5], (DEPTH, DEC_BATCH, CONV_W - 1, D_CONV)),
        page_table=jax.random.permutation(ks[6], n_pool)[:n_used].reshape(DEC_BATCH, n_pages).astype(jnp.int32),
        p_prompt=nrm(ks[7], (DEPTH, BATCH, SEQ, D_PLE)),
        p_sample=nrm(ks[8], (DEPTH, DEC_BATCH, DEC_SEQ, D_PLE)),
        ln_in_g=1.0 + nrm(ks[9], (D_MODEL,), 0.02),
        ln_in_b=nrm(ks[10], (D_MODEL,), 0.02),
        w_in=nrm(ks[11], (DEPTH, D_MODEL, D_IN), D_MODEL ** -0.5),
        b_f=FORGET_BIAS + nrm(ks[12], (DEPTH, N_HEADS), 0.1),
        conv_w=nrm(ks[13], (DEPTH, CONV_W, D_CONV), CONV_W ** -0.5),
        w_br_conv=nrm(ks[14], (DEPTH, D_CONV, D_MODEL), D_CONV ** -0.5),
        w_br_attn=nrm(ks[15], (DEPTH, D_ATTN, D_MODEL), D_ATTN ** -0.5),
        w_o=nrm(ks[16], (DEPTH, D_MODEL, D_MODEL), beta * D_MODEL ** -0.5),
        ln1_g=1.0 + nrm(ks[17], (DEPTH, D_MODEL), 0.02),
        ln1_b=nrm(ks[18], (DEPTH, D_MODEL), 0.02),
        w_router=nrm(ks[19], (DEPTH, D_MODEL, N_EXPERTS), D_MODEL ** -0.5),
        b_router=nrm(ks[20], (DEPTH, N_EXPERTS), 0.01),
        w_gate=nrm(ks[21], (DEPTH, N_EXPERTS, D_MODEL, D_EXPERT), D_MODEL ** -0.5),
        b_gate=nrm(ks[22], (DEPTH, N_EXPERTS, D_EXPERT), 0.01),
        w_up=nrm(ks[23], (DEPTH, N_EXPERTS, D_MODEL, D_EXPERT), D_MODEL ** -0.5),
        b_up=nrm(ks[24], (DEPTH, N_EXPERTS, D_EXPERT), 0.01),
        w_down=nrm(ks[25], (DEPTH, N_EXPERTS, D_EXPERT, D_MODEL), beta * D_EXPERT ** -0.5),
        b_down=nrm(ks[26], (DEPTH, N_EXPERTS, D_MODEL), 0.01),
        ln2_g=1.0 + nrm(ks[27], (DEPTH, D_MODEL), 0.02),
        ln2_b=nrm(ks[28], (DEPTH, D_MODEL), 0.02),
        w_ple_gate=nrm(ks[29], (DEPTH, D_MODEL, D_MODEL), D_MODEL ** -0.5),
        w_ple_proj=nrm(ks[30], (DEPTH, D_PLE, D_MODEL), beta * D_PLE ** -0.5),
    )


def reference(x_prompt, x_sample, cache_k, cache_v, cache_logf, state_conv, page_table,
              p_prompt, p_sample, ln_in_g, ln_in_b, w_in, b_f, conv_w, w_br_conv, w_br_attn,
              w_o, ln1_g, ln1_b, w_router, b_router, w_gate, b_gate, w_up, b_up, w_down,
              b_down, ln2_g, ln2_b, w_ple_gate, w_ple_proj):
    n_seq = page_table.shape[0]
    past_len = page_table.shape[1] * cache_k.shape[2]

    def layer(x, p, conv_prev, attend, l):
        mix, new_state = token_mixer(x, conv_prev, attend, w_in[l], b_f[l], conv_w[l],
                                     w_br_conv[l], w_br_attn[l], w_o[l])
        x = layer_norm(DEEPNORM_ALPHA * x + mix, ln1_g[l], ln1_b[l])
        ffn = moe(x, w_router[l], b_router[l], w_gate[l], b_gate[l], w_up[l], b_up[l],
                  w_down[l], b_down[l])
        x = layer_norm(DEEPNORM_ALPHA * x + ffn, ln2_g[l], ln2_b[l])
        x = x + jax.nn.sigmoid(x @ w_ple_gate[l]) * (p.astype(x.dtype) @ w_ple_proj[l])
        return x, new_state

    xp = layer_norm(x_prompt, ln_in_g, ln_in_b)
    xs = layer_norm(x_sample, ln_in_g, ln_in_b)
    conv_zero = jnp.zeros((xp.shape[0], CONV_W - 1, D_CONV), xp.dtype)
    kp_l, vp_l, fp_l, cp_l = [], [], [], []
    ks_l, vs_l, fs_l, cs_l = [], [], [], []
    for l in range(DEPTH):
        xp, (kp, vp, fp, cp) = layer(xp, p_prompt[l], conv_zero, prompt_attention, l)
        k_past = cache_k[l][page_table].reshape(n_seq, past_len, N_HEADS, HEAD_DIM)
        v_past = cache_v[l][page_table].reshape(n_seq, past_len, N_HEADS, HEAD_DIM)
        f_past = cache_logf[l][page_table].reshape(n_seq, past_len, N_HEADS)
        attend = functools.partial(sample_attention, k_past=k_past, v_past=v_past, logf_past=f_past)
        xs, (k_s, v_s, f_s, c_s) = layer(xs, p_sample[l], state_conv[l], attend, l)
        kp_l.append(kp); vp_l.append(vp); fp_l.append(fp); cp_l.append(cp)
        ks_l.append(k_s); vs_l.append(v_s); fs_l.append(f_s); cs_l.append(c_s)

    return (xp, xs,
            jnp.stack(kp_l), jnp.stack(vp_l), jnp.stack(fp_l), jnp.stack(cp_l),
            jnp.stack(ks_l), jnp.stack(vs_l), jnp.stack(fs_l), jnp.stack(cs_l))
```

```python
import contextlib
import numpy as np
import ml_dtypes
import concourse.bass as bass
import concourse.mybir as mybir
from concourse.bass_utils import run_bass_kernel_spmd

F32 = mybir.dt.float32
BF16 = mybir.dt.bfloat16
I32 = mybir.dt.int32
ALU = mybir.AluOpType
AF = mybir.ActivationFunctionType
AX = mybir.AxisListType

ENGS = ["pe", "act", "dve", "pool", "sp"]

D = 1024
S = 8192
NB = 64
H = 8
HD = 64
E = 32
CAP = 384
NT = 17
TOWN = NT * 128
NPOOLROWS = 2560 * 128
ALPHA = 2.0 ** 0.25
EPS = 1e-5


class Buf:
    __slots__ = ("name", "lw", "rd", "dsem", "excl")

    def __init__(self, name, dsem=None):
        self.name = name
        self.lw = {}
        self.rd = {}
        self.dsem = dsem
        self.excl = False


class Prog:
    def __init__(self, nc, n_dsem=96):
        self.nc = nc
        self.ops = {e: [] for e in ENGS}
        self.sem = []
        self.semctx = []
        self.esem = {}
        for e in ENGS:
            self.esem[e] = self._newsem("e_" + e)
        self.free_dsems = [self._newsem("d%d" % i) for i in range(n_dsem - 24)]
        self.free_swsems = [self._newsem("w%d" % i) for i in range(24)]
        self.swset = set(self.free_swsems)
        self.val = [0] * len(self.sem)
        self.waited = {e: {} for e in ENGS}
        self.enabled = True
        self.dead = False

    def _newsem(self, name):
        ctx = self.nc.semaphore(name)
        h = ctx.__enter__()
        self.semctx.append(ctx)
        self.sem.append(h)
        return len(self.sem) - 1

    def close(self):
        for c in reversed(self.semctx):
            c.__exit__(None, None, None)

    def buf(self, name, dma=False):
        d = None
        if dma == "sw":
            d = self.free_swsems.pop()
        elif dma:
            d = self.free_dsems.pop()
        return Buf(name, d)

    def release(self, *bufs):
        for b in bufs:
            if b.dsem is not None:
                (self.free_swsems if b.dsem in self.swset else self.free_dsems).append(b.dsem)
                b.dsem = None

    def _waits(self, eng, reads, writes):
        w = {}
        for b in reads:
            for s, v in b.lw.items():
                if w.get(s, 0) < v:
                    w[s] = v
            if b.excl:
                for s, v in b.rd.items():
                    if w.get(s, 0) < v:
                        w[s] = v
        for b in writes:
            for s, v in b.lw.items():
                if w.get(s, 0) < v:
                    w[s] = v
            for s, v in b.rd.items():
                if w.get(s, 0) < v:
                    w[s] = v
        out = []
        wd = self.waited[eng]
        for s, v in w.items():
            if eng == "pe" and s == self.esem["pe"]:
                continue
            if wd.get(s, 0) >= v:
                continue
            wd[s] = v
            out.append((s, v))
        return out

    def _commit(self, tok, reads, writes):
        s, v = tok
        for b in writes:
            b.lw = {s: v}
            b.rd = {}
        for b in reads:
            if b.rd.get(s, 0) < v:
                b.rd[s] = v

    def op(self, eng, fn, reads=(), writes=(), inc=True):
        if not self.enabled or self.dead:
            return None
        waits = self._waits(eng, reads, writes)
        s = self.esem[eng]
        if inc:
            self.val[s] += 1
            tok = (s, self.val[s])
        else:
            tok = (s, self.val[s] + 1)
        self.ops[eng].append((fn, waits, (s, 1) if inc else None))
        self._commit(tok, reads, writes)
        return tok

    def dma(self, eng, fn, reads=(), writes=(), owner=None):
        if not self.enabled or self.dead:
            return None
        if owner is None:
            for b in list(writes) + list(reads):
                if b.dsem is not None:
                    owner = b
                    break
        assert owner is not None and owner.dsem is not None, "dma needs owner"
        waits = self._waits(eng, reads, writes)
        s = owner.dsem
        self.val[s] += 16
        tok = (s, self.val[s])
        self.ops[eng].append((fn, waits, (s, 16)))
        self._commit(tok, reads, writes)
        return tok

    def barrier(self):
        for e in ENGS:
            waits = []
            wd = self.waited[e]
            for s in range(len(self.sem)):
                v = self.val[s]
                if v > 0 and wd.get(s, 0) < v:
                    if e == "pe" and s == self.esem["pe"]:
                        continue
                    wd[s] = v
                    waits.append((s, v))
            if waits:
                self.ops[e].append((None, waits, None))

    def emit(self, block):
        sem = self.sem

        def run(engname):
            def body(engine):
                for fn, waits, inc in self.ops[engname]:
                    for s, v in waits:
                        engine.wait_ge(sem[s], v)
                    if fn is not None:
                        ins = fn(engine)
                        if inc is not None:
                            ins.then_inc(sem[inc[0]], inc[1])
            return body

        block.tensor(run("pe"))
        block.scalar(run("act"))
        block.vector(run("dve"))
        block.gpsimd(run("pool"))
        block.sync(run("sp"))


PHASES = ["A", "A3", "A4", "B", "C", "D"]
A1_BLOCKS = list(range(65))
A2_TILES = list(range(NT))
STOPAT = None
A4_SEQS = list(range(16))


def build():
    nc = bass.Bass("TRN2", target_bir_lowering=False)

    def din(name, shape, dt=F32):
        return nc.dram_tensor(name, shape, dt, kind="ExternalInput").ap()

    def dout(name, shape, dt=F32):
        return nc.dram_tensor(name, shape, dt, kind="ExternalOutput").ap()

    def dscr(name, shape, dt):
        return nc.dram_tensor(name, shape, dt, kind="Internal").ap()

    xb = din("xb", [S, D])
    xo = din("xo", [TOWN, D])
    po = din("po", [TOWN, 256])
    xh = din("xh", [128, D])
    hmask = din("hmask", [128, 8])
    dmask = din("dmask", [128, 16, 512], BF16)
    sel = din("sel", [128, 4, 64])
    kvis = din("kvis", [128, 4, 64])
    bd = din("bd", [64, 64])
    pt = din("pt", [1, 256], I32)
    sconv = din("sconv", [32, 512])
    npool_ = NPOOLROWS if "A4" in PHASES else 128
    ne_ = E if "C" in PHASES else 1
    cache_k = din("cache_k", [npool_, 512])
    cache_v = din("cache_v", [npool_, 512])
    cache_f = din("cache_f", [npool_, 8])
    ln_in_g = din("ln_in_g", [1, D]); ln_in_b = din("ln_in_b", [1, D])
    w_in = din("w_in", [D, 5128])
    b_f = din("b_f", [1, 8])
    conv_w = din("conv_w", [3, 512])
    w_br_conv = din("w_br_conv", [512, D])
    w_br_attn = din("w_br_attn", [512, D])
    w_o = din("w_o", [D, D])
    ln1_g = din("ln1_g", [1, D]); ln1_b = din("ln1_b", [1, D])
    w_router = din("w_router", [D, E]); b_router = din("b_router", [1, E])
    w_gate = din("w_gate", [ne_, D, D]); b_gate = din("b_gate", [E, D])
    w_up = din("w_up", [ne_, D, D]); b_up = din("b_up", [E, D])
    w_down = din("w_down", [ne_, D, D]); b_down = din("b_down", [E, D])
    ln2_g = din("ln2_g", [1, D]); ln2_b = din("ln2_b", [1, D])
    w_ple_gate = din("w_ple_gate", [D, D])
    w_ple_proj = din("w_ple_proj", [256, D])

    o_y = dout("o_y", [TOWN, D])
    o_kT = dout("o_kT", [512, 65 * 128])
    o_v = dout("o_v", [65 * 128, 512])
    o_logf = dout("o_logf", [65 * 128, 8])
    o_convs = dout("o_convs", [512, 16, 2])
    o_convp = dout("o_convp", [512, 2])

    yat = dscr("yat", [H, 64, TOWN], BF16)
    x1s = dscr("x1s", [TOWN, D], F32)
    x1b = dscr("x1b", [TOWN, D], BF16)
    tbl = dscr("tbl", [E * CAP, 1], I32)
    ybuf = dscr("ybuf", [E * CAP, D], BF16)

    C_CB, C_CC, C_CH, C_Q, C_K, C_V, C_F, C_GC, C_GA = 0, 512, 1024, 1536, 2048, 2560, 3072, 3080, 4104

    P = Prog(nc)
    print("sbuf bytes/partition at start:", nc.sbuf_bytes_remaining, flush=True)

    def OP(eng, method, *args, reads=(), writes=(), inc=True, **kw):
        return P.op(eng, lambda e: getattr(e, method)(*args, **kw), reads, writes, inc)

    def DMA(eng, out, in_, reads=(), writes=(), owner=None, **kw):
        return P.dma(eng, lambda e: e.dma_start(out=out, in_=in_, **kw), reads, writes, owner)

    def CK(name):
        if STOPAT == name:
            P.dead = True

    def wview(ap2d):
        return ap2d.rearrange("(c p) n -> p c n", p=128)

    b_yat = P.buf("yat", dma=True)
    b_x1s = P.buf("x1s", dma=True)
    b_x1b = P.buf("x1b", dma=True)
    b_tbl = P.buf("tbl", dma="sw")
    b_ybuf = P.buf("ybuf", dma=True)
    b_out = P.buf("outs", dma=True)

    with contextlib.ExitStack() as S0:
        def SB(st, name, shape, dt, dma=False):
            t = st.enter_context(nc.sbuf_tensor(name, shape, dt))
            return t, P.buf(name, dma=dma)

        def PS(st, name, shape, dt=F32):
            full = 512 if dt == F32 else 1024
            t = st.enter_context(nc.psum_tensor(name, [128, full], dt))
            n = int(np.prod(shape[1:]))
            v = t[0:shape[0], 0:n]
            if len(shape) == 3:
                v = v.rearrange("p (a b) -> p a b", b=shape[2])
            pb_ = P.buf(name)
            pb_.excl = True
            return v, pb_

        identf, b_identf = SB(S0, "identf", [128, 128], F32)
        ident, b_ident = SB(S0, "ident", [128, 128], BF16)
        onesf, b_onesf = SB(S0, "onesf", [128, 128], F32)
        Uf, b_Uf = SB(S0, "Uf", [128, 128], F32)
        Lsf, b_Lsf = SB(S0, "Lsf", [128, 128], F32)
        Gsf, b_Gsf = SB(S0, "Gsf", [128, 128], F32)
        ging, b_ging = SB(S0, "ging", [128, 8], F32, dma=True)
        binb, b_binb = SB(S0, "binb", [128, 8], F32, dma=True)
        slot_i, b_slot = SB(S0, "slot_i", [128, NT, 4], I32)
        gk, b_gk = SB(S0, "gk", [128, NT, 4], F32)
        tokid, b_tokid = SB(S0, "tokid", [128, NT], I32)
        KTn, b_KTn = SB(S0, "KTn", [128, 4, 128], BF16)
        Vn, b_Vn = SB(S0, "Vn", [64, 8, 65], BF16)
        QTn, b_QTn = SB(S0, "QTn", [128, 4, 64], BF16)
        lfn, b_lfn = SB(S0, "lfn", [64, 8], F32)

        OP("pool", "memset", onesf[:], 1.0, writes=[b_onesf])
        for t_, b_, cmp_, pat_, cm_ in ((identf, b_identf, ALU.is_equal, [[-1, 128]], 1), (Uf, b_Uf, ALU.is_ge, [[1, 128]], -1),
                                        (Lsf, b_Lsf, ALU.is_gt, [[1, 128]], -1), (Gsf, b_Gsf, ALU.is_gt, [[-1, 128]], 1)):
            OP("pool", "memset", t_[:], 1.0, writes=[b_])
            OP("pool", "affine_select", out=t_[:], in_=t_[:], pattern=pat_, compare_op=cmp_, fill=0.0,
               base=0, channel_multiplier=cm_, reads=[b_], writes=[b_])
        OP("dve", "tensor_copy", out=ident[:], in_=identf[:], reads=[b_identf], writes=[b_ident])
        OP("pool", "iota", tokid[:], pattern=[[128, NT]], base=0, channel_multiplier=1, writes=[b_tokid])
        DMA("sp", ging[:], ln_in_g.rearrange("o (c p) -> p (o c)", p=128), writes=[b_ging], allow_slow_non_contiguous=True)
        DMA("sp", binb[:], ln_in_b.rearrange("o (c p) -> p (o c)", p=128), writes=[b_binb], allow_slow_non_contiguous=True)

        def ln_stats(st6, b_st6, mv, b_mv, rstd, b_rstd, xt_ap, b_xt):
            OP("dve", "bn_stats", out=st6[:, 0:6], in_=xt_ap[:, 0:512], reads=[b_xt], writes=[b_st6])
            OP("dve", "bn_stats", out=st6[:, 6:12], in_=xt_ap[:, 512:1024], reads=[b_xt], writes=[b_st6])
            OP("dve", "bn_aggr", out=mv[:], in_=st6[:], reads=[b_st6], writes=[b_mv])
            OP("dve", "tensor_scalar_add", out=rstd[:], in0=mv[:, 1:2], scalar1=EPS, reads=[b_mv], writes=[b_rstd])
            OP("act", "sqrt", out=rstd[:], in_=rstd[:], reads=[b_rstd], writes=[b_rstd])
            OP("dve", "reciprocal", out=rstd[:], in_=rstd[:], reads=[b_rstd], writes=[b_rstd])

        with contextlib.ExitStack() as SA:
            P.enabled = "A" in PHASES
            KT, _ = SB(SA, "KT", [128, 4, 65 * 128], BF16)
            KTb = [P.buf("KT%d" % g) for g in range(65)]
            V, _ = SB(SA, "V", [128, 65, 8, 65], BF16)
            Vb = [P.buf("V%d" % g) for g in range(65)]
            QT, _ = SB(SA, "QT", [128, 4, TOWN], BF16)
            QTb = [P.buf("QT%d" % t) for t in range(NT)]
            tf, b_tf = SB(SA, "tf", [128, 65, 8], F32, dma=True)
            cc, b_cc = SB(SA, "cc", [128, 64, 8], F32)
            bfb, b_bfb = SB(SA, "bfb", [128, 8], F32, dma=True)
            DMA("sp", bfb[:], b_f.broadcast_to([128, 8]), writes=[b_bfb])
            if len(A1_BLOCKS) < 65:
                OP("pool", "memset", tf[:], 0.0, writes=[b_tf])
                OP("pool", "memset", KT[:], 0.0, writes=KTb)
                OP("pool", "memset", V[:], 0.0, writes=Vb)
                OP("pool", "memset", QT[:], 0.0, writes=QTb)
            if "noVones" not in PHASES:
                OP("pool", "memset", V[:, :, :, 64:65], 1.0, writes=Vb)

            with contextlib.ExitStack() as SA1:
                wk, b_wk = SB(SA1, "wk", [128, 8, 512], BF16, dma="sw")
                wv, b_wv = SB(SA1, "wv", [128, 8, 512], BF16, dma="sw")
                wq, b_wq = wk, b_wk
                wf, b_wf = SB(SA1, "wf", [128, 8, 8], BF16, dma="sw")
                DMA("pool", wk[:], wview(w_in[:, C_K:C_K + 512]), writes=[b_wk])
                DMA("pool", wv[:], wview(w_in[:, C_V:C_V + 512]), writes=[b_wv])
                DMA("pool", wf[:], wview(w_in[:, C_F:C_F + 8]), writes=[b_wf])
                CK("wload")
                NXB = 2
                xts = [SB(SA1, "xt%d" % i, [128, D], F32, dma=True) for i in range(NXB)]
                xns = [SB(SA1, "xn%d" % i, [128, D], BF16) for i in range(2)]
                xTs = [SB(SA1, "xT%d" % i, [128, 8, 128], BF16) for i in range(2)]
                st6s = [SB(SA1, "st6_%d" % i, [128, 12], F32) for i in range(2)]
                mvs = [SB(SA1, "mv%d" % i, [128, 2], F32) for i in range(2)]
                rstds = [SB(SA1, "rstd%d" % i, [128, 1], F32) for i in range(2)]
                kTo = [SB(SA1, "kTo%d" % i, [128, 4, 128], F32, dma=True) for i in range(2)]
                vo = [SB(SA1, "vo%d" % i, [128, 512], F32, dma=True) for i in range(2)]
                pTr = [PS(SA1, "pTr%d" % i, [128, 4, 128], BF16) for i in range(2)]
                pK = [PS(SA1, "pK%d" % i, [128, 4, 128]) for i in range(2)]
                pV = [PS(SA1, "pV%d" % i, [128, 512]) for i in range(2)]
                pF, b_pF = PS(SA1, "pF", [128, 8])

                def ln_xT(i, src_ap):
                    xt, b_xt = xts[i % NXB]
                    xn, b_xn = xns[i % 2]
                    xT, b_xT = xTs[i % 2]
                    st6, b_st6 = st6s[i % 2]; mv, b_mv = mvs[i % 2]; rstd, b_rstd = rstds[i % 2]
                    DMA("sp", xt[:], src_ap, writes=[b_xt])
                    ln_stats(st6, b_st6, mv, b_mv, rstd, b_rstd, xt, b_xt)
                    OP("dve", "tensor_scalar", out=xn[:], in0=xt[:], scalar1=mv[:, 0:1], scalar2=rstd[:, 0:1],
                       op0=ALU.subtract, op1=ALU.mult, reads=[b_xt, b_mv, b_rstd], writes=[b_xn])
                    for half in range(2):
                        pt_, b_pt = pTr[half]
                        for q in range(4):
                            dc = half * 4 + q
                            OP("pe", "transpose", out=pt_[:, q, :], in_=xn[:, dc * 128:(dc + 1) * 128], identity=ident[:],
                               reads=[b_xn, b_ident], writes=[b_pt])
                        for q in range(4):
                            dc = half * 4 + q
                            OP("act", "activation", out=xT[:, dc, :], in_=pt_[:, q, :], func=AF.Identity,
                               bias=binb[:, dc:dc + 1], scale=ging[:, dc:dc + 1],
                               reads=[b_pt, b_ging, b_binb], writes=[b_xT])
                    return xT, b_xT

                for g in A1_BLOCKS:
                    src = xb[g * 128:(g + 1) * 128, :] if g < 64 else xo[2048:2176, :]
                    xT, b_xT = ln_xT(g, src)
                    CK("lnxT")
                    pk, b_pk = pK[g % 2]
                    for hp in range(4):
                        for dc in range(8):
                            OP("pe", "matmul", pk[:, hp, :], lhsT=wk[:, dc, hp * 128:(hp + 1) * 128], rhs=xT[:, dc, :],
                               start=(dc == 0), stop=(dc == 7), reads=[b_xT, b_wk], writes=[b_pk],
                               inc=(hp == 3 and dc == 7))
                    pv, b_pv = pV[g % 2]
                    for dc in range(8):
                        OP("pe", "matmul", pv[:], lhsT=xT[:, dc, :], rhs=wv[:, dc, :], start=(dc == 0), stop=(dc == 7),
                           reads=[b_xT, b_wv], writes=[b_pv], inc=(dc == 7))
                    for dc in range(8):
                        OP("pe", "matmul", pF[:], lhsT=xT[:, dc, :], rhs=wf[:, dc, :], start=(dc == 0), stop=(dc == 7),
                           reads=[b_xT, b_wf], writes=[b_pF], inc=(dc == 7))
                    CK("mm")
                    OP("dve", "tensor_copy", out=KT[:, :, g * 128:(g + 1) * 128], in_=pk[:], reads=[b_pk], writes=[KTb[g]])
                    CK("ktcopy")
                    ko, b_ko = kTo[g % 2]
                    OP("act", "copy", out=ko[:], in_=pk[:], reads=[b_pk], writes=[b_ko])
                    DMA("sp", o_kT[:, g * 128:(g + 1) * 128].rearrange("(c p) t -> p c t", p=128), ko[:], reads=[b_ko])
                    CK("kout")
                    OP("dve", "tensor_copy", out=V[:, g, :, 0:64], in_=pv[:].rearrange("p (h d) -> p h d", d=64),
                       reads=[b_pv], writes=[Vb[g]])
                    vo_, b_vo = vo[g % 2]
                    OP("act", "copy", out=vo_[:], in_=pv[:], reads=[b_pv], writes=[b_vo])
                    DMA("sp", o_v[g * 128:(g + 1) * 128, :], vo_[:], reads=[b_vo])
                    OP("dve", "tensor_tensor", out=tf[:, g, :], in0=pF[:], in1=bfb[:], op=ALU.add,
                       reads=[b_pF, b_bfb], writes=[b_tf])
                    CK("evac")
                CK("a1loop")
                tf2 = tf[:].rearrange("p g h -> p (g h)")
                OP("act", "activation", out=tf2, in_=tf2, func=AF.Exp, scale=-1.0, reads=[b_tf], writes=[b_tf])
                OP("act", "activation", out=tf2, in_=tf2, func=AF.Ln, bias=1.0, scale=1.0, reads=[b_tf], writes=[b_tf])
                OP("dve", "tensor_scalar_mul", out=tf2, in0=tf2, scalar1=-1.0, reads=[b_tf], writes=[b_tf])
                if "noLogfOut" not in PHASES:
                    for g in range(65):
                        DMA("sp", o_logf[g * 128:(g + 1) * 128, :], tf[:, g, :], reads=[b_tf])
                OP("pool", "tensor_copy", out=KTn[:], in_=KT[:, :, 8192:8320], reads=[KTb[64]], writes=[b_KTn])
                OP("pool", "tensor_copy", out=Vn[:], in_=V[0:64, 64, :, :], reads=[Vb[64]], writes=[b_Vn])
                OP("pool", "tensor_copy", out=lfn[:], in_=tf[0:64, 64, :], reads=[b_tf], writes=[b_lfn])

                CK("logf")
                pC, b_pC = pV[0]
                pTt, b_pTt = pV[1]
                lf512 = tf[:, 0:64, :].rearrange("p g h -> p (g h)")
                OP("pe", "matmul", pC[:], lhsT=Uf[:], rhs=lf512, start=True, stop=True, reads=[b_Uf, b_tf], writes=[b_pC])
                OP("pe", "matmul", pTt[:], lhsT=onesf[:], rhs=lf512, start=True, stop=True, reads=[b_onesf, b_tf], writes=[b_pTt])
                sa, b_sa = SB(SA1, "sa", [128, 64, 8], F32)
                sb_, b_sb = SB(SA1, "sbb", [128, 64, 8], F32)
                tot, b_tot = SB(SA1, "tot", [128, 64, 8], F32)
                OP("dve", "tensor_copy", out=tot[:].rearrange("p g h -> p (g h)"), in_=pTt[:], reads=[b_pTt], writes=[b_tot])
                OP("dve", "tensor_copy", out=sa[:], in_=tot[:], reads=[b_tot], writes=[b_sa])
                cur, b_cur, nxt, b_nxt = sa, b_sa, sb_, b_sb
                for sft in (1, 2, 4, 8, 16, 32):
                    OP("dve", "tensor_tensor", out=nxt[:, sft:64, :], in0=cur[:, sft:64, :], in1=cur[:, 0:64 - sft, :], op=ALU.add,
                       reads=[b_cur], writes=[b_nxt])
                    OP("dve", "tensor_copy", out=nxt[:, 0:sft, :], in_=cur[:, 0:sft, :], reads=[b_cur], writes=[b_nxt])
                    cur, b_cur, nxt, b_nxt = nxt, b_nxt, cur, b_cur
                OP("dve", "tensor_tensor", out=cur[:], in0=cur[:], in1=tot[:], op=ALU.subtract, reads=[b_cur, b_tot], writes=[b_cur])
                OP("dve", "tensor_tensor", out=cc[:].rearrange("p g h -> p (g h)"), in0=pC[:],
                   in1=cur[:].rearrange("p g h -> p (g h)"), op=ALU.add, reads=[b_pC, b_cur], writes=[b_cc])

                CK("cumsum")
                DMA("pool", wq[:], wview(w_in[:, C_Q:C_Q + 512]), writes=[b_wq])
                for t in A2_TILES:
                    xT, b_xT = ln_xT(65 + t, xo[t * 128:(t + 1) * 128, :])
                    pk, b_pk = pK[t % 2]
                    for hp in range(4):
                        for dc in range(8):
                            OP("pe", "matmul", pk[:, hp, :], lhsT=wq[:, dc, hp * 128:(hp + 1) * 128], rhs=xT[:, dc, :],
                               start=(dc == 0), stop=(dc == 7), reads=[b_xT, b_wq], writes=[b_pk],
                               inc=(hp == 3 and dc == 7))
                    OP("dve", "tensor_scalar_mul", out=QT[:, :, t * 128:(t + 1) * 128], in0=pk[:], scalar1=0.125,
                       reads=[b_pk], writes=[QTb[t]])
                OP("pool", "tensor_copy", out=QTn[:], in_=QT[:, :, 2048:2112], reads=[QTb[16]], writes=[b_QTn])
                P.barrier()
                P.release(b_wk, b_wv, b_wf, *[b for _, b in xts], *[b for _, b in kTo], *[b for _, b in vo])

            with contextlib.ExitStack() as SA3:
                P.enabled = "A3" in PHASES
                dm, b_dm = SB(SA3, "dm", [128, 16, 512], BF16, dma=True)
                selt, b_selt = SB(SA3, "selt", [128, 4, 64], F32, dma=True)
                DMA("sp", dm[:], dmask, writes=[b_dm])
                DMA("sp", selt[:], sel, writes=[b_selt])
                kvt, b_kvt = SB(SA3, "kvt", [128, 4, 64], F32, dma=True)
                DMA("sp", kvt[:], kvis, writes=[b_kvt])
                tmpc, b_tmpc = SB(SA3, "tmpc", [128, 64, 8], F32)
                red, b_red = SB(SA3, "red", [128, 8], F32)
                biasm = [SB(SA3, "biasm%d" % i, [128, 64, 8], F32) for i in range(2)]
                pTs = [SB(SA3, "pTs%d" % i, [128, 512], BF16) for i in range(3)]
                rs, b_rs = SB(SA3, "rs", [128, 512], F32)
                bcs, b_bcs = SB(SA3, "bcs", [64, 512], F32)
                yTs = [SB(SA3, "yTs%d" % i, [64, 512], BF16, dma=True) for i in range(2)]
                pS = [PS(SA3, "pS%d" % i, [128, 512]) for i in range(3)]
                pAcc = [PS(SA3, "pAcc%d" % i, [128, 512]) for i in range(2)]
                pB, b_pB = PS(SA3, "pB", [128, 512])
                pSh, b_pSh = PS(SA3, "pSh", [128, 8])
                it = 0
                for m in range(4):
                    bm, b_bm = biasm[m % 2]
                    OP("dve", "tensor_tensor", out=tmpc[:], in0=cc[:], in1=selt[:, m, :].unsqueeze(2).broadcast_to([128, 64, 8]),
                       op=ALU.mult, reads=[b_cc, b_selt], writes=[b_tmpc])
                    OP("dve", "tensor_reduce", out=red[:], in_=tmpc[:].rearrange("p g h -> p h g"), axis=AX.X, op=ALU.add,
                       reads=[b_tmpc], writes=[b_red])
                    OP("pe", "matmul", pSh[:], lhsT=onesf[:], rhs=red[:], start=True, stop=True, reads=[b_onesf, b_red], writes=[b_pSh])
                    OP("dve", "tensor_tensor", out=bm[:], in0=pSh[:].unsqueeze(1).broadcast_to([128, 64, 8]), in1=cc[:],
                       op=ALU.subtract, reads=[b_pSh, b_cc], writes=[b_bm])
                    OP("dve", "tensor_tensor", out=bm[:], in0=bm[:], in1=kvt[:, m, :].unsqueeze(2).broadcast_to([128, 64, 8]),
                       op=ALU.add, reads=[b_bm, b_kvt], writes=[b_bm])
                    nkb = 16 * m + 16
                    for h in range(H):
                        hp, hd0 = h // 2, (h % 2) * 64
                        acc, b_acc = pAcc[(m * H + h) % 2]

                        def s_mm(kb, it_):
                            ps_, b_ps = pS[it_ % 3]
                            OP("pe", "matmul", ps_[:], lhsT=KT[hd0:hd0 + 64, hp, kb * 128:(kb + 1) * 128],
                               rhs=QT[hd0:hd0 + 64, hp, m * 512:(m + 1) * 512], start=True, stop=True,
                               reads=[KTb[kb]] + QTb[4 * m:4 * m + 4], writes=[b_ps])
                        s_mm(0, it)
                        for kb in range(nkb):
                            if kb + 1 < nkb:
                                s_mm(kb + 1, it + 1)
                            ps_, b_ps = pS[it % 3]
                            pT_, b_pT = pTs[it % 3]
                            OP("act", "activation", out=pT_[:], in_=ps_[:], func=AF.Exp, bias=bm[:, kb, h:h + 1], scale=1.0,
                               reads=[b_ps, b_bm], writes=[b_pT])
                            if kb >= 16 * m:
                                OP("pool", "tensor_tensor", out=pT_[:], in0=pT_[:], in1=dm[:, kb - 16 * m, :], op=ALU.mult,
                                   reads=[b_pT, b_dm], writes=[b_pT])
                            OP("pe", "matmul", acc[0:65, :], lhsT=V[:, kb, h, :], rhs=pT_[:], start=(kb == 0), stop=(kb == nkb - 1),
                               reads=[Vb[kb], b_pT], writes=[b_acc], inc=(kb == nkb - 1))
                            it += 1
                        OP("dve", "reciprocal", out=rs[64:65, :], in_=acc[64:65, :], reads=[b_acc], writes=[b_rs])
                        OP("pe", "matmul", pB[0:64, :], lhsT=onesf[64:65, 0:64], rhs=rs[64:65, :], start=True, stop=True,
                           reads=[b_onesf, b_rs], writes=[b_pB])
                        OP("act", "copy", out=bcs[:], in_=pB[0:64, :], reads=[b_pB], writes=[b_bcs])
                        yT_, b_yT = yTs[h % 2]
                        OP("dve", "tensor_tensor", out=yT_[:], in0=acc[0:64, :], in1=bcs[:], op=ALU.mult,
                           reads=[b_acc, b_bcs], writes=[b_yT])
                        DMA("sp", yat[h, :, m * 512:(m + 1) * 512], yT_[:], reads=[b_yT, b_yat], owner=b_yT)
                P.barrier()
                P.release(b_dm, b_selt, *[b for _, b in yTs])
            P.release(b_tf, b_bfb)

        with contextlib.ExitStack() as S4:
            P.enabled = "A4" in PHASES
            ptb, b_ptb = SB(S4, "ptb", [128, 256], I32, dma=True)
            DMA("sp", ptb[:], pt.broadcast_to([128, 256]), writes=[b_ptb])
            pio, b_pio = SB(S4, "pio", [128, 1], I32)
            OP("pool", "iota", pio[:], pattern=[[0, 1]], base=0, channel_multiplier=1, writes=[b_pio])
            ridx, b_ridx = SB(S4, "ridx", [128, 256], I32)
            ptf, b_ptf = SB(S4, "ptf", [128, 256], F32)
            piof, b_piof = SB(S4, "piof", [128, 1], F32)
            OP("dve", "tensor_copy", out=ptf[:], in_=ptb[:], reads=[b_ptb], writes=[b_ptf])
            OP("dve", "tensor_copy", out=piof[:], in_=pio[:], reads=[b_pio], writes=[b_piof])
            OP("dve", "tensor_scalar", out=ptf[:], in0=ptf[:], scalar1=128.0, scalar2=piof[:, 0:1], op0=ALU.mult, op1=ALU.add,
               reads=[b_ptf, b_piof], writes=[b_ptf])
            OP("dve", "tensor_copy", out=ridx[:], in_=ptf[:], reads=[b_ptf], writes=[b_ridx])
            CK("ridx")
            bdt, b_bdt = SB(S4, "bdt", [64, 64], F32, dma=True)
            DMA("sp", bdt[:], bd, writes=[b_bdt])
            bdb, b_bdb = SB(S4, "bdb", [64, 64], BF16)
            OP("dve", "tensor_copy", out=bdb[:], in_=bdt[:], reads=[b_bdt], writes=[b_bdb])
            kst = [SB(S4, "kst%d" % i, [128, 512], F32, dma="sw") for i in range(3)]
            vst = [SB(S4, "vst%d" % i, [128, 512], F32, dma="sw") for i in range(3)]
            fst = [SB(S4, "fst%d" % i, [128, 16, 8], F32, dma="sw") for i in range(2)]
            kb16 = [SB(S4, "kb16_%d" % i, [128, 512], BF16) for i in range(2)]
            KTs = [SB(S4, "KTs%d" % i, [128, 4, 2048], BF16) for i in range(2)]
            Vs = [SB(S4, "Vs%d" % i, [128, 16, 8, 65], BF16) for i in range(2)]
            for i in range(2):
                OP("pool", "memset", Vs[i][0][:, :, :, 64:65], 1.0, writes=[Vs[i][1]])
            bia, b_bia = SB(S4, "bia", [128, 16, 8], F32)
            ta, b_ta = SB(S4, "ta", [128, 16, 8], F32)
            tb_, b_tb = SB(S4, "tbb", [128, 16, 8], F32)
            tt, b_tt = SB(S4, "tt", [128, 16, 8], F32)
            scs, b_scs = SB(S4, "scs", [128, 16, 8, 4], F32)
            pts_, b_pts = SB(S4, "pts", [128, 16, 8, 4], BF16)
            pTk = [PS(S4, "pTk%d" % i, [128, 4, 128], BF16) for i in range(2)]
            pSs, b_pSs = PS(S4, "pSs", [128, 512])
            pSo, b_pSo = PS(S4, "pSo", [128, 512])
            pSpar = [(pSs, b_pSs), (pSo, b_pSo)]
            pWT, b_pW = PS(S4, "pWT", [128, 256])
            pW = pWT[:, 0:128]
            pTo = pWT[:, 128:256]
            b_pTo = b_pW
            pAp, b_pAp = PS(S4, "pAp", [128, 8, 64])
            pAn, b_pAn = PS(S4, "pAn", [128, 8, 64])
            if len(A4_SEQS) < 16:
                OP("dve", "memset", pAp[:], 0.0, writes=[b_pAp])
            for s in A4_SEQS:
                KTs_, b_KTs = KTs[s % 2]
                Vs_, b_Vs = Vs[s % 2]
                f_, b_f_ = fst[s % 2]
                for pg in range(16):
                    col = s * 16 + pg
                    k_, b_k = kst[pg % 3]
                    v_, b_v = vst[pg % 3]
                    P.dma("pool", lambda e, k_=k_, col=col: e.indirect_dma_start(
                        out=k_[:], out_offset=None, in_=cache_k,
                        in_offset=bass.IndirectOffsetOnAxis(ap=ridx[:, col:col + 1], axis=0)),
                        reads=[b_ridx], writes=[b_k])
                    P.dma("pool", lambda e, v_=v_, col=col: e.indirect_dma_start(
                        out=v_[:], out_offset=None, in_=cache_v,
                        in_offset=bass.IndirectOffsetOnAxis(ap=ridx[:, col:col + 1], axis=0)),
                        reads=[b_ridx], writes=[b_v])
                    P.dma("pool", lambda e, f_=f_, col=col, pg=pg: e.indirect_dma_start(
                        out=f_[:, pg, :], out_offset=None, in_=cache_f,
                        in_offset=bass.IndirectOffsetOnAxis(ap=ridx[:, col:col + 1], axis=0)),
                        reads=[b_ridx], writes=[b_f_])
                    CK("gather1")
                    kb_, b_kb = kb16[pg % 2]
                    OP("dve", "tensor_copy", out=kb_[:], in_=k_[:], reads=[b_k], writes=[b_kb])
                    ptk, b_ptk = pTk[pg % 2]
                    for hp in range(4):
                        OP("pe", "transpose", out=ptk[:, hp, :], in_=kb_[:, hp * 128:(hp + 1) * 128], identity=ident[:],
                           reads=[b_kb, b_ident], writes=[b_ptk])
                    OP("act", "copy", out=KTs_[:, :, pg * 128:(pg + 1) * 128], in_=ptk[:], reads=[b_ptk], writes=[b_KTs])
                    OP("pool", "tensor_copy", out=Vs_[:, pg, :, 0:64], in_=v_[:].rearrange("p (h d) -> p h d", d=64),
                       reads=[b_v], writes=[b_Vs])
                CK("pages")
                f128 = f_[:].rearrange("p g h -> p (g h)")
                OP("pe", "matmul", pW[:], lhsT=Gsf[:], rhs=f128, start=True, stop=True, reads=[b_Gsf, b_f_], writes=[b_pW])
                OP("pe", "matmul", pTo[:], lhsT=onesf[:], rhs=f128, start=True, stop=True, reads=[b_onesf, b_f_], writes=[b_pTo])
                OP("dve", "tensor_copy", out=tt[:].rearrange("p g h -> p (g h)"), in_=pTo[:], reads=[b_pTo], writes=[b_tt])
                OP("dve", "tensor_copy", out=ta[:], in_=tt[:], reads=[b_tt], writes=[b_ta])
                cur, b_cur, nxt, b_nxt = ta, b_ta, tb_, b_tb
                for sft in (1, 2, 4, 8):
                    OP("dve", "tensor_tensor", out=nxt[:, 0:16 - sft, :], in0=cur[:, 0:16 - sft, :], in1=cur[:, sft:16, :], op=ALU.add,
                       reads=[b_cur], writes=[b_nxt])
                    OP("dve", "tensor_copy", out=nxt[:, 16 - sft:16, :], in_=cur[:, 16 - sft:16, :], reads=[b_cur], writes=[b_nxt])
                    cur, b_cur, nxt, b_nxt = nxt, b_nxt, cur, b_cur
                OP("dve", "tensor_tensor", out=cur[:], in0=cur[:], in1=tt[:], op=ALU.subtract, reads=[b_cur, b_tt], writes=[b_cur])
                OP("dve", "tensor_tensor", out=bia[:].rearrange("p g h -> p (g h)"), in0=pW[:],
                   in1=cur[:].rearrange("p g h -> p (g h)"), op=ALU.add, reads=[b_pW, b_cur], writes=[b_bia])
                for par in range(2):
                    pS_, b_pS_ = pSpar[par]
                    for pg in range(16):
                        for hh in range(4):
                            hp, hd0 = hh, par * 64
                            c0 = (pg * 4 + hh) * 4
                            OP("pe", "matmul", pS_[:, c0:c0 + 4],
                               lhsT=KTs_[hd0:hd0 + 64, hp, pg * 128:(pg + 1) * 128], rhs=QTn[hd0:hd0 + 64, hp, 4 * s:4 * s + 4],
                               start=True, stop=True, reads=[b_KTs, b_QTn], writes=[b_pS_], inc=(pg == 15 and hh == 3))
                for par in range(2):
                    pS_, b_pS_ = pSpar[par]
                    OP("dve", "tensor_tensor",
                       out=scs[:].rearrange("p g (hh two) q -> p g hh two q", two=2)[:, :, :, par, :],
                       in0=pS_[:, 0:256].rearrange("p (g hh q) -> p g hh q", hh=4, q=4),
                       in1=bia[:].rearrange("p g (hh two) -> p g hh two", two=2)[:, :, :, par].unsqueeze(3).broadcast_to([128, 16, 4, 4]),
                       op=ALU.add, reads=[b_pS_, b_bia], writes=[b_scs])
                OP("act", "activation", out=pts_[:].rearrange("p g h q -> p (g h q)"), in_=scs[:].rearrange("p g h q -> p (g h q)"),
                   func=AF.Exp, reads=[b_scs], writes=[b_pts])
                for h in range(H):
                    for pg in range(16):
                        OP("pe", "matmul", pAp[0:65, h, 4 * s:4 * s + 4], lhsT=Vs_[:, pg, h, :], rhs=pts_[:, pg, h, :],
                           start=(pg == 0), stop=(pg == 15), reads=[b_Vs, b_pts], writes=[b_pAp], inc=(pg == 15 and h == 7))
                CK("seq1")
            csn, b_csn = SB(S4, "csn", [64, 8], F32)
            OP("pe", "matmul", pW[0:64, 0:8], lhsT=bdt[:], rhs=lfn[:], start=True, stop=True, reads=[b_bdt, b_lfn], writes=[b_pW])
            OP("dve", "tensor_scalar_mul", out=csn[:], in0=pW[0:64, 0:8], scalar1=-1.0, reads=[b_pW], writes=[b_csn])
            CK("t1")
            for par in range(2):
                pS_, b_pS_ = pSpar[par]
                for hh in range(4):
                    OP("pe", "matmul", pS_[:, hh * 64:(hh + 1) * 64], lhsT=KTn[par * 64:par * 64 + 64, hh, :],
                       rhs=QTn[par * 64:par * 64 + 64, hh, :], start=True, stop=True,
                       reads=[b_KTn, b_QTn], writes=[b_pS_], inc=(hh == 3))
            CK("t2")
            scn, b_scn = SB(S4, "scn", [64, 8, 64], F32)
            ptn, b_ptn = SB(S4, "ptn", [64, 8, 64], BF16)
            for par in range(2):
                pS_, b_pS_ = pSpar[par]
                OP("dve", "tensor_tensor", out=scn[:].rearrange("p (hh two) q -> p hh two q", two=2)[:, :, par, :],
                   in0=pS_[0:64, 0:256].rearrange("p (hh q) -> p hh q", q=64),
                   in1=csn[:].rearrange("p (hh two) -> p hh two", two=2)[:, :, par].unsqueeze(2).broadcast_to([64, 4, 64]),
                   op=ALU.add, reads=[b_pS_, b_csn], writes=[b_scn])
            OP("act", "activation", out=ptn[:], in_=scn[:], func=AF.Exp, reads=[b_scn], writes=[b_ptn])
            OP("dve", "tensor_tensor", out=ptn[:], in0=ptn[:], in1=bdb[:].unsqueeze(1).broadcast_to([64, 8, 64]), op=ALU.mult,
               reads=[b_ptn, b_bdb], writes=[b_ptn])
            CK("t3")
            for h in range(H):
                OP("pe", "matmul", pAn[0:65, h, :], lhsT=Vn[:, h, :], rhs=ptn[:, h, :], start=True, stop=True,
                   reads=[b_Vn, b_ptn], writes=[b_pAn], inc=(h == 7))
            CK("t4")
            asum, b_asum = SB(S4, "asum", [128, 8, 64], F32)
            OP("dve", "tensor_copy", out=asum[0:65], in_=pAp[0:65], reads=[b_pAp], writes=[b_asum])
            OP("dve", "tensor_tensor", out=asum[0:65], in0=asum[0:65], in1=pAn[0:65], op=ALU.add, reads=[b_asum, b_pAn], writes=[b_asum])
            rs2, b_rs2 = SB(S4, "rs2", [128, 512], F32)
            OP("dve", "reciprocal", out=rs2[64:65, :], in_=asum[64:65].rearrange("p h q -> p (h q)"), reads=[b_asum], writes=[b_rs2])
            OP("pe", "matmul", pSs[0:64, :], lhsT=onesf[64:65, 0:64], rhs=rs2[64:65, :], start=True, stop=True,
               reads=[b_onesf, b_rs2], writes=[b_pSs])
            CK("t5")
            ysn, b_ysn = SB(S4, "ysn", [64, 8, 64], BF16, dma=True)
            OP("dve", "tensor_tensor", out=ysn[:].rearrange("p h q -> p (h q)"), in0=asum[0:64].rearrange("p h q -> p (h q)"),
               in1=pSs[0:64, :], op=ALU.mult, reads=[b_asum, b_pSs], writes=[b_ysn])
            DMA("sp", yat[:, :, 2048:2112].rearrange("h d q -> d h q"), ysn[:], reads=[b_ysn, b_yat], owner=b_ysn)
            zpad, b_zpad = SB(S4, "zpad", [64, 8, 64], BF16, dma=True)
            OP("pool", "memset", zpad[:], 0.0, writes=[b_zpad])
            DMA("sp", yat[:, :, 2112:2176].rearrange("h d q -> d h q"), zpad[:], reads=[b_zpad, b_yat], owner=b_zpad)
            P.barrier()
            P.release(b_ptb, b_bdt, b_ysn, b_zpad, *[b for _, b in kst], *[b for _, b in vst], *[b for _, b in fst])

        with contextlib.ExitStack() as SBk:
            P.enabled = "B" in PHASES
            wcv, b_wcv = SB(SBk, "wcv", [128, 8, 1536], BF16, dma="sw")
            wgc, b_wgc = SB(SBk, "wgc", [128, 8, 1024], BF16, dma="sw")
            wga, b_wga = SB(SBk, "wga", [128, 8, 1024], BF16, dma="sw")
            wbc, b_wbc = SB(SBk, "wbc", [128, 4, 1024], BF16, dma="sw")
            wba, b_wba = SB(SBk, "wba", [64, 8, 1024], BF16, dma="sw")
            wo, b_wo = SB(SBk, "wo", [128, 8, 1024], BF16, dma="sw")
            wr, b_wr = SB(SBk, "wr", [128, 8, 32], F32, dma=True)
            DMA("pool", wcv[:], wview(w_in[:, 0:1536]), writes=[b_wcv])
            DMA("pool", wgc[:], wview(w_in[:, C_GC:C_GC + 1024]), writes=[b_wgc])
            DMA("pool", wga[:], wview(w_in[:, C_GA:C_GA + 1024]), writes=[b_wga])
            DMA("pool", wbc[:], wview(w_br_conv), writes=[b_wbc])
            DMA("pool", wba[:], w_br_attn.rearrange("(h d) n -> d h n", d=64), writes=[b_wba])
            DMA("pool", wo[:], wview(w_o), writes=[b_wo])
            DMA("sp", wr[:], wview(w_router), writes=[b_wr])
            cw, b_cw = SB(SBk, "cw", [128, 3, 4], F32, dma=True)
            for k_ in range(3):
                DMA("sp", cw[:, k_, :], conv_w[k_:k_ + 1, :].rearrange("o (c p) -> p (o c)", p=128), writes=[b_cw],
                    allow_slow_non_contiguous=True)
            hm, b_hm = SB(SBk, "hm", [128, 8], F32, dma=True)
            DMA("sp", hm[:], hmask, writes=[b_hm])
            gin_bc, b_gin_bc = SB(SBk, "gin_bc", [128, D], F32, dma=True)
            bin_bc, b_bin_bc = SB(SBk, "bin_bc", [128, D], F32, dma=True)
            g1_bc, b_g1_bc = SB(SBk, "g1_bc", [128, D], F32, dma=True)
            b1_bc, b_b1_bc = SB(SBk, "b1_bc", [128, D], F32, dma=True)
            brb, b_brb = SB(SBk, "brb", [128, E], F32, dma=True)
            DMA("sp", gin_bc[:], ln_in_g.broadcast_to([128, D]), writes=[b_gin_bc])
            DMA("sp", bin_bc[:], ln_in_b.broadcast_to([128, D]), writes=[b_bin_bc])
            DMA("sp", g1_bc[:], ln1_g.broadcast_to([128, D]), writes=[b_g1_bc])
            DMA("sp", b1_bc[:], ln1_b.broadcast_to([128, D]), writes=[b_b1_bc])
            DMA("sp", brb[:], b_router.broadcast_to([128, E]), writes=[b_brb])
            eoff, b_eoff = SB(SBk, "eoff", [128, E], F32)
            OP("pool", "iota", eoff[:], pattern=[[CAP, E]], base=1, channel_multiplier=0, allow_small_or_imprecise_dtypes=True,
               writes=[b_eoff])
            macc, b_macc = SB(SBk, "macc", [128, E], F32)
            OP("pool", "memset", macc[:], 0.0, writes=[b_macc])
            ztb, b_ztb = SB(SBk, "ztb", [128, 96], I32, dma=True)
            OP("pool", "memset", ztb[:], 0, writes=[b_ztb])
            DMA("sp", tbl.rearrange("(p f) o -> p (f o)", p=128), ztb[:], reads=[b_ztb], writes=[b_tbl], owner=b_ztb)
            uh, b_uh = SB(SBk, "uh", [128, 4, 8], BF16)
            scT, b_scT = SB(SBk, "scT", [32, 512], F32, dma=True)
            DMA("sp", scT[:], sconv, writes=[b_scT])
            ush, b_ush = SB(SBk, "ush", [128, 4, 32], BF16)

            NG = 256
            xt4 = [SB(SBk, "bxt%d" % i, [128, D], F32, dma=True) for i in range(3)]
            xln = [SB(SBk, "xln%d" % i, [128, D], F32) for i in range(2)]
            xnb = [SB(SBk, "bxn%d" % i, [128, D], BF16) for i in range(2)]
            xTg, b_xTg = SB(SBk, "xTg", [128, 8, NG], BF16)
            st6, b_st6 = SB(SBk, "bst6", [128, 12], F32)
            mv, b_mv = SB(SBk, "bmv", [128, 2], F32)
            rstd, b_rstd = SB(SBk, "brstd", [128, 1], F32)
            ccs, b_ccs = SB(SBk, "ccs", [128, NG], F32)
            ext, b_ext = SB(SBk, "ext", [128, 4, NG + 2], BF16)
            exs, b_exs = SB(SBk, "exs", [128, 4, 16, 6], BF16)
            yc, b_yc = SB(SBk, "yc", [128, NG], F32)
            ycT, b_ycT = SB(SBk, "ycT", [128, 4, NG], BF16)
            yaT, b_yaT = SB(SBk, "yaT", [64, 8, NG], BF16, dma=True)
            sgc, b_sgc = SB(SBk, "sgc", [128, NG], F32)
            sga, b_sga = SB(SBk, "sga", [128, NG], F32)
            t1, b_t1 = SB(SBk, "t1", [128, NG], F32)
            mT, b_mT = SB(SBk, "mT", [128, 8, NG], BF16)
            x1t = [SB(SBk, "x1t%d" % i, [128, D], F32, dma=True) for i in range(2)]
            x1bt = [SB(SBk, "x1bt%d" % i, [128, D], BF16, dma=True) for i in range(2)]
            x1T, b_x1T = SB(SBk, "x1T", [128, 8, 128], F32)
            lg, b_lg = SB(SBk, "lg", [128, E], F32)
            m8, b_m8 = SB(SBk, "m8", [128, 8], F32)
            msk, b_msk = SB(SBk, "msk", [128, E], F32)
            ex, b_ex = SB(SBk, "ex", [128, E], F32)
            sm, b_sm = SB(SBk, "sm", [128, 4], F32)
            Gt, b_Gt = SB(SBk, "Gt", [128, E], F32)
            key, b_key = SB(SBk, "key", [128, E], F32)
            k8, b_k8 = SB(SBk, "k8", [128, 8], F32)
            oh, b_oh = SB(SBk, "oh", [128, E], F32)
            sf, b_sf = SB(SBk, "sf", [128, 4], F32)
            convo, b_convo = SB(SBk, "convo", [128, 4, 32], F32, dma=True)
            convp, b_convp = SB(SBk, "convp", [128, 4, 2], F32, dma=True)

            pTr = [PS(SBk, "bpTr%d" % i, [128, 4, 128], BF16) for i in range(2)]
            pA = [PS(SBk, "bpA%d" % i, [128, 512]) for i in range(4)]
            pX, b_pX = PS(SBk, "bpX", [128, 4, 128])
            pL, b_pL = PS(SBk, "bpL", [128, 64])
            pa_i = [0]

            def nextpA():
                r = pA[pa_i[0] % 4]
                pa_i[0] += 1
                return r

            for ci in range(4):
                OP("pe", "transpose", out=pX[:, ci, 0:32], in_=scT[:, ci * 128:(ci + 1) * 128], identity=identf[0:32, 0:32],
                   reads=[b_scT, b_identf], writes=[b_pX])
            OP("dve", "tensor_copy", out=ush[:], in_=pX[:, :, 0:32], reads=[b_pX], writes=[b_ush])

            tile_ctr = [0]

            def group(rows0, n, kind):
                nt = n // 128
                lnt = []
                for ti in range(nt):
                    i = tile_ctr[0]; tile_ctr[0] += 1
                    xt, b_xt = xt4[i % 3]
                    src = xh if kind == "halo" else xo[rows0 + ti * 128: rows0 + (ti + 1) * 128, :]
                    DMA("sp", xt[:], src, writes=[b_xt])
                    ln_stats(st6, b_st6, mv, b_mv, rstd, b_rstd, xt, b_xt)
                    xn_, b_xn = xnb[i % 2]
                    OP("dve", "tensor_scalar", out=xn_[:], in0=xt[:], scalar1=mv[:, 0:1], scalar2=rstd[:, 0:1],
                       op0=ALU.subtract, op1=ALU.mult, reads=[b_xt, b_mv, b_rstd], writes=[b_xn])
                    if kind != "halo":
                        xl, b_xl = xln[ti % 2]
                        OP("pool", "tensor_tensor", out=xl[:], in0=xn_[:], in1=gin_bc[:], op=ALU.mult, reads=[b_xn, b_gin_bc], writes=[b_xl])
                        OP("pool", "tensor_tensor", out=xl[:], in0=xl[:], in1=bin_bc[:], op=ALU.add, reads=[b_xl, b_bin_bc], writes=[b_xl])
                        lnt.append((xl, b_xl))
                    for half in range(2):
                        pt_, b_pt = pTr[half]
                        for q in range(4):
                            dc = half * 4 + q
                            OP("pe", "transpose", out=pt_[:, q, :], in_=xn_[:, dc * 128:(dc + 1) * 128], identity=ident[:],
                               reads=[b_xn, b_ident], writes=[b_pt])
                        for q in range(4):
                            dc = half * 4 + q
                            OP("act", "activation", out=xTg[:, dc, ti * 128:(ti + 1) * 128], in_=pt_[:, q, :], func=AF.Identity,
                               bias=binb[:, dc:dc + 1], scale=ging[:, dc:dc + 1], reads=[b_pt, b_ging, b_binb], writes=[b_xTg])
                return lnt

            def proj_fm(wt, b_wt, col0, ps_ap, b_ps, n, last=True):
                for dc in range(8):
                    OP("pe", "matmul", ps_ap, lhsT=wt[:, dc, col0:col0 + 128], rhs=xTg[:, dc, 0:n], start=(dc == 0), stop=(dc == 7),
                       reads=[b_xTg, b_wt], writes=[b_ps], inc=(dc == 7))

            def conv_u(n, ci, dst_ap):
                pc, b_pc = nextpA()
                proj_fm(wcv, b_wcv, 512 + ci * 128, pc[:, 0:n], b_pc, n)
                ph, b_ph = nextpA()
                proj_fm(wcv, b_wcv, 1024 + ci * 128, ph[:, 0:n], b_ph, n)
                OP("act", "copy", out=ccs[:, 0:n], in_=pc[:, 0:n], reads=[b_pc], writes=[b_ccs])
                return ph, b_ph

            group(0, 128, "halo")
            for ci in range(4):
                ph, b_ph = conv_u(128, ci, None)
                OP("dve", "tensor_tensor", out=yc[:, 0:8], in0=ph[:, 0:8], in1=ccs[:, 0:8], op=ALU.mult, reads=[b_ph, b_ccs], writes=[b_yc])
                OP("dve", "tensor_tensor", out=uh[:, ci, :], in0=yc[:, 0:8], in1=hm[:], op=ALU.mult, reads=[b_yc, b_hm], writes=[b_uh])

            def token_groups():
                for gi in range(8):
                    yield gi * 256, 256, "prompt", gi
                yield 2048, 128, "sample", 8

            for rows0, n, kind, gi in token_groups():
                lnt = group(rows0, n, kind)
                DMA("sp", yaT[:, :, 0:n], yat[:, :, rows0:rows0 + n].rearrange("h d t -> d h t"), reads=[b_yat], writes=[b_yaT])
                for ci in range(4):
                    ph, b_ph = conv_u(n, ci, None)
                    if kind == "prompt":
                        m, half = gi // 2, gi % 2
                        if half == 0:
                            OP("pool", "tensor_copy", out=ext[:, ci, 0:2], in_=uh[:, ci, 2 * m:2 * m + 2], reads=[b_uh], writes=[b_ext])
                        else:
                            OP("pool", "tensor_copy", out=ext[:, ci, 0:2], in_=ext[:, ci, n:n + 2], reads=[b_ext], writes=[b_ext])
                        OP("dve", "tensor_tensor", out=ext[:, ci, 2:n + 2], in0=ph[:, 0:n], in1=ccs[:, 0:n], op=ALU.mult,
                           reads=[b_ph, b_ccs], writes=[b_ext])
                        e0, e1, e2 = ext[:, ci, 0:n], ext[:, ci, 1:n + 1], ext[:, ci, 2:n + 2]
                        ycv = yc[:, 0:n]
                        if gi == 7:
                            OP("dve", "tensor_tensor", out=convp[:, ci, :], in0=ph[:, n - 2:n], in1=ccs[:, n - 2:n], op=ALU.mult,
                               reads=[b_ph, b_ccs], writes=[b_convp])
                    else:
                        OP("pool", "tensor_copy", out=exs[:, ci, :, 0:2], in_=ush[:, ci, :].rearrange("p (s r) -> p s r", r=2),
                           reads=[b_ush], writes=[b_exs])
                        OP("dve", "tensor_tensor", out=exs[:, ci, :, 2:6], in0=ph[:, 0:64].rearrange("p (s i) -> p s i", i=4),
                           in1=ccs[:, 0:64].rearrange("p (s i) -> p s i", i=4), op=ALU.mult, reads=[b_ph, b_ccs], writes=[b_exs])
                        e0, e1, e2 = exs[:, ci, :, 0:4], exs[:, ci, :, 1:5], exs[:, ci, :, 2:6]
                        ycv = yc[:, 0:64].rearrange("p (s i) -> p s i", i=4)
                        OP("dve", "tensor_tensor", out=convo[:, ci, :].rearrange("p (s r) -> p s r", r=2),
                           in0=ph[:, 0:64].rearrange("p (s i) -> p s i", i=4)[:, :, 2:4],
                           in1=ccs[:, 0:64].rearrange("p (s i) -> p s i", i=4)[:, :, 2:4], op=ALU.mult,
                           reads=[b_ph, b_ccs], writes=[b_convo])
                        OP("pool", "memset", yc[:, 64:128], 0.0, writes=[b_yc])
                    b_e = b_ext if kind == "prompt" else b_exs
                    OP("dve", "tensor_scalar_mul", out=ycv, in0=e0, scalar1=cw[:, 0, ci:ci + 1], reads=[b_e, b_cw], writes=[b_yc])
                    OP("dve", "scalar_tensor_tensor", out=ycv, in0=e1, scalar=cw[:, 1, ci:ci + 1], in1=ycv, op0=ALU.mult, op1=ALU.add,
                       reads=[b_e, b_cw, b_yc], writes=[b_yc])
                    OP("dve", "scalar_tensor_tensor", out=ycv, in0=e2, scalar=cw[:, 2, ci:ci + 1], in1=ycv, op0=ALU.mult, op1=ALU.add,
                       reads=[b_e, b_cw, b_yc], writes=[b_yc])
                    pb, b_pb = nextpA()
                    proj_fm(wcv, b_wcv, ci * 128, pb[:, 0:n], b_pb, n)
                    OP("dve", "tensor_tensor", out=ycT[:, ci, 0:n], in0=pb[:, 0:n], in1=yc[:, 0:n], op=ALU.mult,
                       reads=[b_pb, b_yc], writes=[b_ycT])
                for nc_ in range(8):
                    pg_, b_pg = nextpA()
                    proj_fm(wgc, b_wgc, nc_ * 128, pg_[:, 0:n], b_pg, n)
                    OP("act", "activation", out=sgc[:, 0:n], in_=pg_[:, 0:n], func=AF.Sigmoid, reads=[b_pg], writes=[b_sgc])
                    pg2, b_pg2 = nextpA()
                    proj_fm(wga, b_wga, nc_ * 128, pg2[:, 0:n], b_pg2, n)
                    OP("act", "activation", out=sga[:, 0:n], in_=pg2[:, 0:n], func=AF.Sigmoid, reads=[b_pg2], writes=[b_sga])
                    pbc, b_pbc = nextpA()
                    for ci in range(4):
                        OP("pe", "matmul", pbc[:, 0:n], lhsT=wbc[:, ci, nc_ * 128:(nc_ + 1) * 128], rhs=ycT[:, ci, 0:n],
                           start=(ci == 0), stop=(ci == 3), reads=[b_wbc, b_ycT], writes=[b_pbc], inc=(ci == 3))
                    pba, b_pba = nextpA()
                    for h in range(H):
                        OP("pe", "matmul", pba[:, 0:n], lhsT=wba[:, h, nc_ * 128:(nc_ + 1) * 128], rhs=yaT[:, h, 0:n],
                           start=(h == 0), stop=(h == 7), reads=[b_wba, b_yaT], writes=[b_pba], inc=(h == 7))
                    OP("dve", "tensor_tensor", out=t1[:, 0:n], in0=pbc[:, 0:n], in1=sgc[:, 0:n], op=ALU.mult, reads=[b_pbc, b_sgc], writes=[b_t1])
                    OP("dve", "tensor_tensor", out=sga[:, 0:n], in0=pba[:, 0:n], in1=sga[:, 0:n], op=ALU.mult, reads=[b_pba, b_sga], writes=[b_sga])
                    OP("pool", "tensor_tensor", out=mT[:, nc_, 0:n], in0=t1[:, 0:n], in1=sga[:, 0:n], op=ALU.add, reads=[b_t1, b_sga], writes=[b_mT])
                for ti in range(n // 128):
                    t = (rows0 // 128) + ti
                    xl, b_xl = lnt[ti]
                    x1_, b_x1 = x1t[t % 2]
                    for nh in range(2):
                        po_, b_po = nextpA()
                        for dc in range(8):
                            OP("pe", "matmul", po_[:], lhsT=mT[:, dc, ti * 128:(ti + 1) * 128], rhs=wo[:, dc, nh * 512:(nh + 1) * 512],
                               start=(dc == 0), stop=(dc == 7), reads=[b_mT, b_wo], writes=[b_po], inc=(dc == 7))
                        OP("dve", "scalar_tensor_tensor", out=x1_[:, nh * 512:(nh + 1) * 512], in0=xl[:, nh * 512:(nh + 1) * 512],
                           scalar=ALPHA, in1=po_[:], op0=ALU.mult, op1=ALU.add, reads=[b_xl, b_po], writes=[b_x1])
                    ln_stats(st6, b_st6, mv, b_mv, rstd, b_rstd, x1_, b_x1)
                    OP("dve", "tensor_scalar", out=x1_[:], in0=x1_[:], scalar1=mv[:, 0:1], scalar2=rstd[:, 0:1],
                       op0=ALU.subtract, op1=ALU.mult, reads=[b_x1, b_mv, b_rstd], writes=[b_x1])
                    OP("pool", "tensor_tensor", out=x1_[:], in0=x1_[:], in1=g1_bc[:], op=ALU.mult, reads=[b_x1, b_g1_bc], writes=[b_x1])
                    OP("pool", "tensor_tensor", out=x1_[:], in0=x1_[:], in1=b1_bc[:], op=ALU.add, reads=[b_x1, b_b1_bc], writes=[b_x1])
                    x1b_, b_x1b_ = x1bt[t % 2]
                    OP("pool", "tensor_copy", out=x1b_[:], in_=x1_[:], reads=[b_x1], writes=[b_x1b_])
                    DMA("sp", x1s[t * 128:(t + 1) * 128, :], x1_[:], reads=[b_x1, b_x1s], owner=b_x1)
                    DMA("sp", x1b[t * 128:(t + 1) * 128, :], x1b_[:], reads=[b_x1b_, b_x1b], owner=b_x1b_)
                    for half in range(2):
                        for q in range(4):
                            dc = half * 4 + q
                            OP("pe", "transpose", out=pX[:, q, :], in_=x1_[:, dc * 128:(dc + 1) * 128], identity=identf[:],
                               reads=[b_x1, b_identf], writes=[b_pX])
                        OP("act", "copy", out=x1T[:, half * 4:half * 4 + 4, :], in_=pX[:], reads=[b_pX], writes=[b_x1T])
                    for dc in range(8):
                        OP("pe", "matmul", pL[:, 0:32], lhsT=x1T[:, dc, :], rhs=wr[:, dc, :], start=(dc == 0), stop=(dc == 7),
                           reads=[b_x1T, b_wr], writes=[b_pL], inc=(dc == 7))
                    OP("dve", "tensor_tensor", out=lg[:], in0=pL[:, 0:32], in1=brb[:], op=ALU.add, reads=[b_pL, b_brb], writes=[b_lg])
                    OP("dve", "max", out=m8[:], in_=lg[:], reads=[b_lg], writes=[b_m8])
                    OP("dve", "tensor_scalar", out=msk[:], in0=lg[:], scalar1=m8[:, 3:4], scalar2=None, op0=ALU.is_ge,
                       reads=[b_lg, b_m8], writes=[b_msk])
                    OP("dve", "tensor_scalar_mul", out=sm[:, 0:1], in0=m8[:, 0:1], scalar1=-1.0, reads=[b_m8], writes=[b_sm])
                    OP("act", "activation", out=ex[:], in_=lg[:], func=AF.Exp, bias=sm[:, 0:1], scale=1.0, reads=[b_lg, b_sm], writes=[b_ex])
                    OP("dve", "tensor_tensor", out=ex[:], in0=ex[:], in1=msk[:], op=ALU.mult, reads=[b_ex, b_msk], writes=[b_ex])
                    OP("dve", "tensor_reduce", out=sm[:, 1:2], in_=ex[:], axis=AX.X, op=ALU.add, reads=[b_ex], writes=[b_sm])
                    OP("dve", "reciprocal", out=sm[:, 2:3], in_=sm[:, 1:2], reads=[b_sm], writes=[b_sm])
                    OP("dve", "tensor_scalar_mul", out=Gt[:], in0=ex[:], scalar1=sm[:, 2:3], reads=[b_ex, b_sm], writes=[b_Gt])
                    OP("pe", "matmul", pL[:, 32:64], lhsT=Lsf[:], rhs=msk[:], start=True, stop=False, reads=[b_Lsf, b_msk], writes=[b_pL], inc=False)
                    OP("pe", "matmul", pL[:, 32:64], lhsT=onesf[:], rhs=macc[:], start=False, stop=True, reads=[b_onesf, b_macc], writes=[b_pL])
                    OP("dve", "tensor_tensor", out=key[:], in0=pL[:, 32:64], in1=eoff[:], op=ALU.add, reads=[b_pL, b_eoff], writes=[b_key])
                    OP("dve", "tensor_tensor", out=key[:], in0=key[:], in1=msk[:], op=ALU.mult, reads=[b_key, b_msk], writes=[b_key])
                    OP("dve", "tensor_tensor", out=macc[:], in0=macc[:], in1=msk[:], op=ALU.add, reads=[b_macc, b_msk], writes=[b_macc])
                    OP("dve", "max", out=k8[:], in_=key[:], reads=[b_key], writes=[b_k8])
                    OP("dve", "tensor_scalar_add", out=sf[:], in0=k8[:, 0:4], scalar1=-1.0, reads=[b_k8], writes=[b_sf])
                    OP("dve", "tensor_copy", out=slot_i[:, t, :], in_=sf[:], reads=[b_sf], writes=[b_slot])
                    for k in range(4):
                        OP("dve", "tensor_scalar", out=oh[:], in0=key[:], scalar1=k8[:, k:k + 1], scalar2=None, op0=ALU.is_equal,
                           reads=[b_key, b_k8], writes=[b_oh])
                        OP("dve", "tensor_tensor", out=oh[:], in0=oh[:], in1=Gt[:], op=ALU.mult, reads=[b_oh, b_Gt], writes=[b_oh])
                        OP("dve", "tensor_reduce", out=gk[:, t, k:k + 1], in_=oh[:], axis=AX.X, op=ALU.add, reads=[b_oh], writes=[b_gk])
                        P.dma("pool", lambda e, t=t, k=k: e.indirect_dma_start(
                            out=tbl, out_offset=bass.IndirectOffsetOnAxis(ap=slot_i[:, t, k:k + 1], axis=0),
                            in_=tokid[:, t:t + 1], in_offset=None),
                            reads=[b_slot, b_tokid, b_tbl], owner=b_tbl)
            DMA("sp", o_convs.rearrange("(c p) s r -> p c (s r)", p=128), convo[:], reads=[b_convo])
            DMA("sp", o_convp.rearrange("(c p) r -> p c r", p=128), convp[:], reads=[b_convp])
            P.barrier()
            P.release(b_wcv, b_wgc, b_wga, b_wbc, b_wba, b_wo, b_wr, b_cw, b_hm, b_gin_bc, b_bin_bc, b_g1_bc, b_b1_bc, b_brb,
                      b_ztb, b_scT, b_yaT, b_convo, b_convp, *[b for _, b in xt4], *[b for _, b in x1t], *[b for _, b in x1bt])

        with contextlib.ExitStack() as SC:
            P.enabled = "C" in PHASES
            wgs = [SB(SC, "wg%d" % i, [128, 8, D], BF16, dma="sw") for i in range(2)]
            wus = [SB(SC, "wu%d" % i, [128, 8, D], BF16, dma="sw") for i in range(2)]
            wds = [SB(SC, "wd%d" % i, [128, 8, D], BF16, dma="sw") for i in range(2)]
            bgu = [SB(SC, "bgu%d" % i, [128, 2, 8], F32, dma=True) for i in range(2)]
            bdn = [SB(SC, "bdn%d" % i, [128, D], F32, dma=True) for i in range(2)]
            idx = [SB(SC, "idx%d" % i, [128, 3], I32, dma=True) for i in range(2)]
            xg = [SB(SC, "xg%d" % i, [128, 3, D], BF16, dma="sw") for i in range(2)]
            xgT, b_xgT = SB(SC, "xgT", [128, 8, CAP], BF16)
            hT, b_hT = SB(SC, "hT", [128, 8, CAP], BF16)
            gs_, b_gs = SB(SC, "gs", [128, CAP], F32)
            us_, b_us = SB(SC, "us", [128, CAP], F32)
            sg_, b_sg = SB(SC, "sg", [128, CAP], F32)
            yo = [SB(SC, "yo%d" % i, [128, D], BF16, dma=True) for i in range(2)]
            pTr = [PS(SC, "cpTr%d" % i, [128, 4, 128], BF16) for i in range(2)]
            pG = [PS(SC, "cpG%d" % i, [128, 512]) for i in range(2)]
            pU = [PS(SC, "cpU%d" % i, [128, 512]) for i in range(2)]
            pY = [PS(SC, "cpY%d" % i, [128, 512]) for i in range(2)]

            def load_expert(e):
                i = e % 2
                DMA("pool", wgs[i][0][:], wview(w_gate[min(e, ne_ - 1)]), writes=[wgs[i][1]])
                DMA("pool", wus[i][0][:], wview(w_up[min(e, ne_ - 1)]), writes=[wus[i][1]])
                DMA("pool", wds[i][0][:], wview(w_down[min(e, ne_ - 1)]), writes=[wds[i][1]])
                DMA("sp", bgu[i][0][:, 0, :], b_gate[e:e + 1, :].rearrange("o (c p) -> p (o c)", p=128), writes=[bgu[i][1]],
                    allow_slow_non_contiguous=True)
                DMA("sp", bgu[i][0][:, 1, :], b_up[e:e + 1, :].rearrange("o (c p) -> p (o c)", p=128), writes=[bgu[i][1]],
                    allow_slow_non_contiguous=True)
                DMA("sp", bdn[i][0][:], b_down[e:e + 1, :].broadcast_to([128, D]), writes=[bdn[i][1]])
                DMA("sp", idx[i][0][:], tbl[e * CAP:(e + 1) * CAP, :].rearrange("(j p) o -> p (j o)", p=128), reads=[b_tbl],
                    writes=[idx[i][1]], allow_slow_non_contiguous=True)
                for j in range(3):
                    P.dma("pool", lambda en, i=i, j=j: en.indirect_dma_start(
                        out=xg[i][0][:, j, :], out_offset=None, in_=x1b,
                        in_offset=bass.IndirectOffsetOnAxis(ap=idx[i][0][:, j:j + 1], axis=0)),
                        reads=[idx[i][1], b_x1b], writes=[xg[i][1]])

            load_expert(0)
            yo_i = 0
            for e in range(E):
                i = e % 2
                if e + 1 < E:
                    load_expert(e + 1)
                wg_, b_wg = wgs[i]; wu_, b_wu = wus[i]; wd_, b_wd = wds[i]
                xg_, b_xg = xg[i]
                for j in range(3):
                    for half in range(2):
                        pt_, b_pt = pTr[half]
                        for q in range(4):
                            dc = half * 4 + q
                            OP("pe", "transpose", out=pt_[:, q, :], in_=xg_[:, j, dc * 128:(dc + 1) * 128], identity=ident[:],
                               reads=[b_xg, b_ident], writes=[b_pt])
                        OP("act", "copy", out=xgT[:, half * 4:half * 4 + 4, j * 128:(j + 1) * 128], in_=pt_[:], reads=[b_pt], writes=[b_xgT])
                for fo in range(8):
                    pg_, b_pg = pG[fo % 2]
                    pu_, b_pu = pU[fo % 2]
                    for dc in range(8):
                        OP("pe", "matmul", pg_[:, 0:CAP], lhsT=wg_[:, dc, fo * 128:(fo + 1) * 128], rhs=xgT[:, dc, :],
                           start=(dc == 0), stop=(dc == 7), reads=[b_wg, b_xgT], writes=[b_pg], inc=(dc == 7))
                    for dc in range(8):
                        OP("pe", "matmul", pu_[:, 0:CAP], lhsT=wu_[:, dc, fo * 128:(fo + 1) * 128], rhs=xgT[:, dc, :],
                           start=(dc == 0), stop=(dc == 7), reads=[b_wu, b_xgT], writes=[b_pu], inc=(dc == 7))
                    OP("dve", "tensor_scalar", out=gs_[:], in0=pg_[:, 0:CAP], scalar1=bgu[i][0][:, 0, fo:fo + 1], scalar2=7.0,
                       op0=ALU.add, op1=ALU.min, reads=[b_pg, bgu[i][1]], writes=[b_gs])
                    OP("act", "activation", out=sg_[:], in_=gs_[:], func=AF.Sigmoid, scale=1.702, reads=[b_gs], writes=[b_sg])
                    OP("dve", "tensor_scalar", out=us_[:], in0=pu_[:, 0:CAP], scalar1=bgu[i][0][:, 1, fo:fo + 1], scalar2=7.0,
                       op0=ALU.add, op1=ALU.min, reads=[b_pu, bgu[i][1]], writes=[b_us])
                    OP("pool", "tensor_scalar", out=us_[:], in0=us_[:], scalar1=-7.0, scalar2=1.0, op0=ALU.max, op1=ALU.add,
                       reads=[b_us], writes=[b_us])
                    OP("pool", "tensor_tensor", out=gs_[:], in0=gs_[:], in1=sg_[:], op=ALU.mult, reads=[b_gs, b_sg], writes=[b_gs])
                    OP("pool", "tensor_tensor", out=hT[:, fo, :], in0=gs_[:], in1=us_[:], op=ALU.mult, reads=[b_gs, b_us], writes=[b_hT])
                for j in range(3):
                    yo_, b_yo = yo[yo_i % 2]; yo_i += 1
                    for nh in range(2):
                        py_, b_py = pY[nh]
                        for fo in range(8):
                            OP("pe", "matmul", py_[:], lhsT=hT[:, fo, j * 128:(j + 1) * 128], rhs=wd_[:, fo, nh * 512:(nh + 1) * 512],
                               start=(fo == 0), stop=(fo == 7), reads=[b_hT, b_wd], writes=[b_py], inc=(fo == 7))
                        OP("dve", "tensor_tensor", out=yo_[:, nh * 512:(nh + 1) * 512], in0=py_[:], in1=bdn[i][0][:, nh * 512:(nh + 1) * 512],
                           op=ALU.add, reads=[b_py, bdn[i][1]], writes=[b_yo])
                    r0 = e * CAP + j * 128
                    DMA("sp", ybuf[r0:r0 + 128, :], yo_[:], reads=[b_yo, b_ybuf], owner=b_yo)
            P.barrier()
            P.release(*[b for _, b in wgs], *[b for _, b in wus], *[b for _, b in wds], *[b for _, b in bgu], *[b for _, b in bdn],
                      *[b for _, b in idx], *[b for _, b in xg], *[b for _, b in yo])

        with contextlib.ExitStack() as SD:
            P.enabled = "D" in PHASES
            wpg, b_wpg = SB(SD, "wpg", [128, 8, D], BF16, dma="sw")
            wpp, b_wpp = SB(SD, "wpp", [128, 2, D], BF16, dma="sw")
            DMA("pool", wpg[:], wview(w_ple_gate), writes=[b_wpg])
            DMA("pool", wpp[:], wview(w_ple_proj), writes=[b_wpp])
            g2_bc, b_g2_bc = SB(SD, "g2_bc", [128, D], F32, dma=True)
            b2_bc, b_b2_bc = SB(SD, "b2_bc", [128, D], F32, dma=True)
            DMA("sp", g2_bc[:], ln2_g.broadcast_to([128, D]), writes=[b_g2_bc])
            DMA("sp", b2_bc[:], ln2_b.broadcast_to([128, D]), writes=[b_b2_bc])
            yk = [SB(SD, "yk%d" % i, [128, 4, D], BF16, dma="sw") for i in range(2)]
            x1r = [SB(SD, "x1r%d" % i, [128, D], F32, dma=True) for i in range(2)]
            pr = [SB(SD, "pr%d" % i, [128, 256], F32, dma=True) for i in range(2)]
            prb, b_prb = SB(SD, "prb", [128, 256], BF16)
            pT_, b_pT = SB(SD, "pTd", [128, 2, 128], BF16)
            acc_, b_acc = SB(SD, "accd", [128, D], F32)
            x2b, b_x2b = SB(SD, "x2b", [128, D], BF16)
            x2T, b_x2T = SB(SD, "x2T", [128, 8, 128], BF16)
            sgp, b_sgp = SB(SD, "sgp", [128, 512], F32)
            outs = [SB(SD, "outd%d" % i, [128, D], F32, dma=True) for i in range(2)]
            st6, b_st6 = SB(SD, "dst6", [128, 12], F32)
            mv, b_mv = SB(SD, "dmv", [128, 2], F32)
            rstd, b_rstd = SB(SD, "drstd", [128, 1], F32)
            pTr = [PS(SD, "dpTr%d" % i, [128, 4, 128], BF16) for i in range(2)]
            pGa = [PS(SD, "dpG%d" % i, [128, 512]) for i in range(2)]
            pPr = [PS(SD, "dpP%d" % i, [128, 512]) for i in range(2)]
            for t in range(NT):
                yk_, b_yk = yk[t % 2]
                for k in range(4):
                    P.dma("pool", lambda en, yk_=yk_, t=t, k=k: en.indirect_dma_start(
                        out=yk_[:, k, :], out_offset=None, in_=ybuf,
                        in_offset=bass.IndirectOffsetOnAxis(ap=slot_i[:, t, k:k + 1], axis=0)),
                        reads=[b_slot, b_ybuf], writes=[b_yk])
                x1_, b_x1 = x1r[t % 2]
                DMA("sp", x1_[:], x1s[t * 128:(t + 1) * 128, :], reads=[b_x1s], writes=[b_x1])
                p_, b_p = pr[t % 2]
                DMA("sp", p_[:], po[t * 128:(t + 1) * 128, :], writes=[b_p])
                OP("dve", "tensor_scalar_mul", out=acc_[:], in0=x1_[:], scalar1=ALPHA, reads=[b_x1], writes=[b_acc])
                for k in range(4):
                    OP("dve", "scalar_tensor_tensor", out=acc_[:], in0=yk_[:, k, :], scalar=gk[:, t, k:k + 1], in1=acc_[:],
                       op0=ALU.mult, op1=ALU.add, reads=[b_yk, b_gk, b_acc], writes=[b_acc])
                ln_stats(st6, b_st6, mv, b_mv, rstd, b_rstd, acc_, b_acc)
                OP("dve", "tensor_scalar", out=acc_[:], in0=acc_[:], scalar1=mv[:, 0:1], scalar2=rstd[:, 0:1],
                   op0=ALU.subtract, op1=ALU.mult, reads=[b_acc, b_mv, b_rstd], writes=[b_acc])
                OP("pool", "tensor_tensor", out=acc_[:], in0=acc_[:], in1=g2_bc[:], op=ALU.mult, reads=[b_acc, b_g2_bc], writes=[b_acc])
                OP("pool", "tensor_tensor", out=acc_[:], in0=acc_[:], in1=b2_bc[:], op=ALU.add, reads=[b_acc, b_b2_bc], writes=[b_acc])
                OP("pool", "tensor_copy", out=x2b[:], in_=acc_[:], reads=[b_acc], writes=[b_x2b])
                OP("pool", "tensor_copy", out=prb[:], in_=p_[:], reads=[b_p], writes=[b_prb])
                for half in range(2):
                    pt_, b_pt = pTr[half]
                    for q in range(4):
                        dc = half * 4 + q
                        OP("pe", "transpose", out=pt_[:, q, :], in_=x2b[:, dc * 128:(dc + 1) * 128], identity=ident[:],
                           reads=[b_x2b, b_ident], writes=[b_pt])
                    OP("act", "copy", out=x2T[:, half * 4:half * 4 + 4, :], in_=pt_[:], reads=[b_pt], writes=[b_x2T])
                pt_, b_pt = pTr[0]
                for q in range(2):
                    OP("pe", "transpose", out=pt_[:, q, :], in_=prb[:, q * 128:(q + 1) * 128], identity=ident[:],
                       reads=[b_prb, b_ident], writes=[b_pt])
                OP("act", "copy", out=pT_[:], in_=pt_[:, 0:2, :], reads=[b_pt], writes=[b_pT])
                o_, b_o = outs[t % 2]
                for nh in range(2):
                    pg_, b_pg = pGa[nh]
                    for dc in range(8):
                        OP("pe", "matmul", pg_[:], lhsT=x2T[:, dc, :], rhs=wpg[:, dc, nh * 512:(nh + 1) * 512], start=(dc == 0), stop=(dc == 7),
                           reads=[b_x2T, b_wpg], writes=[b_pg], inc=(dc == 7))
                    pp_, b_pp = pPr[nh]
                    for dc in range(2):
                        OP("pe", "matmul", pp_[:], lhsT=pT_[:, dc, :], rhs=wpp[:, dc, nh * 512:(nh + 1) * 512], start=(dc == 0), stop=(dc == 1),
                           reads=[b_pT, b_wpp], writes=[b_pp], inc=(dc == 1))
                    OP("act", "activation", out=sgp[:], in_=pg_[:], func=AF.Sigmoid, reads=[b_pg], writes=[b_sgp])
                    OP("dve", "tensor_tensor", out=sgp[:], in0=pp_[:], in1=sgp[:], op=ALU.mult, reads=[b_pp, b_sgp], writes=[b_sgp])
                    OP("dve", "tensor_tensor", out=o_[:, nh * 512:(nh + 1) * 512], in0=sgp[:], in1=acc_[:, nh * 512:(nh + 1) * 512], op=ALU.add,
                       reads=[b_sgp, b_acc], writes=[b_o])
                DMA("sp", o_y[t * 128:(t + 1) * 128, :], o_[:], reads=[b_o])
            P.barrier()

        P.enabled = True
        P.barrier()
        with nc.Block() as block:
            P.emit(block)
    P.close()
    return nc


_NC_CACHE = {}


def _prep_core(c, I):
    b, j = c // 4, c % 4
    f32 = np.float32
    xp = I["x_prompt"]; xs = I["x_sample"]
    Gs = [j + 4 * m for m in range(4)]
    xo = np.zeros((TOWN, D), f32)
    po = np.zeros((TOWN, 256), f32)
    xh = np.zeros((128, D), f32)
    hmask = np.zeros((128, 8), f32)
    sel = np.zeros((128, 4, 64), f32)
    kvis = np.zeros((128, 4, 64), f32)
    for m, G in enumerate(Gs):
        xo[m * 512:(m + 1) * 512] = xp[b, G * 512:(G + 1) * 512]
        po[m * 512:(m + 1) * 512] = I["p_prompt"][0, b, G * 512:(G + 1) * 512]
        if G > 0:
            xh[2 * m:2 * m + 2] = xp[b, G * 512 - 2:G * 512]
            hmask[:, 2 * m:2 * m + 2] = 1.0
        sel[127, m, 4 * G + 3] = 1.0
        kvis[:, m, 4 * G + 4:] = -30000.0
    xo[2048:2112] = xs[16 * c:16 * c + 16].reshape(64, D)
    po[2048:2112] = I["p_sample"][0, 16 * c:16 * c + 16].reshape(64, 256)
    dm = np.zeros((128, 16, 512), f32)
    kp = np.arange(128)[:, None]
    qi = np.arange(128)[None, :]
    tri = (kp <= qi).astype(f32)
    for r in range(16):
        rel = r - 4 * j
        if rel < 0:
            dm[:, r, :] = 1.0
        elif rel < 4:
            for qs in range(4):
                if qs > rel:
                    dm[:, r, qs * 128:(qs + 1) * 128] = 1.0
                elif qs == rel:
                    dm[:, r, qs * 128:(qs + 1) * 128] = tri
    bdm = np.zeros((64, 64), f32)
    for s in range(16):
        for i2 in range(4):
            for i1 in range(i2 + 1):
                bdm[4 * s + i1, 4 * s + i2] = 1.0
    return dict(
        xb=np.ascontiguousarray(xp[b]), xo=xo, po=po, xh=xh, hmask=hmask,
        dmask=dm.astype(ml_dtypes.bfloat16), sel=sel, kvis=kvis, bd=bdm,
        pt=np.ascontiguousarray(I["page_table"][16 * c:16 * c + 16]).reshape(1, 256).astype(np.int32),
        sconv=np.ascontiguousarray(I["state_conv"][0, 16 * c:16 * c + 16]).reshape(32, 512),
    )


def kernel(**I):
    I = {k: np.asarray(v) for k, v in I.items()}
    if "nc" not in _NC_CACHE:
        _NC_CACHE["nc"] = build()
    nc = _NC_CACHE["nc"]
    shared = dict(
        cache_k=I["cache_k"].reshape(NPOOLROWS, 512), cache_v=I["cache_v"].reshape(NPOOLROWS, 512),
        cache_f=I["cache_logf"].reshape(NPOOLROWS, 8),
        ln_in_g=I["ln_in_g"].reshape(1, D), ln_in_b=I["ln_in_b"].reshape(1, D),
        w_in=I["w_in"][0], b_f=I["b_f"].reshape(1, 8), conv_w=I["conv_w"][0],
        w_br_conv=I["w_br_conv"][0], w_br_attn=I["w_br_attn"][0], w_o=I["w_o"][0],
        ln1_g=I["ln1_g"].reshape(1, D), ln1_b=I["ln1_b"].reshape(1, D),
        w_router=I["w_router"][0], b_router=I["b_router"].reshape(1, E),
        w_gate=I["w_gate"][0], b_gate=I["b_gate"][0], w_up=I["w_up"][0], b_up=I["b_up"][0],
        w_down=I["w_down"][0], b_down=I["b_down"][0],
        ln2_g=I["ln2_g"].reshape(1, D), ln2_b=I["ln2_b"].reshape(1, D),
        w_ple_gate=I["w_ple_gate"][0], w_ple_proj=I["w_ple_proj"][0],
    )
    shared = {k: np.ascontiguousarray(v) for k, v in shared.items()}
    in_maps = []
    for c in range(8):
        d = dict(shared)
        d.update(_prep_core(c, I))
        in_maps.append(d)
    res = run_bass_kernel_spmd(nc, in_maps, core_ids=list(range(8)))
    R = res.results
    f32 = np.float32
    y_prompt = np.zeros((2, S, D), f32); y_sample = np.zeros((128, 4, D), f32)
    k_prompt = np.zeros((1, 2, S, H, HD), f32); v_prompt = np.zeros((1, 2, S, H, HD), f32)
    logf_prompt = np.zeros((1, 2, S, H), f32); conv_prompt = np.zeros((1, 2, 2, 512), f32)
    k_sample = np.zeros((1, 128, 4, H, HD), f32); v_sample = np.zeros((1, 128, 4, H, HD), f32)
    logf_sample = np.zeros((1, 128, 4, H), f32); conv_sample = np.zeros((1, 128, 2, 512), f32)
    for c in range(8):
        b, j = c // 4, c % 4
        r = R[c]
        oy = np.asarray(r["o_y"])
        for m in range(4):
            G = j + 4 * m
            y_prompt[b, G * 512:(G + 1) * 512] = oy[m * 512:(m + 1) * 512]
        y_sample[16 * c:16 * c + 16] = oy[2048:2112].reshape(16, 4, D)
        kT = np.asarray(r["o_kT"]); ov = np.asarray(r["o_v"]); olf = np.asarray(r["o_logf"])
        if j == 0:
            k_prompt[0, b] = kT[:, :S].T.reshape(S, H, HD)
            v_prompt[0, b] = ov[:S].reshape(S, H, HD)
            logf_prompt[0, b] = olf[:S]
        if j == 3:
            conv_prompt[0, b] = np.asarray(r["o_convp"]).T
        k_sample[0, 16 * c:16 * c + 16] = kT[:, S:S + 64].T.reshape(16, 4, H, HD)
        v_sample[0, 16 * c:16 * c + 16] = ov[S:S + 64].reshape(16, 4, H, HD)
        logf_sample[0, 16 * c:16 * c + 16] = olf[S:S + 64].reshape(16, 4, H)
        conv_sample[0, 16 * c:16 * c + 16] = np.transpose(np.asarray(r["o_convs"]), (1, 2, 0))
    return (y_prompt, y_sample, k_prompt, v_prompt, logf_prompt, conv_prompt,
            k_sample, v_sample, logf_sample, conv_sample)
```

```python
import contextlib
import numpy as np
import ml_dtypes
import concourse.bass as bass
import concourse.mybir as mybir
from concourse.bass_utils import run_bass_kernel_spmd

F32 = mybir.dt.float32
BF16 = mybir.dt.bfloat16
I32 = mybir.dt.int32
ALU = mybir.AluOpType
AF = mybir.ActivationFunctionType
AX = mybir.AxisListType

ENGS = ["pe", "act", "dve", "pool", "sp"]

D = 1024
S = 8192
NB = 64
H = 8
HD = 64
E = 32
CAP = 384
NT = 17
TOWN = NT * 128
NPOOLROWS = 2560 * 128
ALPHA = 2.0 ** 0.25
EPS = 1e-5


class Buf:
    __slots__ = ("name", "lw", "rd", "dsem", "excl")

    def __init__(self, name, dsem=None):
        self.name = name
        self.lw = {}
        self.rd = {}
        self.dsem = dsem
        self.excl = False


class Prog:
    def __init__(self, nc, n_dsem=96):
        self.nc = nc
        self.ops = {e: [] for e in ENGS}
        self.sem = []
        self.semctx = []
        self.esem = {}
        for e in ENGS:
            self.esem[e] = self._newsem("e_" + e)
        self.free_dsems = [self._newsem("d%d" % i) for i in range(n_dsem - 24)]
        self.free_swsems = [self._newsem("w%d" % i) for i in range(24)]
        self.swset = set(self.free_swsems)
        self.val = [0] * len(self.sem)
        self.waited = {e: {} for e in ENGS}
        self.enabled = True
        self.dead = False

    def _newsem(self, name):
        ctx = self.nc.semaphore(name)
        h = ctx.__enter__()
        self.semctx.append(ctx)
        self.sem.append(h)
        return len(self.sem) - 1

    def close(self):
        for c in reversed(self.semctx):
            c.__exit__(None, None, None)

    def buf(self, name, dma=False):
        d = None
        if dma == "sw":
            d = self.free_swsems.pop()
        elif dma:
            d = self.free_dsems.pop()
        return Buf(name, d)

    def release(self, *bufs):
        for b in bufs:
            if b.dsem is not None:
                (self.free_swsems if b.dsem in self.swset else self.free_dsems).append(b.dsem)
                b.dsem = None

    def _waits(self, eng, reads, writes):
        w = {}
        for b in reads:
            for s, v in b.lw.items():
                if w.get(s, 0) < v:
                    w[s] = v
            if b.excl:
                for s, v in b.rd.items():
                    if w.get(s, 0) < v:
                        w[s] = v
        for b in writes:
            for s, v in b.lw.items():
                if w.get(s, 0) < v:
                    w[s] = v
            for s, v in b.rd.items():
                if w.get(s, 0) < v:
                    w[s] = v
        out = []
        wd = self.waited[eng]
        for s, v in w.items():
            if eng == "pe" and s == self.esem["pe"]:
                continue
            if wd.get(s, 0) >= v:
                continue
            wd[s] = v
            out.append((s, v))
        return out

    def _commit(self, tok, reads, writes):
        s, v = tok
        for b in writes:
            b.lw = {s: v}
            b.rd = {}
        for b in reads:
            if b.rd.get(s, 0) < v:
                b.rd[s] = v

    def op(self, eng, fn, reads=(), writes=(), inc=True):
        if not self.enabled or self.dead:
            return None
        waits = self._waits(eng, reads, writes)
        s = self.esem[eng]
        if inc:
            self.val[s] += 1
            tok = (s, self.val[s])
        else:
            tok = (s, self.val[s] + 1)
        self.ops[eng].append((fn, waits, (s, 1) if inc else None))
        self._commit(tok, reads, writes)
        return tok

    def dma(self, eng, fn, reads=(), writes=(), owner=None):
        if not self.enabled or self.dead:
            return None
        if owner is None:
            for b in list(writes) + list(reads):
                if b.dsem is not None:
                    owner = b
                    break
        assert owner is not None and owner.dsem is not None, "dma needs owner"
        waits = self._waits(eng, reads, writes)
        s = owner.dsem
        self.val[s] += 16
        tok = (s, self.val[s])
        self.ops[eng].append((fn, waits, (s, 16)))
        self._commit(tok, reads, writes)
        return tok

    def barrier(self):
        for e in ENGS:
            waits = []
            wd = self.waited[e]
            for s in range(len(self.sem)):
                v = self.val[s]
                if v > 0 and wd.get(s, 0) < v:
                    if e == "pe" and s == self.esem["pe"]:
                        continue
                    wd[s] = v
                    waits.append((s, v))
            if waits:
                self.ops[e].append((None, waits, None))

    def emit(self, block):
        sem = self.sem

        def run(engname):
            def body(engine):
                for fn, waits, inc in self.ops[engname]:
                    for s, v in waits:
                        engine.wait_ge(sem[s], v)
                    if fn is not None:
                        ins = fn(engine)
                        if inc is not None:
                            ins.then_inc(sem[inc[0]], inc[1])
            return body

        block.tensor(run("pe"))
        block.scalar(run("act"))
        block.vector(run("dve"))
        block.gpsimd(run("pool"))
        block.sync(run("sp"))


PHASES = ["A", "A3", "A4", "B", "C", "D"]
A1_BLOCKS = list(range(65))
A2_TILES = list(range(NT))
STOPAT = None
A4_SEQS = list(range(16))


def build():
    nc = bass.Bass("TRN2", target_bir_lowering=False)

    def din(name, shape, dt=F32):
        return nc.dram_tensor(name, shape, dt, kind="ExternalInput").ap()

    def dout(name, shape, dt=F32):
        return nc.dram_tensor(name, shape, dt, kind="ExternalOutput").ap()

    def dscr(name, shape, dt):
        return nc.dram_tensor(name, shape, dt, kind="Internal").ap()

    xb = din("xb", [S, D])
    xo = din("xo", [TOWN, D])
    po = din("po", [TOWN, 256])
    xh = din("xh", [128, D])
    hmask = din("hmask", [128, 8])
    dmask = din("dmask", [128, 16, 512], BF16)
    sel = din("sel", [128, 4, 64])
    kvis = din("kvis", [128, 4, 64])
    bd = din("bd", [64, 64])
    pt = din("pt", [1, 256], I32)
    sconv = din("sconv", [32, 512])
    npool_ = NPOOLROWS if "A4" in PHASES else 128
    ne_ = E if "C" in PHASES else 1
    cache_k = din("cache_k", [npool_, 512])
    cache_v = din("cache_v", [npool_, 512])
    cache_f = din("cache_f", [npool_, 8])
    ln_in_g = din("ln_in_g", [1, D]); ln_in_b = din("ln_in_b", [1, D])
    w_in = din("w_in", [D, 5128])
    b_f = din("b_f", [1, 8])
    conv_w = din("conv_w", [3, 512])
    w_br_conv = din("w_br_conv", [512, D])
    w_br_attn = din("w_br_attn", [512, D])
    w_o = din("w_o", [D, D])
    ln1_g = din("ln1_g", [1, D]); ln1_b = din("ln1_b", [1, D])
    w_router = din("w_router", [D, E]); b_router = din("b_router", [1, E])
    w_gate = din("w_gate", [ne_, D, D]); b_gate = din("b_gate", [E, D])
    w_up = din("w_up", [ne_, D, D]); b_up = din("b_up", [E, D])
    w_down = din("w_down", [ne_, D, D]); b_down = din("b_down", [E, D])
    ln2_g = din("ln2_g", [1, D]); ln2_b = din("ln2_b", [1, D])
    w_ple_gate = din("w_ple_gate", [D, D])
    w_ple_proj = din("w_ple_proj", [256, D])

    o_y = dout("o_y", [TOWN, D])
    o_kT = dout("o_kT", [512, 65 * 128])
    o_v = dout("o_v", [65 * 128, 512])
    o_logf = dout("o_logf", [65 * 128, 8])
    o_convs = dout("o_convs", [512, 16, 2])
    o_convp = dout("o_convp", [512, 2])

    yat = dscr("yat", [H, 64, TOWN], BF16)
    x1s = dscr("x1s", [TOWN, D], F32)
    x1b = dscr("x1b", [TOWN, D], BF16)
    tbl = dscr("tbl", [E * CAP, 1], I32)
    ybuf = dscr("ybuf", [E * CAP, D], BF16)

    C_CB, C_CC, C_CH, C_Q, C_K, C_V, C_F, C_GC, C_GA = 0, 512, 1024, 1536, 2048, 2560, 3072, 3080, 4104

    P = Prog(nc)
    print("sbuf bytes/partition at start:", nc.sbuf_bytes_remaining, flush=True)

    def OP(eng, method, *args, reads=(), writes=(), inc=True, **kw):
        return P.op(eng, lambda e: getattr(e, method)(*args, **kw), reads, writes, inc)

    def DMA(eng, out, in_, reads=(), writes=(), owner=None, **kw):
        return P.dma(eng, lambda e: e.dma_start(out=out, in_=in_, **kw), reads, writes, owner)

    def CK(name):
        if STOPAT == name:
            P.dead = True

    def wview(ap2d):
        return ap2d.rearrange("(c p) n -> p c n", p=128)

    b_yat = P.buf("yat", dma=True)
    b_x1s = P.buf("x1s", dma=True)
    b_x1b = P.buf("x1b", dma=True)
    b_tbl = P.buf("tbl", dma="sw")
    b_ybuf = P.buf("ybuf", dma=True)
    b_out = P.buf("outs", dma=True)

    with contextlib.ExitStack() as S0:
        def SB(st, name, shape, dt, dma=False):
            t = st.enter_context(nc.sbuf_tensor(name, shape, dt))
            return t, P.buf(name, dma=dma)

        def PS(st, name, shape, dt=F32):
            full = 512 if dt == F32 else 1024
            t = st.enter_context(nc.psum_tensor(name, [128, full], dt))
            n = int(np.prod(shape[1:]))
            v = t[0:shape[0], 0:n]
            if len(shape) == 3:
                v = v.rearrange("p (a b) -> p a b", b=shape[2])
            pb_ = P.buf(name)
            pb_.excl = True
            return v, pb_

        identf, b_identf = SB(S0, "identf", [128, 128], F32)
        ident, b_ident = SB(S0, "ident", [128, 128], BF16)
        onesf, b_onesf = SB(S0, "onesf", [128, 128], F32)
        Uf, b_Uf = SB(S0, "Uf", [128, 128], F32)
        Lsf, b_Lsf = SB(S0, "Lsf", [128, 128], F32)
        Gsf, b_Gsf = SB(S0, "Gsf", [128, 128], F32)
        ging, b_ging = SB(S0, "ging", [128, 8], F32, dma=True)
        binb, b_binb = SB(S0, "binb", [128, 8], F32, dma=True)
        slot_i, b_slot = SB(S0, "slot_i", [128, NT, 4], I32)
        gk, b_gk = SB(S0, "gk", [128, NT, 4], F32)
        tokid, b_tokid = SB(S0, "tokid", [128, NT], I32)
        KTn, b_KTn = SB(S0, "KTn", [128, 4, 128], BF16)
        Vn, b_Vn = SB(S0, "Vn", [64, 8, 65], BF16)
        QTn, b_QTn = SB(S0, "QTn", [128, 4, 64], BF16)
        lfn, b_lfn = SB(S0, "lfn", [64, 8], F32)

        OP("pool", "memset", onesf[:], 1.0, writes=[b_onesf])
        for t_, b_, cmp_, pat_, cm_ in ((identf, b_identf, ALU.is_equal, [[-1, 128]], 1), (Uf, b_Uf, ALU.is_ge, [[1, 128]], -1),
                                        (Lsf, b_Lsf, ALU.is_gt, [[1, 128]], -1), (Gsf, b_Gsf, ALU.is_gt, [[-1, 128]], 1)):
            OP("pool", "memset", t_[:], 1.0, writes=[b_])
            OP("pool", "affine_select", out=t_[:], in_=t_[:], pattern=pat_, compare_op=cmp_, fill=0.0,
               base=0, channel_multiplier=cm_, reads=[b_], writes=[b_])
        OP("dve", "tensor_copy", out=ident[:], in_=identf[:], reads=[b_identf], writes=[b_ident])
        OP("pool", "iota", tokid[:], pattern=[[128, NT]], base=0, channel_multiplier=1, writes=[b_tokid])
        DMA("sp", ging[:], ln_in_g.rearrange("o (c p) -> p (o c)", p=128), writes=[b_ging], allow_slow_non_contiguous=True)
        DMA("sp", binb[:], ln_in_b.rearrange("o (c p) -> p (o c)", p=128), writes=[b_binb], allow_slow_non_contiguous=True)

        def ln_stats(st6, b_st6, mv, b_mv, rstd, b_rstd, xt_ap, b_xt):
            OP("dve", "bn_stats", out=st6[:, 0:6], in_=xt_ap[:, 0:512], reads=[b_xt], writes=[b_st6])
            OP("dve", "bn_stats", out=st6[:, 6:12], in_=xt_ap[:, 512:1024], reads=[b_xt], writes=[b_st6])
            OP("dve", "bn_aggr", out=mv[:], in_=st6[:], reads=[b_st6], writes=[b_mv])
            OP("dve", "tensor_scalar_add", out=rstd[:], in0=mv[:, 1:2], scalar1=EPS, reads=[b_mv], writes=[b_rstd])
            OP("act", "sqrt", out=rstd[:], in_=rstd[:], reads=[b_rstd], writes=[b_rstd])
            OP("dve", "reciprocal", out=rstd[:], in_=rstd[:], reads=[b_rstd], writes=[b_rstd])

        with contextlib.ExitStack() as SA:
            P.enabled = "A" in PHASES
            KT, _ = SB(SA, "KT", [128, 4, 65 * 128], BF16)
            KTb = [P.buf("KT%d" % g) for g in range(65)]
            V, _ = SB(SA, "V", [128, 65, 8, 65], BF16)
            Vb = [P.buf("V%d" % g) for g in range(65)]
            QT, _ = SB(SA, "QT", [128, 4, TOWN], BF16)
            QTb = [P.buf("QT%d" % t) for t in range(NT)]
            tf, b_tf = SB(SA, "tf", [128, 65, 8], F32, dma=True)
            cc, b_cc = SB(SA, "cc", [128, 64, 8], F32)
            bfb, b_bfb = SB(SA, "bfb", [128, 8], F32, dma=True)
            DMA("sp", bfb[:], b_f.broadcast_to([128, 8]), writes=[b_bfb])
            if len(A1_BLOCKS) < 65:
                OP("pool", "memset", tf[:], 0.0, writes=[b_tf])
                OP("pool", "memset", KT[:], 0.0, writes=KTb)
                OP("pool", "memset", V[:], 0.0, writes=Vb)
                OP("pool", "memset", QT[:], 0.0, writes=QTb)
            if "noVones" not in PHASES:
                OP("pool", "memset", V[:, :, :, 64:65], 1.0, writes=Vb)

            with contextlib.ExitStack() as SA1:
                wk, b_wk = SB(SA1, "wk", [128, 8, 512], BF16, dma="sw")
                wv, b_wv = SB(SA1, "wv", [128, 8, 512], BF16, dma="sw")
                wq, b_wq = wk, b_wk
                wf, b_wf = SB(SA1, "wf", [128, 8, 8], BF16, dma="sw")
                DMA("pool", wk[:], wview(w_in[:, C_K:C_K + 512]), writes=[b_wk])
                DMA("pool", wv[:], wview(w_in[:, C_V:C_V + 512]), writes=[b_wv])
                DMA("pool", wf[:], wview(w_in[:, C_F:C_F + 8]), writes=[b_wf])
                CK("wload")
                NXB = 2
                xts = [SB(SA1, "xt%d" % i, [128, D], F32, dma=True) for i in range(NXB)]
                xns = [SB(SA1, "xn%d" % i, [128, D], BF16) for i in range(2)]
                xTs = [SB(SA1, "xT%d" % i, [128, 8, 128], BF16) for i in range(2)]
                st6s = [SB(SA1, "st6_%d" % i, [128, 12], F32) for i in range(2)]
                mvs = [SB(SA1, "mv%d" % i, [128, 2], F32) for i in range(2)]
                rstds = [SB(SA1, "rstd%d" % i, [128, 1], F32) for i in range(2)]
                kTo = [SB(SA1, "kTo%d" % i, [128, 4, 128], F32, dma=True) for i in range(2)]
                vo = [SB(SA1, "vo%d" % i, [128, 512], F32, dma=True) for i in range(2)]
                pTr = [PS(SA1, "pTr%d" % i, [128, 4, 128], BF16) for i in range(2)]
                pK = [PS(SA1, "pK%d" % i, [128, 4, 128]) for i in range(2)]
                pV = [PS(SA1, "pV%d" % i, [128, 512]) for i in range(2)]
                pF, b_pF = PS(SA1, "pF", [128, 8])

                def ln_xT(i, src_ap):
                    xt, b_xt = xts[i % NXB]
                    xn, b_xn = xns[i % 2]
                    xT, b_xT = xTs[i % 2]
                    st6, b_st6 = st6s[i % 2]; mv, b_mv = mvs[i % 2]; rstd, b_rstd = rstds[i % 2]
                    DMA("sp", xt[:], src_ap, writes=[b_xt])
                    ln_stats(st6, b_st6, mv, b_mv, rstd, b_rstd, xt, b_xt)
                    OP("dve", "tensor_scalar", out=xn[:], in0=xt[:], scalar1=mv[:, 0:1], scalar2=rstd[:, 0:1],
                       op0=ALU.subtract, op1=ALU.mult, reads=[b_xt, b_mv, b_rstd], writes=[b_xn])
                    for half in range(2):
                        pt_, b_pt = pTr[half]
                        for q in range(4):
                            dc = half * 4 + q
                            OP("pe", "transpose", out=pt_[:, q, :], in_=xn[:, dc * 128:(dc + 1) * 128], identity=ident[:],
                               reads=[b_xn, b_ident], writes=[b_pt])
                        for q in range(4):
                            dc = half * 4 + q
                            OP("act", "activation", out=xT[:, dc, :], in_=pt_[:, q, :], func=AF.Identity,
                               bias=binb[:, dc:dc + 1], scale=ging[:, dc:dc + 1],
                               reads=[b_pt, b_ging, b_binb], writes=[b_xT])
                    return xT, b_xT

                for g in A1_BLOCKS:
                    src = xb[g * 128:(g + 1) * 128, :] if g < 64 else xo[2048:2176, :]
                    xT, b_xT = ln_xT(g, src)
                    CK("lnxT")
                    pk, b_pk = pK[g % 2]
                    for hp in range(4):
                        for dc in range(8):
                            OP("pe", "matmul", pk[:, hp, :], lhsT=wk[:, dc, hp * 128:(hp + 1) * 128], rhs=xT[:, dc, :],
                               start=(dc == 0), stop=(dc == 7), reads=[b_xT, b_wk], writes=[b_pk],
                               inc=(hp == 3 and dc == 7))
                    pv, b_pv = pV[g % 2]
                    for dc in range(8):
                        OP("pe", "matmul", pv[:], lhsT=xT[:, dc, :], rhs=wv[:, dc, :], start=(dc == 0), stop=(dc == 7),
                           reads=[b_xT, b_wv], writes=[b_pv], inc=(dc == 7))
                    for dc in range(8):
                        OP("pe", "matmul", pF[:], lhsT=xT[:, dc, :], rhs=wf[:, dc, :], start=(dc == 0), stop=(dc == 7),
                           reads=[b_xT, b_wf], writes=[b_pF], inc=(dc == 7))
                    CK("mm")
                    OP("dve", "tensor_copy", out=KT[:, :, g * 128:(g + 1) * 128], in_=pk[:], reads=[b_pk], writes=[KTb[g]])
                    CK("ktcopy")
                    ko, b_ko = kTo[g % 2]
                    OP("act", "copy", out=ko[:], in_=pk[:], reads=[b_pk], writes=[b_ko])
                    DMA("sp", o_kT[:, g * 128:(g + 1) * 128].rearrange("(c p) t -> p c t", p=128), ko[:], reads=[b_ko])
                    CK("kout")
                    OP("dve", "tensor_copy", out=V[:, g, :, 0:64], in_=pv[:].rearrange("p (h d) -> p h d", d=64),
                       reads=[b_pv], writes=[Vb[g]])
                    vo_, b_vo = vo[g % 2]
                    OP("act", "copy", out=vo_[:], in_=pv[:], reads=[b_pv], writes=[b_vo])
                    DMA("sp", o_v[g * 128:(g + 1) * 128, :], vo_[:], reads=[b_vo])
                    OP("dve", "tensor_tensor", out=tf[:, g, :], in0=pF[:], in1=bfb[:], op=ALU.add,
                       reads=[b_pF, b_bfb], writes=[b_tf])
                    CK("evac")
                CK("a1loop")
                tf2 = tf[:].rearrange("p g h -> p (g h)")
                OP("act", "activation", out=tf2, in_=tf2, func=AF.Exp, scale=-1.0, reads=[b_tf], writes=[b_tf])
                OP("act", "activation", out=tf2, in_=tf2, func=AF.Ln, bias=1.0, scale=1.0, reads=[b_tf], writes=[b_tf])
                OP("dve", "tensor_scalar_mul", out=tf2, in0=tf2, scalar1=-1.0, reads=[b_tf], writes=[b_tf])
                if "noLogfOut" not in PHASES:
                    for g in range(65):
                        DMA("sp", o_logf[g * 128:(g + 1) * 128, :], tf[:, g, :], reads=[b_tf])
                OP("pool", "tensor_copy", out=KTn[:], in_=KT[:, :, 8192:8320], reads=[KTb[64]], writes=[b_KTn])
                OP("pool", "tensor_copy", out=Vn[:], in_=V[0:64, 64, :, :], reads=[Vb[64]], writes=[b_Vn])
                OP("pool", "tensor_copy", out=lfn[:], in_=tf[0:64, 64, :], reads=[b_tf], writes=[b_lfn])

                CK("logf")
                pC, b_pC = pV[0]
                pTt, b_pTt = pV[1]
                lf512 = tf[:, 0:64, :].rearrange("p g h -> p (g h)")
                OP("pe", "matmul", pC[:], lhsT=Uf[:], rhs=lf512, start=True, stop=True, reads=[b_Uf, b_tf], writes=[b_pC])
                OP("pe", "matmul", pTt[:], lhsT=onesf[:], rhs=lf512, start=True, stop=True, reads=[b_onesf, b_tf], writes=[b_pTt])
                sa, b_sa = SB(SA1, "sa", [128, 64, 8], F32)
                sb_, b_sb = SB(SA1, "sbb", [128, 64, 8], F32)
                tot, b_tot = SB(SA1, "tot", [128, 64, 8], F32)
                OP("dve", "tensor_copy", out=tot[:].rearrange("p g h -> p (g h)"), in_=pTt[:], reads=[b_pTt], writes=[b_tot])
                OP("dve", "tensor_copy", out=sa[:], in_=tot[:], reads=[b_tot], writes=[b_sa])
                cur, b_cur, nxt, b_nxt = sa, b_sa, sb_, b_sb
                for sft in (1, 2, 4, 8, 16, 32):
                    OP("dve", "tensor_tensor", out=nxt[:, sft:64, :], in0=cur[:, sft:64, :], in1=cur[:, 0:64 - sft, :], op=ALU.add,
                       reads=[b_cur], writes=[b_nxt])
                    OP("dve", "tensor_copy", out=nxt[:, 0:sft, :], in_=cur[:, 0:sft, :], reads=[b_cur], writes=[b_nxt])
                    cur, b_cur, nxt, b_nxt = nxt, b_nxt, cur, b_cur
                OP("dve", "tensor_tensor", out=cur[:], in0=cur[:], in1=tot[:], op=ALU.subtract, reads=[b_cur, b_tot], writes=[b_cur])
                OP("dve", "tensor_tensor", out=cc[:].rearrange("p g h -> p (g h)"), in0=pC[:],
                   in1=cur[:].rearrange("p g h -> p (g h)"), op=ALU.add, reads=[b_pC, b_cur], writes=[b_cc])

                CK("cumsum")
                DMA("pool", wq[:], wview(w_in[:, C_Q:C_Q + 512]), writes=[b_wq])
                for t in A2_TILES:
                    xT, b_xT = ln_xT(65 + t, xo[t * 128:(t + 1) * 128, :])
                    pk, b_pk = pK[t % 2]
                    for hp in range(4):
                        for dc in range(8):
                            OP("pe", "matmul", pk[:, hp, :], lhsT=wq[:, dc, hp * 128:(hp + 1) * 128], rhs=xT[:, dc, :],
                               start=(dc == 0), stop=(dc == 7), reads=[b_xT, b_wq], writes=[b_pk],
                               inc=(hp == 3 and dc == 7))
                    OP("dve", "tensor_scalar_mul", out=QT[:, :, t * 128:(t + 1) * 128], in0=pk[:], scalar1=0.125,
                       reads=[b_pk], writes=[QTb[t]])
                OP("pool", "tensor_copy", out=QTn[:], in_=QT[:, :, 2048:2112], reads=[QTb[16]], writes=[b_QTn])
                P.barrier()
                P.release(b_wk, b_wv, b_wf, *[b for _, b in xts], *[b for _, b in kTo], *[b for _, b in vo])

            with contextlib.ExitStack() as SA3:
                P.enabled = "A3" in PHASES
                dm, b_dm = SB(SA3, "dm", [128, 16, 512], BF16, dma=True)
                selt, b_selt = SB(SA3, "selt", [128, 4, 64], F32, dma=True)
                DMA("sp", dm[:], dmask, writes=[b_dm])
                DMA("sp", selt[:], sel, writes=[b_selt])
                kvt, b_kvt = SB(SA3, "kvt", [128, 4, 64], F32, dma=True)
                DMA("sp", kvt[:], kvis, writes=[b_kvt])
                tmpc, b_tmpc = SB(SA3, "tmpc", [128, 64, 8], F32)
                red, b_red = SB(SA3, "red", [128, 8], F32)
                biasm = [SB(SA3, "biasm%d" % i, [128, 64, 8], F32) for i in range(2)]
                pTs = [SB(SA3, "pTs%d" % i, [128, 512], BF16) for i in range(3)]
                rs, b_rs = SB(SA3, "rs", [128, 512], F32)
                bcs, b_bcs = SB(SA3, "bcs", [64, 512], F32)
                yTs = [SB(SA3, "yTs%d" % i, [64, 512], BF16, dma=True) for i in range(2)]
                pS = [PS(SA3, "pS%d" % i, [128, 512]) for i in range(3)]
                pAcc = [PS(SA3, "pAcc%d" % i, [128, 512]) for i in range(2)]
                pB, b_pB = PS(SA3, "pB", [128, 512])
                pSh, b_pSh = PS(SA3, "pSh", [128, 8])
                it = 0
                for m in range(4):
                    bm, b_bm = biasm[m % 2]
                    OP("dve", "tensor_tensor", out=tmpc[:], in0=cc[:], in1=selt[:, m, :].unsqueeze(2).broadcast_to([128, 64, 8]),
                       op=ALU.mult, reads=[b_cc, b_selt], writes=[b_tmpc])
                    OP("dve", "tensor_reduce", out=red[:], in_=tmpc[:].rearrange("p g h -> p h g"), axis=AX.X, op=ALU.add,
                       reads=[b_tmpc], writes=[b_red])
                    OP("pe", "matmul", pSh[:], lhsT=onesf[:], rhs=red[:], start=True, stop=True, reads=[b_onesf, b_red], writes=[b_pSh])
                    OP("dve", "tensor_tensor", out=bm[:], in0=pSh[:].unsqueeze(1).broadcast_to([128, 64, 8]), in1=cc[:],
                       op=ALU.subtract, reads=[b_pSh, b_cc], writes=[b_bm])
                    OP("dve", "tensor_tensor", out=bm[:], in0=bm[:], in1=kvt[:, m, :].unsqueeze(2).broadcast_to([128, 64, 8]),
                       op=ALU.add, reads=[b_bm, b_kvt], writes=[b_bm])
                    nkb = 16 * m + 16
                    for h in range(H):
                        hp, hd0 = h // 2, (h % 2) * 64
                        acc, b_acc = pAcc[(m * H + h) % 2]

                        def s_mm(kb, it_):
                            ps_, b_ps = pS[it_ % 3]
                            OP("pe", "matmul", ps_[:], lhsT=KT[hd0:hd0 + 64, hp, kb * 128:(kb + 1) * 128],
                               rhs=QT[hd0:hd0 + 64, hp, m * 512:(m + 1) * 512], start=True, stop=True,
                               reads=[KTb[kb]] + QTb[4 * m:4 * m + 4], writes=[b_ps])
                        s_mm(0, it)
                        for kb in range(nkb):
                            if kb + 1 < nkb:
                                s_mm(kb + 1, it + 1)
                            ps_, b_ps = pS[it % 3]
                            pT_, b_pT = pTs[it % 3]
                            OP("act", "activation", out=pT_[:], in_=ps_[:], func=AF.Exp, bias=bm[:, kb, h:h + 1], scale=1.0,
                               reads=[b_ps, b_bm], writes=[b_pT])
                            if kb >= 16 * m:
                                OP("pool", "tensor_tensor", out=pT_[:], in0=pT_[:], in1=dm[:, kb - 16 * m, :], op=ALU.mult,
                                   reads=[b_pT, b_dm], writes=[b_pT])
                            OP("pe", "matmul", acc[0:65, :], lhsT=V[:, kb, h, :], rhs=pT_[:], start=(kb == 0), stop=(kb == nkb - 1),
                               reads=[Vb[kb], b_pT], writes=[b_acc], inc=(kb == nkb - 1))
                            it += 1
                        OP("dve", "reciprocal", out=rs[64:65, :], in_=acc[64:65, :], reads=[b_acc], writes=[b_rs])
                        OP("pe", "matmul", pB[0:64, :], lhsT=onesf[64:65, 0:64], rhs=rs[64:65, :], start=True, stop=True,
                           reads=[b_onesf, b_rs], writes=[b_pB])
                        OP("act", "copy", out=bcs[:], in_=pB[0:64, :], reads=[b_pB], writes=[b_bcs])
                        yT_, b_yT = yTs[h % 2]
                        OP("dve", "tensor_tensor", out=yT_[:], in0=acc[0:64, :], in1=bcs[:], op=ALU.mult,
                           reads=[b_acc, b_bcs], writes=[b_yT])
                        DMA("sp", yat[h, :, m * 512:(m + 1) * 512], yT_[:], reads=[b_yT, b_yat], owner=b_yT)
                P.barrier()
                P.release(b_dm, b_selt, *[b for _, b in yTs])
            P.release(b_tf, b_bfb)

        with contextlib.ExitStack() as S4:
            P.enabled = "A4" in PHASES
            ptb, b_ptb = SB(S4, "ptb", [128, 256], I32, dma=True)
            DMA("sp", ptb[:], pt.broadcast_to([128, 256]), writes=[b_ptb])
            pio, b_pio = SB(S4, "pio", [128, 1], I32)
            OP("pool", "iota", pio[:], pattern=[[0, 1]], base=0, channel_multiplier=1, writes=[b_pio])
            ridx, b_ridx = SB(S4, "ridx", [128, 256], I32)
            ptf, b_ptf = SB(S4, "ptf", [128, 256], F32)
            piof, b_piof = SB(S4, "piof", [128, 1], F32)
            OP("dve", "tensor_copy", out=ptf[:], in_=ptb[:], reads=[b_ptb], writes=[b_ptf])
            OP("dve", "tensor_copy", out=piof[:], in_=pio[:], reads=[b_pio], writes=[b_piof])
            OP("dve", "tensor_scalar", out=ptf[:], in0=ptf[:], scalar1=128.0, scalar2=piof[:, 0:1], op0=ALU.mult, op1=ALU.add,
               reads=[b_ptf, b_piof], writes=[b_ptf])
            OP("dve", "tensor_copy", out=ridx[:], in_=ptf[:], reads=[b_ptf], writes=[b_ridx])
            CK("ridx")
            bdt, b_bdt = SB(S4, "bdt", [64, 64], F32, dma=True)
            DMA("sp", bdt[:], bd, writes=[b_bdt])
            bdb, b_bdb = SB(S4, "bdb", [64, 64], BF16)
            OP("dve", "tensor_copy", out=bdb[:], in_=bdt[:], reads=[b_bdt], writes=[b_bdb])
            kst = [SB(S4, "kst%d" % i, [128, 512], F32, dma="sw") for i in range(3)]
            vst = [SB(S4, "vst%d" % i, [128, 512], F32, dma="sw") for i in range(3)]
            fst = [SB(S4, "fst%d" % i, [128, 16, 8], F32, dma="sw") for i in range(2)]
            kb16 = [SB(S4, "kb16_%d" % i, [128, 512], BF16) for i in range(2)]
            KTs = [SB(S4, "KTs%d" % i, [128, 4, 2048], BF16) for i in range(2)]
            Vs = [SB(S4, "Vs%d" % i, [128, 16, 8, 65], BF16) for i in range(2)]
            for i in range(2):
                OP("pool", "memset", Vs[i][0][:, :, :, 64:65], 1.0, writes=[Vs[i][1]])
            bia, b_bia = SB(S4, "bia", [128, 16, 8], F32)
            ta, b_ta = SB(S4, "ta", [128, 16, 8], F32)
            tb_, b_tb = SB(S4, "tbb", [128, 16, 8], F32)
            tt, b_tt = SB(S4, "tt", [128, 16, 8], F32)
            scs, b_scs = SB(S4, "scs", [128, 16, 8, 4], F32)
            pts_, b_pts = SB(S4, "pts", [128, 16, 8, 4], BF16)
            pTk = [PS(S4, "pTk%d" % i, [128, 4, 128], BF16) for i in range(2)]
            pSs, b_pSs = PS(S4, "pSs", [128, 512])
            pSo, b_pSo = PS(S4, "pSo", [128, 512])
            pSpar = [(pSs, b_pSs), (pSo, b_pSo)]
            pWT, b_pW = PS(S4, "pWT", [128, 256])
            pW = pWT[:, 0:128]
            pTo = pWT[:, 128:256]
            b_pTo = b_pW
            pAp, b_pAp = PS(S4, "pAp", [128, 8, 64])
            pAn, b_pAn = PS(S4, "pAn", [128, 8, 64])
            if len(A4_SEQS) < 16:
                OP("dve", "memset", pAp[:], 0.0, writes=[b_pAp])
            for s in A4_SEQS:
                KTs_, b_KTs = KTs[s % 2]
                Vs_, b_Vs = Vs[s % 2]
                f_, b_f_ = fst[s % 2]
                for pg in range(16):
                    col = s * 16 + pg
                    k_, b_k = kst[pg % 3]
                    v_, b_v = vst[pg % 3]
                    P.dma("pool", lambda e, k_=k_, col=col: e.indirect_dma_start(
                        out=k_[:], out_offset=None, in_=cache_k,
                        in_offset=bass.IndirectOffsetOnAxis(ap=ridx[:, col:col + 1], axis=0)),
                        reads=[b_ridx], writes=[b_k])
                    P.dma("pool", lambda e, v_=v_, col=col: e.indirect_dma_start(
                        out=v_[:], out_offset=None, in_=cache_v,
                        in_offset=bass.IndirectOffsetOnAxis(ap=ridx[:, col:col + 1], axis=0)),
                        reads=[b_ridx], writes=[b_v])
                    P.dma("pool", lambda e, f_=f_, col=col, pg=pg: e.indirect_dma_start(
                        out=f_[:, pg, :], out_offset=None, in_=cache_f,
                        in_offset=bass.IndirectOffsetOnAxis(ap=ridx[:, col:col + 1], axis=0)),
                        reads=[b_ridx], writes=[b_f_])
                    CK("gather1")
                    kb_, b_kb = kb16[pg % 2]
                    OP("dve", "tensor_copy", out=kb_[:], in_=k_[:], reads=[b_k], writes=[b_kb])
                    ptk, b_ptk = pTk[pg % 2]
                    for hp in range(4):
                        OP("pe", "transpose", out=ptk[:, hp, :], in_=kb_[:, hp * 128:(hp + 1) * 128], identity=ident[:],
                           reads=[b_kb, b_ident], writes=[b_ptk])
                    OP("act", "copy", out=KTs_[:, :, pg * 128:(pg + 1) * 128], in_=ptk[:], reads=[b_ptk], writes=[b_KTs])
                    OP("pool", "tensor_copy", out=Vs_[:, pg, :, 0:64], in_=v_[:].rearrange("p (h d) -> p h d", d=64),
                       reads=[b_v], writes=[b_Vs])
                CK("pages")
                f128 = f_[:].rearrange("p g h -> p (g h)")
                OP("pe", "matmul", pW[:], lhsT=Gsf[:], rhs=f128, start=True, stop=True, reads=[b_Gsf, b_f_], writes=[b_pW])
                OP("pe", "matmul", pTo[:], lhsT=onesf[:], rhs=f128, start=True, stop=True, reads=[b_onesf, b_f_], writes=[b_pTo])
                OP("dve", "tensor_copy", out=tt[:].rearrange("p g h -> p (g h)"), in_=pTo[:], reads=[b_pTo], writes=[b_tt])
                OP("dve", "tensor_copy", out=ta[:], in_=tt[:], reads=[b_tt], writes=[b_ta])
                cur, b_cur, nxt, b_nxt = ta, b_ta, tb_, b_tb
                for sft in (1, 2, 4, 8):
                    OP("dve", "tensor_tensor", out=nxt[:, 0:16 - sft, :], in0=cur[:, 0:16 - sft, :], in1=cur[:, sft:16, :], op=ALU.add,
                       reads=[b_cur], writes=[b_nxt])
                    OP("dve", "tensor_copy", out=nxt[:, 16 - sft:16, :], in_=cur[:, 16 - sft:16, :], reads=[b_cur], writes=[b_nxt])
                    cur, b_cur, nxt, b_nxt = nxt, b_nxt, cur, b_cur
                OP("dve", "tensor_tensor", out=cur[:], in0=cur[:], in1=tt[:], op=ALU.subtract, reads=[b_cur, b_tt], writes=[b_cur])
                OP("dve", "tensor_tensor", out=bia[:].rearrange("p g h -> p (g h)"), in0=pW[:],
                   in1=cur[:].rearrange("p g h -> p (g h)"), op=ALU.add, reads=[b_pW, b_cur], writes=[b_bia])
                for par in range(2):
                    pS_, b_pS_ = pSpar[par]
                    for pg in range(16):
                        for hh in range(4):
                            hp, hd0 = hh, par * 64
                            c0 = (pg * 4 + hh) * 4
                            OP("pe", "matmul", pS_[:, c0:c0 + 4],
                               lhsT=KTs_[hd0:hd0 + 64, hp, pg * 128:(pg + 1) * 128], rhs=QTn[hd0:hd0 + 64, hp, 4 * s:4 * s + 4],
                               start=True, stop=True, reads=[b_KTs, b_QTn], writes=[b_pS_], inc=(pg == 15 and hh == 3))
                for par in range(2):
                    pS_, b_pS_ = pSpar[par]
                    OP("dve", "tensor_tensor",
                       out=scs[:].rearrange("p g (hh two) q -> p g hh two q", two=2)[:, :, :, par, :],
                       in0=pS_[:, 0:256].rearrange("p (g hh q) -> p g hh q", hh=4, q=4),
                       in1=bia[:].rearrange("p g (hh two) -> p g hh two", two=2)[:, :, :, par].unsqueeze(3).broadcast_to([128, 16, 4, 4]),
                       op=ALU.add, reads=[b_pS_, b_bia], writes=[b_scs])
                OP("act", "activation", out=pts_[:].rearrange("p g h q -> p (g h q)"), in_=scs[:].rearrange("p g h q -> p (g h q)"),
                   func=AF.Exp, reads=[b_scs], writes=[b_pts])
                for h in range(H):
                    for pg in range(16):
                        OP("pe", "matmul", pAp[0:65, h, 4 * s:4 * s + 4], lhsT=Vs_[:, pg, h, :], rhs=pts_[:, pg, h, :],
                           start=(pg == 0), stop=(pg == 15), reads=[b_Vs, b_pts], writes=[b_pAp], inc=(pg == 15 and h == 7))
                CK("seq1")
            csn, b_csn = SB(S4, "csn", [64, 8], F32)
            OP("pe", "matmul", pW[0:64, 0:8], lhsT=bdt[:], rhs=lfn[:], start=True, stop=True, reads=[b_bdt, b_lfn], writes=[b_pW])
            OP("dve", "tensor_scalar_mul", out=csn[:], in0=pW[0:64, 0:8], scalar1=-1.0, reads=[b_pW], writes=[b_csn])
            CK("t1")
            for par in range(2):
                pS_, b_pS_ = pSpar[par]
                for hh in range(4):
                    OP("pe", "matmul", pS_[:, hh * 64:(hh + 1) * 64], lhsT=KTn[par * 64:par * 64 + 64, hh, :],
                       rhs=QTn[par * 64:par * 64 + 64, hh, :], start=True, stop=True,
                       reads=[b_KTn, b_QTn], writes=[b_pS_], inc=(hh == 3))
            CK("t2")
            scn, b_scn = SB(S4, "scn", [64, 8, 64], F32)
            ptn, b_ptn = SB(S4, "ptn", [64, 8, 64], BF16)
            for par in range(2):
                pS_, b_pS_ = pSpar[par]
                OP("dve", "tensor_tensor", out=scn[:].rearrange("p (hh two) q -> p hh two q", two=2)[:, :, par, :],
                   in0=pS_[0:64, 0:256].rearrange("p (hh q) -> p hh q", q=64),
                   in1=csn[:].rearrange("p (hh two) -> p hh two", two=2)[:, :, par].unsqueeze(2).broadcast_to([64, 4, 64]),
                   op=ALU.add, reads=[b_pS_, b_csn], writes=[b_scn])
            OP("act", "activation", out=ptn[:], in_=scn[:], func=AF.Exp, reads=[b_scn], writes=[b_ptn])
            OP("dve", "tensor_tensor", out=ptn[:], in0=ptn[:], in1=bdb[:].unsqueeze(1).broadcast_to([64, 8, 64]), op=ALU.mult,
               reads=[b_ptn, b_bdb], writes=[b_ptn])
            CK("t3")
            for h in range(H):
                OP("pe", "matmul", pAn[0:65, h, :], lhsT=Vn[:, h, :], rhs=ptn[:, h, :], start=True, stop=True,
                   reads=[b_Vn, b_ptn], writes=[b_pAn], inc=(h == 7))
            CK("t4")
            asum, b_asum = SB(S4, "asum", [128, 8, 64], F32)
            OP("dve", "tensor_copy", out=asum[0:65], in_=pAp[0:65], reads=[b_pAp], writes=[b_asum])
            OP("dve", "tensor_tensor", out=asum[0:65], in0=asum[0:65], in1=pAn[0:65], op=ALU.add, reads=[b_asum, b_pAn], writes=[b_asum])
            rs2, b_rs2 = SB(S4, "rs2", [128, 512], F32)
            OP("dve", "reciprocal", out=rs2[64:65, :], in_=asum[64:65].rearrange("p h q -> p (h q)"), reads=[b_asum], writes=[b_rs2])
            OP("pe", "matmul", pSs[0:64, :], lhsT=onesf[64:65, 0:64], rhs=rs2[64:65, :], start=True, stop=True,
               reads=[b_onesf, b_rs2], writes=[b_pSs])
            CK("t5")
            ysn, b_ysn = SB(S4, "ysn", [64, 8, 64], BF16, dma=True)
            OP("dve", "tensor_tensor", out=ysn[:].rearrange("p h q -> p (h q)"), in0=asum[0:64].rearrange("p h q -> p (h q)"),
               in1=pSs[0:64, :], op=ALU.mult, reads=[b_asum, b_pSs], writes=[b_ysn])
            DMA("sp", yat[:, :, 2048:2112].rearrange("h d q -> d h q"), ysn[:], reads=[b_ysn, b_yat], owner=b_ysn)
            zpad, b_zpad = SB(S4, "zpad", [64, 8, 64], BF16, dma=True)
            OP("pool", "memset", zpad[:], 0.0, writes=[b_zpad])
            DMA("sp", yat[:, :, 2112:2176].rearrange("h d q -> d h q"), zpad[:], reads=[b_zpad, b_yat], owner=b_zpad)
            P.barrier()
            P.release(b_ptb, b_bdt, b_ysn, b_zpad, *[b for _, b in kst], *[b for _, b in vst], *[b for _, b in fst])

        with contextlib.ExitStack() as SBk:
            P.enabled = "B" in PHASES
            wcv, b_wcv = SB(SBk, "wcv", [128, 8, 1536], BF16, dma="sw")
            wgc, b_wgc = SB(SBk, "wgc", [128, 8, 1024], BF16, dma="sw")
            wga, b_wga = SB(SBk, "wga", [128, 8, 1024], BF16, dma="sw")
            wbc, b_wbc = SB(SBk, "wbc", [128, 4, 1024], BF16, dma="sw")
            wba, b_wba = SB(SBk, "wba", [64, 8, 1024], BF16, dma="sw")
            wo, b_wo = SB(SBk, "wo", [128, 8, 1024], BF16, dma="sw")
            wr, b_wr = SB(SBk, "wr", [128, 8, 32], F32, dma=True)
            DMA("pool", wcv[:], wview(w_in[:, 0:1536]), writes=[b_wcv])
            DMA("pool", wgc[:], wview(w_in[:, C_GC:C_GC + 1024]), writes=[b_wgc])
            DMA("pool", wga[:], wview(w_in[:, C_GA:C_GA + 1024]), writes=[b_wga])
            DMA("pool", wbc[:], wview(w_br_conv), writes=[b_wbc])
            DMA("pool", wba[:], w_br_attn.rearrange("(h d) n -> d h n", d=64), writes=[b_wba])
            DMA("pool", wo[:], wview(w_o), writes=[b_wo])
            DMA("sp", wr[:], wview(w_router), writes=[b_wr])
            cw, b_cw = SB(SBk, "cw", [128, 3, 4], F32, dma=True)
            for k_ in range(3):
                DMA("sp", cw[:, k_, :], conv_w[k_:k_ + 1, :].rearrange("o (c p) -> p (o c)", p=128), writes=[b_cw],
                    allow_slow_non_contiguous=True)
            hm, b_hm = SB(SBk, "hm", [128, 8], F32, dma=True)
            DMA("sp", hm[:], hmask, writes=[b_hm])
            gin_bc, b_gin_bc = SB(SBk, "gin_bc", [128, D], F32, dma=True)
            bin_bc, b_bin_bc = SB(SBk, "bin_bc", [128, D], F32, dma=True)
            g1_bc, b_g1_bc = SB(SBk, "g1_bc", [128, D], F32, dma=True)
            b1_bc, b_b1_bc = SB(SBk, "b1_bc", [128, D], F32, dma=True)
            brb, b_brb = SB(SBk, "brb", [128, E], F32, dma=True)
            DMA("sp", gin_bc[:], ln_in_g.broadcast_to([128, D]), writes=[b_gin_bc])
            DMA("sp", bin_bc[:], ln_in_b.broadcast_to([128, D]), writes=[b_bin_bc])
            DMA("sp", g1_bc[:], ln1_g.broadcast_to([128, D]), writes=[b_g1_bc])
            DMA("sp", b1_bc[:], ln1_b.broadcast_to([128, D]), writes=[b_b1_bc])
            DMA("sp", brb[:], b_router.broadcast_to([128, E]), writes=[b_brb])
            eoff, b_eoff = SB(SBk, "eoff", [128, E], F32)
            OP("pool", "iota", eoff[:], pattern=[[CAP, E]], base=1, channel_multiplier=0, allow_small_or_imprecise_dtypes=True,
               writes=[b_eoff])
            macc, b_macc = SB(SBk, "macc", [128, E], F32)
            OP("pool", "memset", macc[:], 0.0, writes=[b_macc])
            ztb, b_ztb = SB(SBk, "ztb", [128, 96], I32, dma=True)
            OP("pool", "memset", ztb[:], 0, writes=[b_ztb])
            DMA("sp", tbl.rearrange("(p f) o -> p (f o)", p=128), ztb[:], reads=[b_ztb], writes=[b_tbl], owner=b_ztb)
            uh, b_uh = SB(SBk, "uh", [128, 4, 8], BF16)
            scT, b_scT = SB(SBk, "scT", [32, 512], F32, dma=True)
            DMA("sp", scT[:], sconv, writes=[b_scT])
            ush, b_ush = SB(SBk, "ush", [128, 4, 32], BF16)

            NG = 256
            xt4 = [SB(SBk, "bxt%d" % i, [128, D], F32, dma=True) for i in range(3)]
            xln = [SB(SBk, "xln%d" % i, [128, D], F32) for i in range(2)]
            xnb = [SB(SBk, "bxn%d" % i, [128, D], BF16) for i in range(2)]
            xTg, b_xTg = SB(SBk, "xTg", [128, 8, NG], BF16)
            st6, b_st6 = SB(SBk, "bst6", [128, 12], F32)
            mv, b_mv = SB(SBk, "bmv", [128, 2], F32)
            rstd, b_rstd = SB(SBk, "brstd", [128, 1], F32)
            ccs, b_ccs = SB(SBk, "ccs", [128, NG], F32)
            ext, b_ext = SB(SBk, "ext", [128, 4, NG + 2], BF16)
            exs, b_exs = SB(SBk, "exs", [128, 4, 16, 6], BF16)
            yc, b_yc = SB(SBk, "yc", [128, NG], F32)
            ycT, b_ycT = SB(SBk, "ycT", [128, 4, NG], BF16)
            yaT, b_yaT = SB(SBk, "yaT", [64, 8, NG], BF16, dma=True)
            sgc, b_sgc = SB(SBk, "sgc", [128, NG], F32)
            sga, b_sga = SB(SBk, "sga", [128, NG], F32)
            t1, b_t1 = SB(SBk, "t1", [128, NG], F32)
            mT, b_mT = SB(SBk, "mT", [128, 8, NG], BF16)
            x1t = [SB(SBk, "x1t%d" % i, [128, D], F32, dma=True) for i in range(2)]
            x1bt = [SB(SBk, "x1bt%d" % i, [128, D], BF16, dma=True) for i in range(2)]
            x1T, b_x1T = SB(SBk, "x1T", [128, 8, 128], F32)
            lg, b_lg = SB(SBk, "lg", [128, E], F32)
            m8, b_m8 = SB(SBk, "m8", [128, 8], F32)
            msk, b_msk = SB(SBk, "msk", [128, E], F32)
            ex, b_ex = SB(SBk, "ex", [128, E], F32)
            sm, b_sm = SB(SBk, "sm", [128, 4], F32)
            Gt, b_Gt = SB(SBk, "Gt", [128, E], F32)
            key, b_key = SB(SBk, "key", [128, E], F32)
            k8, b_k8 = SB(SBk, "k8", [128, 8], F32)
            oh, b_oh = SB(SBk, "oh", [128, E], F32)
            sf, b_sf = SB(SBk, "sf", [128, 4], F32)
            convo, b_convo = SB(SBk, "convo", [128, 4, 32], F32, dma=True)
            convp, b_convp = SB(SBk, "convp", [128, 4, 2], F32, dma=True)

            pTr = [PS(SBk, "bpTr%d" % i, [128, 4, 128], BF16) for i in range(2)]
            pA = [PS(SBk, "bpA%d" % i, [128, 512]) for i in range(4)]
            pX, b_pX = PS(SBk, "bpX", [128, 4, 128])
            pL, b_pL = PS(SBk, "bpL", [128, 64])
            pa_i = [0]

            def nextpA():
                r = pA[pa_i[0] % 4]
                pa_i[0] += 1
                return r

            for ci in range(4):
                OP("pe", "transpose", out=pX[:, ci, 0:32], in_=scT[:, ci * 128:(ci + 1) * 128], identity=identf[0:32, 0:32],
                   reads=[b_scT, b_identf], writes=[b_pX])
            OP("dve", "tensor_copy", out=ush[:], in_=pX[:, :, 0:32], reads=[b_pX], writes=[b_ush])

            tile_ctr = [0]

            def group(rows0, n, kind):
                nt = n // 128
                lnt = []
                for ti in range(nt):
                    i = tile_ctr[0]; tile_ctr[0] += 1
                    xt, b_xt = xt4[i % 3]
                    src = xh if kind == "halo" else xo[rows0 + ti * 128: rows0 + (ti + 1) * 128, :]
                    DMA("sp", xt[:], src, writes=[b_xt])
                    ln_stats(st6, b_st6, mv, b_mv, rstd, b_rstd, xt, b_xt)
                    xn_, b_xn = xnb[i % 2]
                    OP("dve", "tensor_scalar", out=xn_[:], in0=xt[:], scalar1=mv[:, 0:1], scalar2=rstd[:, 0:1],
                       op0=ALU.subtract, op1=ALU.mult, reads=[b_xt, b_mv, b_rstd], writes=[b_xn])
                    if kind != "halo":
                        xl, b_xl = xln[ti % 2]
                        OP("pool", "tensor_tensor", out=xl[:], in0=xn_[:], in1=gin_bc[:], op=ALU.mult, reads=[b_xn, b_gin_bc], writes=[b_xl])
                        OP("pool", "tensor_tensor", out=xl[:], in0=xl[:], in1=bin_bc[:], op=ALU.add, reads=[b_xl, b_bin_bc], writes=[b_xl])
                        lnt.append((xl, b_xl))
                    for half in range(2):
                        pt_, b_pt = pTr[half]
                        for q in range(4):
                            dc = half * 4 + q
                            OP("pe", "transpose", out=pt_[:, q, :], in_=xn_[:, dc * 128:(dc + 1) * 128], identity=ident[:],
                               reads=[b_xn, b_ident], writes=[b_pt])
                        for q in range(4):
                            dc = half * 4 + q
                            OP("act", "activation", out=xTg[:, dc, ti * 128:(ti + 1) * 128], in_=pt_[:, q, :], func=AF.Identity,
                               bias=binb[:, dc:dc + 1], scale=ging[:, dc:dc + 1], reads=[b_pt, b_ging, b_binb], writes=[b_xTg])
                return lnt

            def proj_fm(wt, b_wt, col0, ps_ap, b_ps, n, last=True):
                for dc in range(8):
                    OP("pe", "matmul", ps_ap, lhsT=wt[:, dc, col0:col0 + 128], rhs=xTg[:, dc, 0:n], start=(dc == 0), stop=(dc == 7),
                       reads=[b_xTg, b_wt], writes=[b_ps], inc=(dc == 7))

            def conv_u(n, ci, dst_ap):
                pc, b_pc = nextpA()
                proj_fm(wcv, b_wcv, 512 + ci * 128, pc[:, 0:n], b_pc, n)
                ph, b_ph = nextpA()
                proj_fm(wcv, b_wcv, 1024 + ci * 128, ph[:, 0:n], b_ph, n)
                OP("act", "copy", out=ccs[:, 0:n], in_=pc[:, 0:n], reads=[b_pc], writes=[b_ccs])
                return ph, b_ph

            group(0, 128, "halo")
            for ci in range(4):
                ph, b_ph = conv_u(128, ci, None)
                OP("dve", "tensor_tensor", out=yc[:, 0:8], in0=ph[:, 0:8], in1=ccs[:, 0:8], op=ALU.mult, reads=[b_ph, b_ccs], writes=[b_yc])
                OP("dve", "tensor_tensor", out=uh[:, ci, :], in0=yc[:, 0:8], in1=hm[:], op=ALU.mult, reads=[b_yc, b_hm], writes=[b_uh])

            def token_groups():
                for gi in range(8):
                    yield gi * 256, 256, "prompt", gi
                yield 2048, 128, "sample", 8

            for rows0, n, kind, gi in token_groups():
                lnt = group(rows0, n, kind)
                DMA("sp", yaT[:, :, 0:n], yat[:, :, rows0:rows0 + n].rearrange("h d t -> d h t"), reads=[b_yat], writes=[b_yaT])
                for ci in range(4):
                    ph, b_ph = conv_u(n, ci, None)
                    if kind == "prompt":
                        m, half = gi // 2, gi % 2
                        if half == 0:
                            OP("pool", "tensor_copy", out=ext[:, ci, 0:2], in_=uh[:, ci, 2 * m:2 * m + 2], reads=[b_uh], writes=[b_ext])
                        else:
                            OP("pool", "tensor_copy", out=ext[:, ci, 0:2], in_=ext[:, ci, n:n + 2], reads=[b_ext], writes=[b_ext])
                        OP("dve", "tensor_tensor", out=ext[:, ci, 2:n + 2], in0=ph[:, 0:n], in1=ccs[:, 0:n], op=ALU.mult,
                           reads=[b_ph, b_ccs], writes=[b_ext])
                        e0, e1, e2 = ext[:, ci, 0:n], ext[:, ci, 1:n + 1], ext[:, ci, 2:n + 2]
                        ycv = yc[:, 0:n]
                        if gi == 7:
                            OP("dve", "tensor_tensor", out=convp[:, ci, :], in0=ph[:, n - 2:n], in1=ccs[:, n - 2:n], op=ALU.mult,
                               reads=[b_ph, b_ccs], writes=[b_convp])
                    else:
                        OP("pool", "tensor_copy", out=exs[:, ci, :, 0:2], in_=ush[:, ci, :].rearrange("p (s r) -> p s r", r=2),
                           reads=[b_ush], writes=[b_exs])
                        OP("dve", "tensor_tensor", out=exs[:, ci, :, 2:6], in0=ph[:, 0:64].rearrange("p (s i) -> p s i", i=4),
                           in1=ccs[:, 0:64].rearrange("p (s i) -> p s i", i=4), op=ALU.mult, reads=[b_ph, b_ccs], writes=[b_exs])
                        e0, e1, e2 = exs[:, ci, :, 0:4], exs[:, ci, :, 1:5], exs[:, ci, :, 2:6]
                        ycv = yc[:, 0:64].rearrange("p (s i) -> p s i", i=4)
                        OP("dve", "tensor_tensor", out=convo[:, ci, :].rearrange("p (s r) -> p s r", r=2),
                           in0=ph[:, 0:64].rearrange("p (s i) -> p s i", i=4)[:, :, 2:4],
                           in1=ccs[:, 0:64].rearrange("p (s i) -> p s i", i=4)[:, :, 2:4], op=ALU.mult,
                           reads=[b_ph, b_ccs], writes=[b_convo])
                        OP("pool", "memset", yc[:, 64:128], 0.0, writes=[b_yc])
                    b_e = b_ext if kind == "prompt" else b_exs
                    OP("dve", "tensor_scalar_mul", out=ycv, in0=e0, scalar1=cw[:, 0, ci:ci + 1], reads=[b_e, b_cw], writes=[b_yc])
                    OP("dve", "scalar_tensor_tensor", out=ycv, in0=e1, scalar=cw[:, 1, ci:ci + 1], in1=ycv, op0=ALU.mult, op1=ALU.add,
                       reads=[b_e, b_cw, b_yc], writes=[b_yc])
                    OP("dve", "scalar_tensor_tensor", out=ycv, in0=e2, scalar=cw[:, 2, ci:ci + 1], in1=ycv, op0=ALU.mult, op1=ALU.add,
                       reads=[b_e, b_cw, b_yc], writes=[b_yc])
                    pb, b_pb = nextpA()
                    proj_fm(wcv, b_wcv, ci * 128, pb[:, 0:n], b_pb, n)
                    OP("dve", "tensor_tensor", out=ycT[:, ci, 0:n], in0=pb[:, 0:n], in1=yc[:, 0:n], op=ALU.mult,
                       reads=[b_pb, b_yc], writes=[b_ycT])
                for nc_ in range(8):
                    pg_, b_pg = nextpA()
                    proj_fm(wgc, b_wgc, nc_ * 128, pg_[:, 0:n], b_pg, n)
                    OP("act", "activation", out=sgc[:, 0:n], in_=pg_[:, 0:n], func=AF.Sigmoid, reads=[b_pg], writes=[b_sgc])
                    pg2, b_pg2 = nextpA()
                    proj_fm(wga, b_wga, nc_ * 128, pg2[:, 0:n], b_pg2, n)
                    OP("act", "activation", out=sga[:, 0:n], in_=pg2[:, 0:n], func=AF.Sigmoid, reads=[b_pg2], writes=[b_sga])
                    pbc, b_pbc = nextpA()
                    for ci in range(4):
                        OP("pe", "matmul", pbc[:, 0:n], lhsT=wbc[:, ci, nc_ * 128:(nc_ + 1) * 128], rhs=ycT[:, ci, 0:n],
                           start=(ci == 0), stop=(ci == 3), reads=[b_wbc, b_ycT], writes=[b_pbc], inc=(ci == 3))
                    pba, b_pba = nextpA()
                    for h in range(H):
                        OP("pe", "matmul", pba[:, 0:n], lhsT=wba[:, h, nc_ * 128:(nc_ + 1) * 128], rhs=yaT[:, h, 0:n],
                           start=(h == 0), stop=(h == 7), reads=[b_wba, b_yaT], writes=[b_pba], inc=(h == 7))
                    OP("dve", "tensor_tensor", out=t1[:, 0:n], in0=pbc[:, 0:n], in1=sgc[:, 0:n], op=ALU.mult, reads=[b_pbc, b_sgc], writes=[b_t1])
                    OP("dve", "tensor_tensor", out=sga[:, 0:n], in0=pba[:, 0:n], in1=sga[:, 0:n], op=ALU.mult, reads=[b_pba, b_sga], writes=[b_sga])
                    OP("pool", "tensor_tensor", out=mT[:, nc_, 0:n], in0=t1[:, 0:n], in1=sga[:, 0:n], op=ALU.add, reads=[b_t1, b_sga], writes=[b_mT])
                for ti in range(n // 128):
                    t = (rows0 // 128) + ti
                    xl, b_xl = lnt[ti]
                    x1_, b_x1 = x1t[t % 2]
                    for nh in range(2):
                        po_, b_po = nextpA()
                        for dc in range(8):
                            OP("pe", "matmul", po_[:], lhsT=mT[:, dc, ti * 128:(ti + 1) * 128], rhs=wo[:, dc, nh * 512:(nh + 1) * 512],
                               start=(dc == 0), stop=(dc == 7), reads=[b_mT, b_wo], writes=[b_po], inc=(dc == 7))
                        OP("dve", "scalar_tensor_tensor", out=x1_[:, nh * 512:(nh + 1) * 512], in0=xl[:, nh * 512:(nh + 1) * 512],
                           scalar=ALPHA, in1=po_[:], op0=ALU.mult, op1=ALU.add, reads=[b_xl, b_po], writes=[b_x1])
                    ln_stats(st6, b_st6, mv, b_mv, rstd, b_rstd, x1_, b_x1)
                    OP("dve", "tensor_scalar", out=x1_[:], in0=x1_[:], scalar1=mv[:, 0:1], scalar2=rstd[:, 0:1],
                       op0=ALU.subtract, op1=ALU.mult, reads=[b_x1, b_mv, b_rstd], writes=[b_x1])
                    OP("pool", "tensor_tensor", out=x1_[:], in0=x1_[:], in1=g1_bc[:], op=ALU.mult, reads=[b_x1, b_g1_bc], writes=[b_x1])
                    OP("pool", "tensor_tensor", out=x1_[:], in0=x1_[:], in1=b1_bc[:], op=ALU.add, reads=[b_x1, b_b1_bc], writes=[b_x1])
                    x1b_, b_x1b_ = x1bt[t % 2]
                    OP("pool", "tensor_copy", out=x1b_[:], in_=x1_[:], reads=[b_x1], writes=[b_x1b_])
                    DMA("sp", x1s[t * 128:(t + 1) * 128, :], x1_[:], reads=[b_x1, b_x1s], owner=b_x1)
                    DMA("sp", x1b[t * 128:(t + 1) * 128, :], x1b_[:], reads=[b_x1b_, b_x1b], owner=b_x1b_)
                    for half in range(2):
                        for q in range(4):
                            dc = half * 4 + q
                            OP("pe", "transpose", out=pX[:, q, :], in_=x1_[:, dc * 128:(dc + 1) * 128], identity=identf[:],
                               reads=[b_x1, b_identf], writes=[b_pX])
                        OP("act", "copy", out=x1T[:, half * 4:half * 4 + 4, :], in_=pX[:], reads=[b_pX], writes=[b_x1T])
                    for dc in range(8):
                        OP("pe", "matmul", pL[:, 0:32], lhsT=x1T[:, dc, :], rhs=wr[:, dc, :], start=(dc == 0), stop=(dc == 7),
                           reads=[b_x1T, b_wr], writes=[b_pL], inc=(dc == 7))
                    OP("dve", "tensor_tensor", out=lg[:], in0=pL[:, 0:32], in1=brb[:], op=ALU.add, reads=[b_pL, b_brb], writes=[b_lg])
                    OP("dve", "max", out=m8[:], in_=lg[:], reads=[b_lg], writes=[b_m8])
                    OP("dve", "tensor_scalar", out=msk[:], in0=lg[:], scalar1=m8[:, 3:4], scalar2=None, op0=ALU.is_ge,
                       reads=[b_lg, b_m8], writes=[b_msk])
                    OP("dve", "tensor_scalar_mul", out=sm[:, 0:1], in0=m8[:, 0:1], scalar1=-1.0, reads=[b_m8], writes=[b_sm])
                    OP("act", "activation", out=ex[:], in_=lg[:], func=AF.Exp, bias=sm[:, 0:1], scale=1.0, reads=[b_lg, b_sm], writes=[b_ex])
                    OP("dve", "tensor_tensor", out=ex[:], in0=ex[:], in1=msk[:], op=ALU.mult, reads=[b_ex, b_msk], writes=[b_ex])
                    OP("dve", "tensor_reduce", out=sm[:, 1:2], in_=ex[:], axis=AX.X, op=ALU.add, reads=[b_ex], writes=[b_sm])
                    OP("dve", "reciprocal", out=sm[:, 2:3], in_=sm[:, 1:2], reads=[b_sm], writes=[b_sm])
                    OP("dve", "tensor_scalar_mul", out=Gt[:], in0=ex[:], scalar1=sm[:, 2:3], reads=[b_ex, b_sm], writes=[b_Gt])
                    OP("pe", "matmul", pL[:, 32:64], lhsT=Lsf[:], rhs=msk[:], start=True, stop=False, reads=[b_Lsf, b_msk], writes=[b_pL], inc=False)
                    OP("pe", "matmul", pL[:, 32:64], lhsT=onesf[:], rhs=macc[:], start=False, stop=True, reads=[b_onesf, b_macc], writes=[b_pL])
                    OP("dve", "tensor_tensor", out=key[:], in0=pL[:, 32:64], in1=eoff[:], op=ALU.add, reads=[b_pL, b_eoff], writes=[b_key])
                    OP("dve", "tensor_tensor", out=key[:], in0=key[:], in1=msk[:], op=ALU.mult, reads=[b_key, b_msk], writes=[b_key])
                    OP("dve", "tensor_tensor", out=macc[:], in0=macc[:], in1=msk[:], op=ALU.add, reads=[b_macc, b_msk], writes=[b_macc])
                    OP("dve", "max", out=k8[:], in_=key[:], reads=[b_key], writes=[b_k8])
                    OP("dve", "tensor_scalar_add", out=sf[:], in0=k8[:, 0:4], scalar1=-1.0, reads=[b_k8], writes=[b_sf])
                    OP("dve", "tensor_copy", out=slot_i[:, t, :], in_=sf[:], reads=[b_sf], writes=[b_slot])
                    for k in range(4):
                        OP("dve", "tensor_scalar", out=oh[:], in0=key[:], scalar1=k8[:, k:k + 1], scalar2=None, op0=ALU.is_equal,
                           reads=[b_key, b_k8], writes=[b_oh])
                        OP("dve", "tensor_tensor", out=oh[:], in0=oh[:], in1=Gt[:], op=ALU.mult, reads=[b_oh, b_Gt], writes=[b_oh])
                        OP("dve", "tensor_reduce", out=gk[:, t, k:k + 1], in_=oh[:], axis=AX.X, op=ALU.add, reads=[b_oh], writes=[b_gk])
                        P.dma("pool", lambda e, t=t, k=k: e.indirect_dma_start(
                            out=tbl, out_offset=bass.IndirectOffsetOnAxis(ap=slot_i[:, t, k:k + 1], axis=0),
                            in_=tokid[:, t:t + 1], in_offset=None),
                            reads=[b_slot, b_tokid, b_tbl], owner=b_tbl)
            DMA("sp", o_convs.rearrange("(c p) s r -> p c (s r)", p=128), convo[:], reads=[b_convo])
            DMA("sp", o_convp.rearrange("(c p) r -> p c r", p=128), convp[:], reads=[b_convp])
            P.barrier()
            P.release(b_wcv, b_wgc, b_wga, b_wbc, b_wba, b_wo, b_wr, b_cw, b_hm, b_gin_bc, b_bin_bc, b_g1_bc, b_b1_bc, b_brb,
                      b_ztb, b_scT, b_yaT, b_convo, b_convp, *[b for _, b in xt4], *[b for _, b in x1t], *[b for _, b in x1bt])

        with contextlib.ExitStack() as SC:
            P.enabled = "C" in PHASES
            wgs = [SB(SC, "wg%d" % i, [128, 8, D], BF16) for i in range(2)]
            wus = [SB(SC, "wu%d" % i, [128, 8, D], BF16) for i in range(2)]
            wds = [SB(SC, "wd%d" % i, [128, 8, D], BF16) for i in range(2)]
            wcb = [[[P.buf("wc%d_%d_%d" % (mi, i, dc)) for dc in range(8)] for i in range(2)] for mi in range(3)]
            NSTG = 6
            stg = [SB(SC, "stg%d" % i, [128, D], F32, dma=True) for i in range(NSTG)]
            stg_i = [0]

            def load_chunk(e, c):
                mi, dc = divmod(c, 8)
                i = e % 2
                wt = (wgs, wus, wds)[mi][i][0]
                src = (w_gate, w_up, w_down)[mi][min(e, ne_ - 1)]
                k = stg_i[0]; stg_i[0] += 1
                st_, b_st = stg[k % NSTG]
                DMA("sp", st_[:], src[dc * 128:(dc + 1) * 128, :], writes=[b_st])
                if k % 2 == 0:
                    OP("act", "copy", out=wt[:, dc, :], in_=st_[:], reads=[b_st], writes=[wcb[mi][i][dc]])
                else:
                    OP("dve", "tensor_copy", out=wt[:, dc, :], in_=st_[:], reads=[b_st], writes=[wcb[mi][i][dc]])
            bgu = [SB(SC, "bgu%d" % i, [128, 2, 8], F32, dma=True) for i in range(2)]
            bdn = [SB(SC, "bdn%d" % i, [128, D], F32, dma=True) for i in range(2)]
            idx = [SB(SC, "idx%d" % i, [128, 3], I32, dma=True) for i in range(2)]
            xg = [SB(SC, "xg%d" % i, [128, 3, D], BF16, dma="sw") for i in range(2)]
            xgT, b_xgT = SB(SC, "xgT", [128, 8, CAP], BF16)
            hT, b_hT = SB(SC, "hT", [128, 8, CAP], BF16)
            gs_, b_gs = SB(SC, "gs", [128, CAP], F32)
            us_, b_us = SB(SC, "us", [128, CAP], F32)
            sg_, b_sg = SB(SC, "sg", [128, CAP], F32)
            yo = [SB(SC, "yo%d" % i, [128, D], BF16, dma=True) for i in range(2)]
            pTr = [PS(SC, "cpTr%d" % i, [128, 4, 128], BF16) for i in range(2)]
            pG = [PS(SC, "cpG%d" % i, [128, 512]) for i in range(2)]
            pU = [PS(SC, "cpU%d" % i, [128, 512]) for i in range(2)]
            pY = [PS(SC, "cpY%d" % i, [128, 512]) for i in range(2)]

            def load_expert(e):
                i = e % 2
                DMA("sp", bgu[i][0][:, 0, :], b_gate[e:e + 1, :].rearrange("o (c p) -> p (o c)", p=128), writes=[bgu[i][1]],
                    allow_slow_non_contiguous=True)
                DMA("sp", bgu[i][0][:, 1, :], b_up[e:e + 1, :].rearrange("o (c p) -> p (o c)", p=128), writes=[bgu[i][1]],
                    allow_slow_non_contiguous=True)
                DMA("sp", bdn[i][0][:], b_down[e:e + 1, :].broadcast_to([128, D]), writes=[bdn[i][1]])
                DMA("sp", idx[i][0][:], tbl[e * CAP:(e + 1) * CAP, :].rearrange("(j p) o -> p (j o)", p=128), reads=[b_tbl],
                    writes=[idx[i][1]], allow_slow_non_contiguous=True)
                for j in range(3):
                    P.dma("pool", lambda en, i=i, j=j: en.indirect_dma_start(
                        out=xg[i][0][:, j, :], out_offset=None, in_=x1b,
                        in_offset=bass.IndirectOffsetOnAxis(ap=idx[i][0][:, j:j + 1], axis=0)),
                        reads=[idx[i][1], b_x1b], writes=[xg[i][1]])

            load_expert(0)
            for c_ in range(24):
                load_chunk(0, c_)
            yo_i = 0
            for e in range(E):
                i = e % 2
                if e + 1 < E:
                    load_expert(e + 1)
                wg_ = wgs[i][0]; wu_ = wus[i][0]; wd_ = wds[i][0]
                xg_, b_xg = xg[i]
                for j in range(3):
                    for half in range(2):
                        pt_, b_pt = pTr[half]
                        for q in range(4):
                            dc = half * 4 + q
                            OP("pe", "transpose", out=pt_[:, q, :], in_=xg_[:, j, dc * 128:(dc + 1) * 128], identity=ident[:],
                               reads=[b_xg, b_ident], writes=[b_pt])
                        OP("act", "copy", out=xgT[:, half * 4:half * 4 + 4, j * 128:(j + 1) * 128], in_=pt_[:], reads=[b_pt], writes=[b_xgT])
                for fo in range(8):
                    pg_, b_pg = pG[fo % 2]
                    pu_, b_pu = pU[fo % 2]
                    for dc in range(8):
                        OP("pe", "matmul", pg_[:, 0:CAP], lhsT=wg_[:, dc, fo * 128:(fo + 1) * 128], rhs=xgT[:, dc, :],
                           start=(dc == 0), stop=(dc == 7), reads=[wcb[0][i][dc], b_xgT], writes=[b_pg], inc=(dc == 7))
                    for dc in range(8):
                        OP("pe", "matmul", pu_[:, 0:CAP], lhsT=wu_[:, dc, fo * 128:(fo + 1) * 128], rhs=xgT[:, dc, :],
                           start=(dc == 0), stop=(dc == 7), reads=[wcb[1][i][dc], b_xgT], writes=[b_pu], inc=(dc == 7))
                    OP("dve", "tensor_scalar", out=gs_[:], in0=pg_[:, 0:CAP], scalar1=bgu[i][0][:, 0, fo:fo + 1], scalar2=7.0,
                       op0=ALU.add, op1=ALU.min, reads=[b_pg, bgu[i][1]], writes=[b_gs])
                    OP("act", "activation", out=sg_[:], in_=gs_[:], func=AF.Sigmoid, scale=1.702, reads=[b_gs], writes=[b_sg])
                    OP("dve", "tensor_scalar", out=us_[:], in0=pu_[:, 0:CAP], scalar1=bgu[i][0][:, 1, fo:fo + 1], scalar2=7.0,
                       op0=ALU.add, op1=ALU.min, reads=[b_pu, bgu[i][1]], writes=[b_us])
                    OP("pool", "tensor_scalar", out=us_[:], in0=us_[:], scalar1=-7.0, scalar2=1.0, op0=ALU.max, op1=ALU.add,
                       reads=[b_us], writes=[b_us])
                    OP("pool", "tensor_tensor", out=gs_[:], in0=gs_[:], in1=sg_[:], op=ALU.mult, reads=[b_gs, b_sg], writes=[b_gs])
                    OP("pool", "tensor_tensor", out=hT[:, fo, :], in0=gs_[:], in1=us_[:], op=ALU.mult, reads=[b_gs, b_us], writes=[b_hT])
                    if e + 1 < E:
                        for r_ in range(3):
                            load_chunk(e + 1, fo * 3 + r_)
                for j in range(3):
                    yo_, b_yo = yo[yo_i % 2]; yo_i += 1
                    for nh in range(2):
                        py_, b_py = pY[nh]
                        for fo in range(8):
                            OP("pe", "matmul", py_[:], lhsT=hT[:, fo, j * 128:(j + 1) * 128], rhs=wd_[:, fo, nh * 512:(nh + 1) * 512],
                               start=(fo == 0), stop=(fo == 7), reads=[b_hT, wcb[2][i][fo]], writes=[b_py], inc=(fo == 7))
                        OP("dve", "tensor_tensor", out=yo_[:, nh * 512:(nh + 1) * 512], in0=py_[:], in1=bdn[i][0][:, nh * 512:(nh + 1) * 512],
                           op=ALU.add, reads=[b_py, bdn[i][1]], writes=[b_yo])
                    r0 = e * CAP + j * 128
                    DMA("sp", ybuf[r0:r0 + 128, :], yo_[:], reads=[b_yo, b_ybuf], owner=b_yo)
            P.barrier()
            P.release(*[b for _, b in stg], *[b for _, b in bgu], *[b for _, b in bdn],
                      *[b for _, b in idx], *[b for _, b in xg], *[b for _, b in yo])

        with contextlib.ExitStack() as SD:
            P.enabled = "D" in PHASES
            wpg, b_wpg = SB(SD, "wpg", [128, 8, D], BF16, dma="sw")
            wpp, b_wpp = SB(SD, "wpp", [128, 2, D], BF16, dma="sw")
            DMA("pool", wpg[:], wview(w_ple_gate), writes=[b_wpg])
            DMA("pool", wpp[:], wview(w_ple_proj), writes=[b_wpp])
            g2_bc, b_g2_bc = SB(SD, "g2_bc", [128, D], F32, dma=True)
            b2_bc, b_b2_bc = SB(SD, "b2_bc", [128, D], F32, dma=True)
            DMA("sp", g2_bc[:], ln2_g.broadcast_to([128, D]), writes=[b_g2_bc])
            DMA("sp", b2_bc[:], ln2_b.broadcast_to([128, D]), writes=[b_b2_bc])
            yk = [SB(SD, "yk%d" % i, [128, 4, D], BF16, dma="sw") for i in range(2)]
            x1r = [SB(SD, "x1r%d" % i, [128, D], F32, dma=True) for i in range(2)]
            pr = [SB(SD, "pr%d" % i, [128, 256], F32, dma=True) for i in range(2)]
            prb, b_prb = SB(SD, "prb", [128, 256], BF16)
            pT_, b_pT = SB(SD, "pTd", [128, 2, 128], BF16)
            acc_, b_acc = SB(SD, "accd", [128, D], F32)
            x2b, b_x2b = SB(SD, "x2b", [128, D], BF16)
            x2T, b_x2T = SB(SD, "x2T", [128, 8, 128], BF16)
            sgp, b_sgp = SB(SD, "sgp", [128, 512], F32)
            outs = [SB(SD, "outd%d" % i, [128, D], F32, dma=True) for i in range(2)]
            st6, b_st6 = SB(SD, "dst6", [128, 12], F32)
            mv, b_mv = SB(SD, "dmv", [128, 2], F32)
            rstd, b_rstd = SB(SD, "drstd", [128, 1], F32)
            pTr = [PS(SD, "dpTr%d" % i, [128, 4, 128], BF16) for i in range(2)]
            pGa = [PS(SD, "dpG%d" % i, [128, 512]) for i in range(2)]
            pPr = [PS(SD, "dpP%d" % i, [128, 512]) for i in range(2)]
            for t in range(NT):
                yk_, b_yk = yk[t % 2]
                for k in range(4):
                    P.dma("pool", lambda en, yk_=yk_, t=t, k=k: en.indirect_dma_start(
                        out=yk_[:, k, :], out_offset=None, in_=ybuf,
                        in_offset=bass.IndirectOffsetOnAxis(ap=slot_i[:, t, k:k + 1], axis=0)),
                        reads=[b_slot, b_ybuf], writes=[b_yk])
                x1_, b_x1 = x1r[t % 2]
                DMA("sp", x1_[:], x1s[t * 128:(t + 1) * 128, :], reads=[b_x1s], writes=[b_x1])
                p_, b_p = pr[t % 2]
                DMA("sp", p_[:], po[t * 128:(t + 1) * 128, :], writes=[b_p])
                OP("dve", "tensor_scalar_mul", out=acc_[:], in0=x1_[:], scalar1=ALPHA, reads=[b_x1], writes=[b_acc])
                for k in range(4):
                    OP("dve", "scalar_tensor_tensor", out=acc_[:], in0=yk_[:, k, :], scalar=gk[:, t, k:k + 1], in1=acc_[:],
                       op0=ALU.mult, op1=ALU.add, reads=[b_yk, b_gk, b_acc], writes=[b_acc])
                ln_stats(st6, b_st6, mv, b_mv, rstd, b_rstd, acc_, b_acc)
                OP("dve", "tensor_scalar", out=acc_[:], in0=acc_[:], scalar1=mv[:, 0:1], scalar2=rstd[:, 0:1],
                   op0=ALU.subtract, op1=ALU.mult, reads=[b_acc, b_mv, b_rstd], writes=[b_acc])
                OP("pool", "tensor_tensor", out=acc_[:], in0=acc_[:], in1=g2_bc[:], op=ALU.mult, reads=[b_acc, b_g2_bc], writes=[b_acc])
                OP("pool", "tensor_tensor", out=acc_[:], in0=acc_[:], in1=b2_bc[:], op=ALU.add, reads=[b_acc, b_b2_bc], writes=[b_acc])
                OP("pool", "tensor_copy", out=x2b[:], in_=acc_[:], reads=[b_acc], writes=[b_x2b])
                OP("pool", "tensor_copy", out=prb[:], in_=p_[:], reads=[b_p], writes=[b_prb])
                for half in range(2):
                    pt_, b_pt = pTr[half]
                    for q in range(4):
                        dc = half * 4 + q
                        OP("pe", "transpose", out=pt_[:, q, :], in_=x2b[:, dc * 128:(dc + 1) * 128], identity=ident[:],
                           reads=[b_x2b, b_ident], writes=[b_pt])
                    OP("act", "copy", out=x2T[:, half * 4:half * 4 + 4, :], in_=pt_[:], reads=[b_pt], writes=[b_x2T])
                pt_, b_pt = pTr[0]
                for q in range(2):
                    OP("pe", "transpose", out=pt_[:, q, :], in_=prb[:, q * 128:(q + 1) * 128], identity=ident[:],
                       reads=[b_prb, b_ident], writes=[b_pt])
                OP("act", "copy", out=pT_[:], in_=pt_[:, 0:2, :], reads=[b_pt], writes=[b_pT])
                o_, b_o = outs[t % 2]
                for nh in range(2):
                    pg_, b_pg = pGa[nh]
                    for dc in range(8):
                        OP("pe", "matmul", pg_[:], lhsT=x2T[:, dc, :], rhs=wpg[:, dc, nh * 512:(nh + 1) * 512], start=(dc == 0), stop=(dc == 7),
                           reads=[b_x2T, b_wpg], writes=[b_pg], inc=(dc == 7))
                    pp_, b_pp = pPr[nh]
                    for dc in range(2):
                        OP("pe", "matmul", pp_[:], lhsT=pT_[:, dc, :], rhs=wpp[:, dc, nh * 512:(nh + 1) * 512], start=(dc == 0), stop=(dc == 1),
                           reads=[b_pT, b_wpp], writes=[b_pp], inc=(dc == 1))
                    OP("act", "activation", out=sgp[:], in_=pg_[:], func=AF.Sigmoid, reads=[b_pg], writes=[b_sgp])
                    OP("dve", "tensor_tensor", out=sgp[:], in0=pp_[:], in1=sgp[:], op=ALU.mult, reads=[b_pp, b_sgp], writes=[b_sgp])
                    OP("dve", "tensor_tensor", out=o_[:, nh * 512:(nh + 1) * 512], in0=sgp[:], in1=acc_[:, nh * 512:(nh + 1) * 512], op=ALU.add,
                       reads=[b_sgp, b_acc], writes=[b_o])
                DMA("sp", o_y[t * 128:(t + 1) * 128, :], o_[:], reads=[b_o])
            P.barrier()

        P.enabled = True
        P.barrier()
        with nc.Block() as block:
            P.emit(block)
    P.close()
    return nc


_NC_CACHE = {}


def _prep_core(c, I):
    b, j = c // 4, c % 4
    f32 = np.float32
    xp = I["x_prompt"]; xs = I["x_sample"]
    Gs = [j + 4 * m for m in range(4)]
    xo = np.zeros((TOWN, D), f32)
    po = np.zeros((TOWN, 256), f32)
    xh = np.zeros((128, D), f32)
    hmask = np.zeros((128, 8), f32)
    sel = np.zeros((128, 4, 64), f32)
    kvis = np.zeros((128, 4, 64), f32)
    for m, G in enumerate(Gs):
        xo[m * 512:(m + 1) * 512] = xp[b, G * 512:(G + 1) * 512]
        po[m * 512:(m + 1) * 512] = I["p_prompt"][0, b, G * 512:(G + 1) * 512]
        if G > 0:
            xh[2 * m:2 * m + 2] = xp[b, G * 512 - 2:G * 512]
            hmask[:, 2 * m:2 * m + 2] = 1.0
        sel[127, m, 4 * G + 3] = 1.0
        kvis[:, m, 4 * G + 4:] = -30000.0
    xo[2048:2112] = xs[16 * c:16 * c + 16].reshape(64, D)
    po[2048:2112] = I["p_sample"][0, 16 * c:16 * c + 16].reshape(64, 256)
    dm = np.zeros((128, 16, 512), f32)
    kp = np.arange(128)[:, None]
    qi = np.arange(128)[None, :]
    tri = (kp <= qi).astype(f32)
    for r in range(16):
        rel = r - 4 * j
        if rel < 0:
            dm[:, r, :] = 1.0
        elif rel < 4:
            for qs in range(4):
                if qs > rel:
                    dm[:, r, qs * 128:(qs + 1) * 128] = 1.0
                elif qs == rel:
                    dm[:, r, qs * 128:(qs + 1) * 128] = tri
    bdm = np.zeros((64, 64), f32)
    for s in range(16):
        for i2 in range(4):
            for i1 in range(i2 + 1):
                bdm[4 * s + i1, 4 * s + i2] = 1.0
    return dict(
        xb=np.ascontiguousarray(xp[b]), xo=xo, po=po, xh=xh, hmask=hmask,
        dmask=dm.astype(ml_dtypes.bfloat16), sel=sel, kvis=kvis, bd=bdm,
        pt=np.ascontiguousarray(I["page_table"][16 * c:16 * c + 16]).reshape(1, 256).astype(np.int32),
        sconv=np.ascontiguousarray(I["state_conv"][0, 16 * c:16 * c + 16]).reshape(32, 512),
    )


def kernel(**I):
    I = {k: np.asarray(v) for k, v in I.items()}
    if "nc" not in _NC_CACHE:
        _NC_CACHE["nc"] = build()
    nc = _NC_CACHE["nc"]
    shared = dict(
        cache_k=I["cache_k"].reshape(NPOOLROWS, 512), cache_v=I["cache_v"].reshape(NPOOLROWS, 512),
        cache_f=I["cache_logf"].reshape(NPOOLROWS, 8),
        ln_in_g=I["ln_in_g"].reshape(1, D), ln_in_b=I["ln_in_b"].reshape(1, D),
        w_in=I["w_in"][0], b_f=I["b_f"].reshape(1, 8), conv_w=I["conv_w"][0],
        w_br_conv=I["w_br_conv"][0], w_br_attn=I["w_br_attn"][0], w_o=I["w_o"][0],
        ln1_g=I["ln1_g"].reshape(1, D), ln1_b=I["ln1_b"].reshape(1, D),
        w_router=I["w_router"][0], b_router=I["b_router"].reshape(1, E),
        w_gate=I["w_gate"][0], b_gate=I["b_gate"][0], w_up=I["w_up"][0], b_up=I["b_up"][0],
        w_down=I["w_down"][0], b_down=I["b_down"][0],
        ln2_g=I["ln2_g"].reshape(1, D), ln2_b=I["ln2_b"].reshape(1, D),
        w_ple_gate=I["w_ple_gate"][0], w_ple_proj=I["w_ple_proj"][0],
    )
    shared = {k: np.ascontiguousarray(v) for k, v in shared.items()}
    in_maps = []
    for c in range(8):
        d = dict(shared)
        d.update(_prep_core(c, I))
        in_maps.append(d)
    res = run_bass_kernel_spmd(nc, in_maps, core_ids=list(range(8)))
    R = res.results
    f32 = np.float32
    y_prompt = np.zeros((2, S, D), f32); y_sample = np.zeros((128, 4, D), f32)
    k_prompt = np.zeros((1, 2, S, H, HD), f32); v_prompt = np.zeros((1, 2, S, H, HD), f32)
    logf_prompt = np.zeros((1, 2, S, H), f32); conv_prompt = np.zeros((1, 2, 2, 512), f32)
    k_sample = np.zeros((1, 128, 4, H, HD), f32); v_sample = np.zeros((1, 128, 4, H, HD), f32)
    logf_sample = np.zeros((1, 128, 4, H), f32); conv_sample = np.zeros((1, 128, 2, 512), f32)
    for c in range(8):
        b, j = c // 4, c % 4
        r = R[c]
        oy = np.asarray(r["o_y"])
        for m in range(4):
            G = j + 4 * m
            y_prompt[b, G * 512:(G + 1) * 512] = oy[m * 512:(m + 1) * 512]
        y_sample[16 * c:16 * c + 16] = oy[2048:2112].reshape(16, 4, D)
        kT = np.asarray(r["o_kT"]); ov = np.asarray(r["o_v"]); olf = np.asarray(r["o_logf"])
        if j == 0:
            k_prompt[0, b] = kT[:, :S].T.reshape(S, H, HD)
            v_prompt[0, b] = ov[:S].reshape(S, H, HD)
            logf_prompt[0, b] = olf[:S]
        if j == 3:
            conv_prompt[0, b] = np.asarray(r["o_convp"]).T
        k_sample[0, 16 * c:16 * c + 16] = kT[:, S:S + 64].T.reshape(16, 4, H, HD)
        v_sample[0, 16 * c:16 * c + 16] = ov[S:S + 64].reshape(16, 4, H, HD)
        logf_sample[0, 16 * c:16 * c + 16] = olf[S:S + 64].reshape(16, 4, H)
        conv_sample[0, 16 * c:16 * c + 16] = np.transpose(np.asarray(r["o_convs"]), (1, 2, 0))
    return (y_prompt, y_sample, k_prompt, v_prompt, logf_prompt, conv_prompt,
            k_sample, v_sample, logf_sample, conv_sample)
```

```python
import contextlib
import numpy as np
import ml_dtypes
import concourse.bass as bass
import concourse.mybir as mybir
from concourse.bass_utils import run_bass_kernel_spmd

F32 = mybir.dt.float32
BF16 = mybir.dt.bfloat16
I32 = mybir.dt.int32
ALU = mybir.AluOpType
AF = mybir.ActivationFunctionType
AX = mybir.AxisListType

ENGS = ["pe", "act", "dve", "pool", "sp"]

D = 1024
S = 8192
NB = 64
H = 8
HD = 64
E = 32
CAP = 384
NT = 17
TOWN = NT * 128
NPOOLROWS = 2560 * 128
ALPHA = 2.0 ** 0.25
EPS = 1e-5


class Buf:
    __slots__ = ("name", "lw", "rd", "dsem", "excl")

    def __init__(self, name, dsem=None):
        self.name = name
        self.lw = {}
        self.rd = {}
        self.dsem = dsem
        self.excl = False


class Prog:
    def __init__(self, nc, n_dsem=96):
        self.nc = nc
        self.ops = {e: [] for e in ENGS}
        self.sem = []
        self.semctx = []
        self.esem = {}
        for e in ENGS:
            self.esem[e] = self._newsem("e_" + e)
        self.free_dsems = [self._newsem("d%d" % i) for i in range(n_dsem - 24)]
        self.free_swsems = [self._newsem("w%d" % i) for i in range(24)]
        self.swset = set(self.free_swsems)
        self.val = [0] * len(self.sem)
        self.waited = {e: {} for e in ENGS}
        self.enabled = True
        self.dead = False

    def _newsem(self, name):
        ctx = self.nc.semaphore(name)
        h = ctx.__enter__()
        self.semctx.append(ctx)
        self.sem.append(h)
        return len(self.sem) - 1

    def close(self):
        for c in reversed(self.semctx):
            c.__exit__(None, None, None)

    def buf(self, name, dma=False):
        d = None
        if dma == "sw":
            d = self.free_swsems.pop()
        elif dma:
            d = self.free_dsems.pop()
        return Buf(name, d)

    def release(self, *bufs):
        for b in bufs:
            if b.dsem is not None:
                (self.free_swsems if b.dsem in self.swset else self.free_dsems).append(b.dsem)
                b.dsem = None

    def _waits(self, eng, reads, writes):
        w = {}
        for b in reads:
            for s, v in b.lw.items():
                if w.get(s, 0) < v:
                    w[s] = v
            if b.excl:
                for s, v in b.rd.items():
                    if w.get(s, 0) < v:
                        w[s] = v
        for b in writes:
            for s, v in b.lw.items():
                if w.get(s, 0) < v:
                    w[s] = v
            for s, v in b.rd.items():
                if w.get(s, 0) < v:
                    w[s] = v
        out = []
        wd = self.waited[eng]
        for s, v in w.items():
            if eng == "pe" and s == self.esem["pe"]:
                continue
            if wd.get(s, 0) >= v:
                continue
            wd[s] = v
            out.append((s, v))
        return out

    def _commit(self, tok, reads, writes):
        s, v = tok
        for b in writes:
            b.lw = {s: v}
            b.rd = {}
        for b in reads:
            if b.rd.get(s, 0) < v:
                b.rd[s] = v

    def op(self, eng, fn, reads=(), writes=(), inc=True):
        if not self.enabled or self.dead:
            return None
        waits = self._waits(eng, reads, writes)
        s = self.esem[eng]
        if inc:
            self.val[s] += 1
            tok = (s, self.val[s])
        else:
            tok = (s, self.val[s] + 1)
        self.ops[eng].append((fn, waits, (s, 1) if inc else None))
        self._commit(tok, reads, writes)
        return tok

    def dma(self, eng, fn, reads=(), writes=(), owner=None):
        if not self.enabled or self.dead:
            return None
        if owner is None:
            for b in list(writes) + list(reads):
                if b.dsem is not None:
                    owner = b
                    break
        assert owner is not None and owner.dsem is not None, "dma needs owner"
        waits = self._waits(eng, reads, writes)
        s = owner.dsem
        self.val[s] += 16
        tok = (s, self.val[s])
        self.ops[eng].append((fn, waits, (s, 16)))
        self._commit(tok, reads, writes)
        return tok

    def barrier(self):
        for e in ENGS:
            waits = []
            wd = self.waited[e]
            for s in range(len(self.sem)):
                v = self.val[s]
                if v > 0 and wd.get(s, 0) < v:
                    if e == "pe" and s == self.esem["pe"]:
                        continue
                    wd[s] = v
                    waits.append((s, v))
            if waits:
                self.ops[e].append((None, waits, None))

    def emit(self, block):
        sem = self.sem

        def run(engname):
            def body(engine):
                for fn, waits, inc in self.ops[engname]:
                    for s, v in waits:
                        engine.wait_ge(sem[s], v)
                    if fn is not None:
                        ins = fn(engine)
                        if inc is not None:
                            ins.then_inc(sem[inc[0]], inc[1])
            return body

        block.tensor(run("pe"))
        block.scalar(run("act"))
        block.vector(run("dve"))
        block.gpsimd(run("pool"))
        block.sync(run("sp"))


PHASES = ["A", "A3", "A4", "B", "C", "D"]
A1_BLOCKS = list(range(65))
A2_TILES = list(range(NT))
STOPAT = None
A4_SEQS = list(range(16))


def build():
    nc = bass.Bass("TRN2", target_bir_lowering=False)

    def din(name, shape, dt=F32):
        return nc.dram_tensor(name, shape, dt, kind="ExternalInput").ap()

    def dout(name, shape, dt=F32):
        return nc.dram_tensor(name, shape, dt, kind="ExternalOutput").ap()

    def dscr(name, shape, dt):
        return nc.dram_tensor(name, shape, dt, kind="Internal").ap()

    xb = din("xb", [S, D])
    xo = din("xo", [TOWN, D])
    po = din("po", [TOWN, 256])
    xh = din("xh", [128, D])
    hmask = din("hmask", [128, 8])
    dmask = din("dmask", [128, 16, 512], BF16)
    sel = din("sel", [128, 4, 64])
    kvis = din("kvis", [128, 4, 64])
    bd = din("bd", [64, 64])
    pt = din("pt", [1, 256], I32)
    sconv = din("sconv", [32, 512])
    npool_ = NPOOLROWS if "A4" in PHASES else 128
    ne_ = E if "C" in PHASES else 1
    cache_k = din("cache_k", [npool_, 512])
    cache_v = din("cache_v", [npool_, 512])
    cache_f = din("cache_f", [npool_, 8])
    ln_in_g = din("ln_in_g", [1, D]); ln_in_b = din("ln_in_b", [1, D])
    w_in = din("w_in", [D, 5128])
    b_f = din("b_f", [1, 8])
    conv_w = din("conv_w", [3, 512])
    w_br_conv = din("w_br_conv", [512, D])
    w_br_attn = din("w_br_attn", [512, D])
    w_o = din("w_o", [D, D])
    ln1_g = din("ln1_g", [1, D]); ln1_b = din("ln1_b", [1, D])
    w_router = din("w_router", [D, E]); b_router = din("b_router", [1, E])
    w_gate = din("w_gate", [ne_, D, D]); b_gate = din("b_gate", [E, D])
    w_up = din("w_up", [ne_, D, D]); b_up = din("b_up", [E, D])
    w_down = din("w_down", [ne_, D, D]); b_down = din("b_down", [E, D])
    ln2_g = din("ln2_g", [1, D]); ln2_b = din("ln2_b", [1, D])
    w_ple_gate = din("w_ple_gate", [D, D])
    w_ple_proj = din("w_ple_proj", [256, D])

    o_y = dout("o_y", [TOWN, D])
    o_kT = dout("o_kT", [512, 65 * 128])
    o_v = dout("o_v", [65 * 128, 512])
    o_logf = dout("o_logf", [65 * 128, 8])
    o_convs = dout("o_convs", [512, 16, 2])
    o_convp = dout("o_convp", [512, 2])

    yat = dscr("yat", [H, 64, TOWN], BF16)
    x1s = dscr("x1s", [TOWN, D], F32)
    x1b = dscr("x1b", [TOWN, D], BF16)
    tbl = dscr("tbl", [E * CAP, 1], I32)
    ybuf = dscr("ybuf", [E * CAP, D], BF16)

    C_CB, C_CC, C_CH, C_Q, C_K, C_V, C_F, C_GC, C_GA = 0, 512, 1024, 1536, 2048, 2560, 3072, 3080, 4104

    P = Prog(nc)
    print("sbuf bytes/partition at start:", nc.sbuf_bytes_remaining, flush=True)

    def OP(eng, method, *args, reads=(), writes=(), inc=True, **kw):
        return P.op(eng, lambda e: getattr(e, method)(*args, **kw), reads, writes, inc)

    def DMA(eng, out, in_, reads=(), writes=(), owner=None, **kw):
        return P.dma(eng, lambda e: e.dma_start(out=out, in_=in_, **kw), reads, writes, owner)

    def CK(name):
        if STOPAT == name:
            P.dead = True

    def wview(ap2d):
        return ap2d.rearrange("(c p) n -> p c n", p=128)

    b_yat = P.buf("yat", dma=True)
    b_x1s = P.buf("x1s", dma=True)
    b_x1b = P.buf("x1b", dma=True)
    b_tbl = P.buf("tbl", dma="sw")
    b_ybuf = P.buf("ybuf", dma=True)
    b_out = P.buf("outs", dma=True)

    with contextlib.ExitStack() as S0:
        def SB(st, name, shape, dt, dma=False):
            t = st.enter_context(nc.sbuf_tensor(name, shape, dt))
            return t, P.buf(name, dma=dma)

        def PS(st, name, shape, dt=F32):
            full = 512 if dt == F32 else 1024
            t = st.enter_context(nc.psum_tensor(name, [128, full], dt))
            n = int(np.prod(shape[1:]))
            v = t[0:shape[0], 0:n]
            if len(shape) == 3:
                v = v.rearrange("p (a b) -> p a b", b=shape[2])
            pb_ = P.buf(name)
            pb_.excl = True
            return v, pb_

        identf, b_identf = SB(S0, "identf", [128, 128], F32)
        ident, b_ident = SB(S0, "ident", [128, 128], BF16)
        onesf, b_onesf = SB(S0, "onesf", [128, 128], F32)
        Uf, b_Uf = SB(S0, "Uf", [128, 128], F32)
        Lsf, b_Lsf = SB(S0, "Lsf", [128, 128], F32)
        Gsf, b_Gsf = SB(S0, "Gsf", [128, 128], F32)
        ging, b_ging = SB(S0, "ging", [128, 8], F32, dma=True)
        binb, b_binb = SB(S0, "binb", [128, 8], F32, dma=True)
        slot_i, b_slot = SB(S0, "slot_i", [128, NT, 4], I32)
        gk, b_gk = SB(S0, "gk", [128, NT, 4], F32)
        tokid, b_tokid = SB(S0, "tokid", [128, NT], I32)
        KTn, b_KTn = SB(S0, "KTn", [128, 4, 128], BF16)
        Vn, b_Vn = SB(S0, "Vn", [64, 8, 65], BF16)
        QTn, b_QTn = SB(S0, "QTn", [128, 4, 64], BF16)
        lfn, b_lfn = SB(S0, "lfn", [64, 8], F32)

        OP("pool", "memset", onesf[:], 1.0, writes=[b_onesf])
        for t_, b_, cmp_, pat_, cm_ in ((identf, b_identf, ALU.is_equal, [[-1, 128]], 1), (Uf, b_Uf, ALU.is_ge, [[1, 128]], -1),
                                        (Lsf, b_Lsf, ALU.is_gt, [[1, 128]], -1), (Gsf, b_Gsf, ALU.is_gt, [[-1, 128]], 1)):
            OP("pool", "memset", t_[:], 1.0, writes=[b_])
            OP("pool", "affine_select", out=t_[:], in_=t_[:], pattern=pat_, compare_op=cmp_, fill=0.0,
               base=0, channel_multiplier=cm_, reads=[b_], writes=[b_])
        OP("dve", "tensor_copy", out=ident[:], in_=identf[:], reads=[b_identf], writes=[b_ident])
        OP("pool", "iota", tokid[:], pattern=[[128, NT]], base=0, channel_multiplier=1, writes=[b_tokid])
        DMA("sp", ging[:], ln_in_g.rearrange("o (c p) -> p (o c)", p=128), writes=[b_ging], allow_slow_non_contiguous=True)
        DMA("sp", binb[:], ln_in_b.rearrange("o (c p) -> p (o c)", p=128), writes=[b_binb], allow_slow_non_contiguous=True)

        def ln_stats(st6, b_st6, mv, b_mv, rstd, b_rstd, xt_ap, b_xt):
            OP("dve", "bn_stats", out=st6[:, 0:6], in_=xt_ap[:, 0:512], reads=[b_xt], writes=[b_st6])
            OP("dve", "bn_stats", out=st6[:, 6:12], in_=xt_ap[:, 512:1024], reads=[b_xt], writes=[b_st6])
            OP("dve", "bn_aggr", out=mv[:], in_=st6[:], reads=[b_st6], writes=[b_mv])
            OP("dve", "tensor_scalar_add", out=rstd[:], in0=mv[:, 1:2], scalar1=EPS, reads=[b_mv], writes=[b_rstd])
            OP("act", "sqrt", out=rstd[:], in_=rstd[:], reads=[b_rstd], writes=[b_rstd])
            OP("dve", "reciprocal", out=rstd[:], in_=rstd[:], reads=[b_rstd], writes=[b_rstd])

        with contextlib.ExitStack() as SA:
            P.enabled = "A" in PHASES
            KT, _ = SB(SA, "KT", [128, 4, 65 * 128], BF16)
            KTb = [P.buf("KT%d" % g) for g in range(65)]
            V, _ = SB(SA, "V", [128, 65, 8, 65], BF16)
            Vb = [P.buf("V%d" % g) for g in range(65)]
            QT, _ = SB(SA, "QT", [128, 4, TOWN], BF16)
            QTb = [P.buf("QT%d" % t) for t in range(NT)]
            tf, b_tf = SB(SA, "tf", [128, 65, 8], F32, dma=True)
            cc, b_cc = SB(SA, "cc", [128, 64, 8], F32)
            bfb, b_bfb = SB(SA, "bfb", [128, 8], F32, dma=True)
            DMA("sp", bfb[:], b_f.broadcast_to([128, 8]), writes=[b_bfb])
            if len(A1_BLOCKS) < 65:
                OP("pool", "memset", tf[:], 0.0, writes=[b_tf])
                OP("pool", "memset", KT[:], 0.0, writes=KTb)
                OP("pool", "memset", V[:], 0.0, writes=Vb)
                OP("pool", "memset", QT[:], 0.0, writes=QTb)
            if "noVones" not in PHASES:
                OP("pool", "memset", V[:, :, :, 64:65], 1.0, writes=Vb)

            with contextlib.ExitStack() as SA1:
                wk, b_wk = SB(SA1, "wk", [128, 8, 512], BF16, dma="sw")
                wv, b_wv = SB(SA1, "wv", [128, 8, 512], BF16, dma="sw")
                wq, b_wq = wk, b_wk
                wf, b_wf = SB(SA1, "wf", [128, 8, 8], BF16, dma="sw")
                DMA("pool", wk[:], wview(w_in[:, C_K:C_K + 512]), writes=[b_wk])
                DMA("pool", wv[:], wview(w_in[:, C_V:C_V + 512]), writes=[b_wv])
                DMA("pool", wf[:], wview(w_in[:, C_F:C_F + 8]), writes=[b_wf])
                CK("wload")
                NXB = 2
                xts = [SB(SA1, "xt%d" % i, [128, D], F32, dma=True) for i in range(NXB)]
                xns = [SB(SA1, "xn%d" % i, [128, D], BF16) for i in range(2)]
                xTs = [SB(SA1, "xT%d" % i, [128, 8, 128], BF16) for i in range(2)]
                st6s = [SB(SA1, "st6_%d" % i, [128, 12], F32) for i in range(2)]
                mvs = [SB(SA1, "mv%d" % i, [128, 2], F32) for i in range(2)]
                rstds = [SB(SA1, "rstd%d" % i, [128, 1], F32) for i in range(2)]
                kTo = [SB(SA1, "kTo%d" % i, [128, 4, 128], F32, dma=True) for i in range(2)]
                vo = [SB(SA1, "vo%d" % i, [128, 512], F32, dma=True) for i in range(2)]
                pTr = [PS(SA1, "pTr%d" % i, [128, 4, 128], BF16) for i in range(2)]
                pK = [PS(SA1, "pK%d" % i, [128, 4, 128]) for i in range(2)]
                pV = [PS(SA1, "pV%d" % i, [128, 512]) for i in range(2)]
                pF, b_pF = PS(SA1, "pF", [128, 8])

                def ln_xT(i, src_ap):
                    xt, b_xt = xts[i % NXB]
                    xn, b_xn = xns[i % 2]
                    xT, b_xT = xTs[i % 2]
                    st6, b_st6 = st6s[i % 2]; mv, b_mv = mvs[i % 2]; rstd, b_rstd = rstds[i % 2]
                    DMA("sp", xt[:], src_ap, writes=[b_xt])
                    ln_stats(st6, b_st6, mv, b_mv, rstd, b_rstd, xt, b_xt)
                    OP("dve", "tensor_scalar", out=xn[:], in0=xt[:], scalar1=mv[:, 0:1], scalar2=rstd[:, 0:1],
                       op0=ALU.subtract, op1=ALU.mult, reads=[b_xt, b_mv, b_rstd], writes=[b_xn])
                    for half in range(2):
                        pt_, b_pt = pTr[half]
                        for q in range(4):
                            dc = half * 4 + q
                            OP("pe", "transpose", out=pt_[:, q, :], in_=xn[:, dc * 128:(dc + 1) * 128], identity=ident[:],
                               reads=[b_xn, b_ident], writes=[b_pt])
                        for q in range(4):
                            dc = half * 4 + q
                            OP("act", "activation", out=xT[:, dc, :], in_=pt_[:, q, :], func=AF.Identity,
                               bias=binb[:, dc:dc + 1], scale=ging[:, dc:dc + 1],
                               reads=[b_pt, b_ging, b_binb], writes=[b_xT])
                    return xT, b_xT

                for g in A1_BLOCKS:
                    src = xb[g * 128:(g + 1) * 128, :] if g < 64 else xo[2048:2176, :]
                    xT, b_xT = ln_xT(g, src)
                    CK("lnxT")
                    pk, b_pk = pK[g % 2]
                    for hp in range(4):
                        for dc in range(8):
                            OP("pe", "matmul", pk[:, hp, :], lhsT=wk[:, dc, hp * 128:(hp + 1) * 128], rhs=xT[:, dc, :],
                               start=(dc == 0), stop=(dc == 7), reads=[b_xT, b_wk], writes=[b_pk],
                               inc=(hp == 3 and dc == 7))
                    pv, b_pv = pV[g % 2]
                    for dc in range(8):
                        OP("pe", "matmul", pv[:], lhsT=xT[:, dc, :], rhs=wv[:, dc, :], start=(dc == 0), stop=(dc == 7),
                           reads=[b_xT, b_wv], writes=[b_pv], inc=(dc == 7))
                    for dc in range(8):
                        OP("pe", "matmul", pF[:], lhsT=xT[:, dc, :], rhs=wf[:, dc, :], start=(dc == 0), stop=(dc == 7),
                           reads=[b_xT, b_wf], writes=[b_pF], inc=(dc == 7))
                    CK("mm")
                    OP("dve", "tensor_copy", out=KT[:, :, g * 128:(g + 1) * 128], in_=pk[:], reads=[b_pk], writes=[KTb[g]])
                    CK("ktcopy")
                    ko, b_ko = kTo[g % 2]
                    OP("act", "copy", out=ko[:], in_=pk[:], reads=[b_pk], writes=[b_ko])
                    DMA("sp", o_kT[:, g * 128:(g + 1) * 128].rearrange("(c p) t -> p c t", p=128), ko[:], reads=[b_ko])
                    CK("kout")
                    OP("dve", "tensor_copy", out=V[:, g, :, 0:64], in_=pv[:].rearrange("p (h d) -> p h d", d=64),
                       reads=[b_pv], writes=[Vb[g]])
                    vo_, b_vo = vo[g % 2]
                    OP("act", "copy", out=vo_[:], in_=pv[:], reads=[b_pv], writes=[b_vo])
                    DMA("sp", o_v[g * 128:(g + 1) * 128, :], vo_[:], reads=[b_vo])
                    OP("dve", "tensor_tensor", out=tf[:, g, :], in0=pF[:], in1=bfb[:], op=ALU.add,
                       reads=[b_pF, b_bfb], writes=[b_tf])
                    CK("evac")
                CK("a1loop")
                tf2 = tf[:].rearrange("p g h -> p (g h)")
                OP("act", "activation", out=tf2, in_=tf2, func=AF.Exp, scale=-1.0, reads=[b_tf], writes=[b_tf])
                OP("act", "activation", out=tf2, in_=tf2, func=AF.Ln, bias=1.0, scale=1.0, reads=[b_tf], writes=[b_tf])
                OP("dve", "tensor_scalar_mul", out=tf2, in0=tf2, scalar1=-1.0, reads=[b_tf], writes=[b_tf])
                if "noLogfOut" not in PHASES:
                    for g in range(65):
                        DMA("sp", o_logf[g * 128:(g + 1) * 128, :], tf[:, g, :], reads=[b_tf])
                OP("pool", "tensor_copy", out=KTn[:], in_=KT[:, :, 8192:8320], reads=[KTb[64]], writes=[b_KTn])
                OP("pool", "tensor_copy", out=Vn[:], in_=V[0:64, 64, :, :], reads=[Vb[64]], writes=[b_Vn])
                OP("pool", "tensor_copy", out=lfn[:], in_=tf[0:64, 64, :], reads=[b_tf], writes=[b_lfn])

                CK("logf")
                pC, b_pC = pV[0]
                pTt, b_pTt = pV[1]
                lf512 = tf[:, 0:64, :].rearrange("p g h -> p (g h)")
                OP("pe", "matmul", pC[:], lhsT=Uf[:], rhs=lf512, start=True, stop=True, reads=[b_Uf, b_tf], writes=[b_pC])
                OP("pe", "matmul", pTt[:], lhsT=onesf[:], rhs=lf512, start=True, stop=True, reads=[b_onesf, b_tf], writes=[b_pTt])
                sa, b_sa = SB(SA1, "sa", [128, 64, 8], F32)
                sb_, b_sb = SB(SA1, "sbb", [128, 64, 8], F32)
                tot, b_tot = SB(SA1, "tot", [128, 64, 8], F32)
                OP("dve", "tensor_copy", out=tot[:].rearrange("p g h -> p (g h)"), in_=pTt[:], reads=[b_pTt], writes=[b_tot])
                OP("dve", "tensor_copy", out=sa[:], in_=tot[:], reads=[b_tot], writes=[b_sa])
                cur, b_cur, nxt, b_nxt = sa, b_sa, sb_, b_sb
                for sft in (1, 2, 4, 8, 16, 32):
                    OP("dve", "tensor_tensor", out=nxt[:, sft:64, :], in0=cur[:, sft:64, :], in1=cur[:, 0:64 - sft, :], op=ALU.add,
                       reads=[b_cur], writes=[b_nxt])
                    OP("dve", "tensor_copy", out=nxt[:, 0:sft, :], in_=cur[:, 0:sft, :], reads=[b_cur], writes=[b_nxt])
                    cur, b_cur, nxt, b_nxt = nxt, b_nxt, cur, b_cur
                OP("dve", "tensor_tensor", out=cur[:], in0=cur[:], in1=tot[:], op=ALU.subtract, reads=[b_cur, b_tot], writes=[b_cur])
                OP("dve", "tensor_tensor", out=cc[:].rearrange("p g h -> p (g h)"), in0=pC[:],
                   in1=cur[:].rearrange("p g h -> p (g h)"), op=ALU.add, reads=[b_pC, b_cur], writes=[b_cc])

                CK("cumsum")
                DMA("pool", wq[:], wview(w_in[:, C_Q:C_Q + 512]), writes=[b_wq])
                for t in A2_TILES:
                    xT, b_xT = ln_xT(65 + t, xo[t * 128:(t + 1) * 128, :])
                    pk, b_pk = pK[t % 2]
                    for hp in range(4):
                        for dc in range(8):
                            OP("pe", "matmul", pk[:, hp, :], lhsT=wq[:, dc, hp * 128:(hp + 1) * 128], rhs=xT[:, dc, :],
                               start=(dc == 0), stop=(dc == 7), reads=[b_xT, b_wq], writes=[b_pk],
                               inc=(hp == 3 and dc == 7))
                    OP("dve", "tensor_scalar_mul", out=QT[:, :, t * 128:(t + 1) * 128], in0=pk[:], scalar1=0.125,
                       reads=[b_pk], writes=[QTb[t]])
                OP("pool", "tensor_copy", out=QTn[:], in_=QT[:, :, 2048:2112], reads=[QTb[16]], writes=[b_QTn])
                P.barrier()
                P.release(b_wk, b_wv, b_wf, *[b for _, b in xts], *[b for _, b in kTo], *[b for _, b in vo])

            with contextlib.ExitStack() as SA3:
                P.enabled = "A3" in PHASES
                dm, b_dm = SB(SA3, "dm", [128, 16, 512], BF16, dma=True)
                selt, b_selt = SB(SA3, "selt", [128, 4, 64], F32, dma=True)
                DMA("sp", dm[:], dmask, writes=[b_dm])
                DMA("sp", selt[:], sel, writes=[b_selt])
                kvt, b_kvt = SB(SA3, "kvt", [128, 4, 64], F32, dma=True)
                DMA("sp", kvt[:], kvis, writes=[b_kvt])
                tmpc, b_tmpc = SB(SA3, "tmpc", [128, 64, 8], F32)
                red, b_red = SB(SA3, "red", [128, 8], F32)
                biasm = [SB(SA3, "biasm%d" % i, [128, 64, 8], F32) for i in range(2)]
                pTs = [SB(SA3, "pTs%d" % i, [128, 512], BF16) for i in range(3)]
                rs, b_rs = SB(SA3, "rs", [128, 512], F32)
                bcs, b_bcs = SB(SA3, "bcs", [64, 512], F32)
                yTs = [SB(SA3, "yTs%d" % i, [64, 512], BF16, dma=True) for i in range(2)]
                pS = [PS(SA3, "pS%d" % i, [128, 512]) for i in range(3)]
                pAcc = [PS(SA3, "pAcc%d" % i, [128, 512]) for i in range(2)]
                pB, b_pB = PS(SA3, "pB", [128, 512])
                pSh, b_pSh = PS(SA3, "pSh", [128, 8])
                it = 0
                for m in range(4):
                    bm, b_bm = biasm[m % 2]
                    OP("dve", "tensor_tensor", out=tmpc[:], in0=cc[:], in1=selt[:, m, :].unsqueeze(2).broadcast_to([128, 64, 8]),
                       op=ALU.mult, reads=[b_cc, b_selt], writes=[b_tmpc])
                    OP("dve", "tensor_reduce", out=red[:], in_=tmpc[:].rearrange("p g h -> p h g"), axis=AX.X, op=ALU.add,
                       reads=[b_tmpc], writes=[b_red])
                    OP("pe", "matmul", pSh[:], lhsT=onesf[:], rhs=red[:], start=True, stop=True, reads=[b_onesf, b_red], writes=[b_pSh])
                    OP("dve", "tensor_tensor", out=bm[:], in0=pSh[:].unsqueeze(1).broadcast_to([128, 64, 8]), in1=cc[:],
                       op=ALU.subtract, reads=[b_pSh, b_cc], writes=[b_bm])
                    OP("dve", "tensor_tensor", out=bm[:], in0=bm[:], in1=kvt[:, m, :].unsqueeze(2).broadcast_to([128, 64, 8]),
                       op=ALU.add, reads=[b_bm, b_kvt], writes=[b_bm])
                    nkb = 16 * m + 16
                    for h in range(H):
                        hp, hd0 = h // 2, (h % 2) * 64
                        acc, b_acc = pAcc[(m * H + h) % 2]

                        def s_mm(kb, it_):
                            ps_, b_ps = pS[it_ % 3]
                            OP("pe", "matmul", ps_[:], lhsT=KT[hd0:hd0 + 64, hp, kb * 128:(kb + 1) * 128],
                               rhs=QT[hd0:hd0 + 64, hp, m * 512:(m + 1) * 512], start=True, stop=True,
                               reads=[KTb[kb]] + QTb[4 * m:4 * m + 4], writes=[b_ps])
                        s_mm(0, it)
                        for kb in range(nkb):
                            if kb + 1 < nkb:
                                s_mm(kb + 1, it + 1)
                            ps_, b_ps = pS[it % 3]
                            pT_, b_pT = pTs[it % 3]
                            OP("act", "activation", out=pT_[:], in_=ps_[:], func=AF.Exp, bias=bm[:, kb, h:h + 1], scale=1.0,
                               reads=[b_ps, b_bm], writes=[b_pT])
                            if kb >= 16 * m:
                                OP("pool", "tensor_tensor", out=pT_[:], in0=pT_[:], in1=dm[:, kb - 16 * m, :], op=ALU.mult,
                                   reads=[b_pT, b_dm], writes=[b_pT])
                            OP("pe", "matmul", acc[0:65, :], lhsT=V[:, kb, h, :], rhs=pT_[:], start=(kb == 0), stop=(kb == nkb - 1),
                               reads=[Vb[kb], b_pT], writes=[b_acc], inc=(kb == nkb - 1))
                            it += 1
                        OP("dve", "reciprocal", out=rs[64:65, :], in_=acc[64:65, :], reads=[b_acc], writes=[b_rs])
                        OP("pe", "matmul", pB[0:64, :], lhsT=onesf[64:65, 0:64], rhs=rs[64:65, :], start=True, stop=True,
                           reads=[b_onesf, b_rs], writes=[b_pB])
                        OP("act", "copy", out=bcs[:], in_=pB[0:64, :], reads=[b_pB], writes=[b_bcs])
                        yT_, b_yT = yTs[h % 2]
                        OP("dve", "tensor_tensor", out=yT_[:], in0=acc[0:64, :], in1=bcs[:], op=ALU.mult,
                           reads=[b_acc, b_bcs], writes=[b_yT])
                        DMA("sp", yat[h, :, m * 512:(m + 1) * 512], yT_[:], reads=[b_yT, b_yat], owner=b_yT)
                P.barrier()
                P.release(b_dm, b_selt, *[b for _, b in yTs])
            P.release(b_tf, b_bfb)

        with contextlib.ExitStack() as S4:
            P.enabled = "A4" in PHASES
            ptb, b_ptb = SB(S4, "ptb", [128, 256], I32, dma=True)
            DMA("sp", ptb[:], pt.broadcast_to([128, 256]), writes=[b_ptb])
            pio, b_pio = SB(S4, "pio", [128, 1], I32)
            OP("pool", "iota", pio[:], pattern=[[0, 1]], base=0, channel_multiplier=1, writes=[b_pio])
            ridx, b_ridx = SB(S4, "ridx", [128, 256], I32)
            ptf, b_ptf = SB(S4, "ptf", [128, 256], F32)
            piof, b_piof = SB(S4, "piof", [128, 1], F32)
            OP("dve", "tensor_copy", out=ptf[:], in_=ptb[:], reads=[b_ptb], writes=[b_ptf])
            OP("dve", "tensor_copy", out=piof[:], in_=pio[:], reads=[b_pio], writes=[b_piof])
            OP("dve", "tensor_scalar", out=ptf[:], in0=ptf[:], scalar1=128.0, scalar2=piof[:, 0:1], op0=ALU.mult, op1=ALU.add,
               reads=[b_ptf, b_piof], writes=[b_ptf])
            OP("dve", "tensor_copy", out=ridx[:], in_=ptf[:], reads=[b_ptf], writes=[b_ridx])
            CK("ridx")
            bdt, b_bdt = SB(S4, "bdt", [64, 64], F32, dma=True)
            DMA("sp", bdt[:], bd, writes=[b_bdt])
            bdb, b_bdb = SB(S4, "bdb", [64, 64], BF16)
            OP("dve", "tensor_copy", out=bdb[:], in_=bdt[:], reads=[b_bdt], writes=[b_bdb])
            NRING = 6
            kst = [SB(S4, "kst%d" % i, [128, 512], F32, dma="sw") for i in range(NRING)]
            vst = [SB(S4, "vst%d" % i, [128, 512], F32, dma="sw") for i in range(NRING)]
            fst = [SB(S4, "fst%d" % i, [128, 16, 8], F32, dma="sw") for i in range(2)]
            kb16 = [SB(S4, "kb16_%d" % i, [128, 512], BF16) for i in range(2)]
            KTs = [SB(S4, "KTs%d" % i, [128, 4, 2048], BF16) for i in range(2)]
            Vs = [SB(S4, "Vs%d" % i, [128, 16, 8, 65], BF16) for i in range(2)]
            for i in range(2):
                OP("pool", "memset", Vs[i][0][:, :, :, 64:65], 1.0, writes=[Vs[i][1]])
            bia, b_bia = SB(S4, "bia", [128, 16, 8], F32)
            ta, b_ta = SB(S4, "ta", [128, 16, 8], F32)
            tb_, b_tb = SB(S4, "tbb", [128, 16, 8], F32)
            tt, b_tt = SB(S4, "tt", [128, 16, 8], F32)
            scs, b_scs = SB(S4, "scs", [128, 16, 8, 4], F32)
            pts_, b_pts = SB(S4, "pts", [128, 16, 8, 4], BF16)
            pTk = [PS(S4, "pTk%d" % i, [128, 4, 128], BF16) for i in range(2)]
            pSs, b_pSs = PS(S4, "pSs", [128, 512])
            pSo, b_pSo = PS(S4, "pSo", [128, 512])
            pSpar = [(pSs, b_pSs), (pSo, b_pSo)]
            pWT, b_pW = PS(S4, "pWT", [128, 256])
            pW = pWT[:, 0:128]
            pTo = pWT[:, 128:256]
            b_pTo = b_pW
            pAp, b_pAp = PS(S4, "pAp", [128, 8, 64])
            pAn, b_pAn = PS(S4, "pAn", [128, 8, 64])
            if len(A4_SEQS) < 16:
                OP("dve", "memset", pAp[:], 0.0, writes=[b_pAp])
            for s in A4_SEQS:
                KTs_, b_KTs = KTs[s % 2]
                Vs_, b_Vs = Vs[s % 2]
                f_, b_f_ = fst[s % 2]
                for pg in range(16):
                    col = s * 16 + pg
                    k_, b_k = kst[pg % NRING]
                    v_, b_v = vst[pg % NRING]
                    P.dma("pool", lambda e, k_=k_, col=col: e.indirect_dma_start(
                        out=k_[:], out_offset=None, in_=cache_k,
                        in_offset=bass.IndirectOffsetOnAxis(ap=ridx[:, col:col + 1], axis=0)),
                        reads=[b_ridx], writes=[b_k])
                    P.dma("pool", lambda e, v_=v_, col=col: e.indirect_dma_start(
                        out=v_[:], out_offset=None, in_=cache_v,
                        in_offset=bass.IndirectOffsetOnAxis(ap=ridx[:, col:col + 1], axis=0)),
                        reads=[b_ridx], writes=[b_v])
                    P.dma("pool", lambda e, f_=f_, col=col, pg=pg: e.indirect_dma_start(
                        out=f_[:, pg, :], out_offset=None, in_=cache_f,
                        in_offset=bass.IndirectOffsetOnAxis(ap=ridx[:, col:col + 1], axis=0)),
                        reads=[b_ridx], writes=[b_f_])
                    CK("gather1")
                    kb_, b_kb = kb16[pg % 2]
                    OP("dve", "tensor_copy", out=kb_[:], in_=k_[:], reads=[b_k], writes=[b_kb])
                    ptk, b_ptk = pTk[pg % 2]
                    for hp in range(4):
                        OP("pe", "transpose", out=ptk[:, hp, :], in_=kb_[:, hp * 128:(hp + 1) * 128], identity=ident[:],
                           reads=[b_kb, b_ident], writes=[b_ptk])
                    OP("act", "copy", out=KTs_[:, :, pg * 128:(pg + 1) * 128], in_=ptk[:], reads=[b_ptk], writes=[b_KTs])
                    OP("act", "copy", out=Vs_[:, pg, :, 0:64], in_=v_[:].rearrange("p (h d) -> p h d", d=64),
                       reads=[b_v], writes=[b_Vs])
                CK("pages")
                f128 = f_[:].rearrange("p g h -> p (g h)")
                OP("pe", "matmul", pW[:], lhsT=Gsf[:], rhs=f128, start=True, stop=True, reads=[b_Gsf, b_f_], writes=[b_pW])
                OP("pe", "matmul", pTo[:], lhsT=onesf[:], rhs=f128, start=True, stop=True, reads=[b_onesf, b_f_], writes=[b_pTo])
                OP("dve", "tensor_copy", out=tt[:].rearrange("p g h -> p (g h)"), in_=pTo[:], reads=[b_pTo], writes=[b_tt])
                OP("dve", "tensor_copy", out=ta[:], in_=tt[:], reads=[b_tt], writes=[b_ta])
                cur, b_cur, nxt, b_nxt = ta, b_ta, tb_, b_tb
                for sft in (1, 2, 4, 8):
                    OP("dve", "tensor_tensor", out=nxt[:, 0:16 - sft, :], in0=cur[:, 0:16 - sft, :], in1=cur[:, sft:16, :], op=ALU.add,
                       reads=[b_cur], writes=[b_nxt])
                    OP("dve", "tensor_copy", out=nxt[:, 16 - sft:16, :], in_=cur[:, 16 - sft:16, :], reads=[b_cur], writes=[b_nxt])
                    cur, b_cur, nxt, b_nxt = nxt, b_nxt, cur, b_cur
                OP("dve", "tensor_tensor", out=cur[:], in0=cur[:], in1=tt[:], op=ALU.subtract, reads=[b_cur, b_tt], writes=[b_cur])
                OP("dve", "tensor_tensor", out=bia[:].rearrange("p g h -> p (g h)"), in0=pW[:],
                   in1=cur[:].rearrange("p g h -> p (g h)"), op=ALU.add, reads=[b_pW, b_cur], writes=[b_bia])
                for par in range(2):
                    pS_, b_pS_ = pSpar[par]
                    for pg in range(16):
                        for hh in range(4):
                            hp, hd0 = hh, par * 64
                            c0 = (pg * 4 + hh) * 4
                            OP("pe", "matmul", pS_[:, c0:c0 + 4],
                               lhsT=KTs_[hd0:hd0 + 64, hp, pg * 128:(pg + 1) * 128], rhs=QTn[hd0:hd0 + 64, hp, 4 * s:4 * s + 4],
                               start=True, stop=True, reads=[b_KTs, b_QTn], writes=[b_pS_], inc=(pg == 15 and hh == 3))
                for par in range(2):
                    pS_, b_pS_ = pSpar[par]
                    OP("dve", "tensor_tensor",
                       out=scs[:].rearrange("p g (hh two) q -> p g hh two q", two=2)[:, :, :, par, :],
                       in0=pS_[:, 0:256].rearrange("p (g hh q) -> p g hh q", hh=4, q=4),
                       in1=bia[:].rearrange("p g (hh two) -> p g hh two", two=2)[:, :, :, par].unsqueeze(3).broadcast_to([128, 16, 4, 4]),
                       op=ALU.add, reads=[b_pS_, b_bia], writes=[b_scs])
                OP("act", "activation", out=pts_[:].rearrange("p g h q -> p (g h q)"), in_=scs[:].rearrange("p g h q -> p (g h q)"),
                   func=AF.Exp, reads=[b_scs], writes=[b_pts])
                for h in range(H):
                    for pg in range(16):
                        OP("pe", "matmul", pAp[0:65, h, 4 * s:4 * s + 4], lhsT=Vs_[:, pg, h, :], rhs=pts_[:, pg, h, :],
                           start=(pg == 0), stop=(pg == 15), reads=[b_Vs, b_pts], writes=[b_pAp], inc=(pg == 15 and h == 7))
                CK("seq1")
            csn, b_csn = SB(S4, "csn", [64, 8], F32)
            OP("pe", "matmul", pW[0:64, 0:8], lhsT=bdt[:], rhs=lfn[:], start=True, stop=True, reads=[b_bdt, b_lfn], writes=[b_pW])
            OP("dve", "tensor_scalar_mul", out=csn[:], in0=pW[0:64, 0:8], scalar1=-1.0, reads=[b_pW], writes=[b_csn])
            CK("t1")
            for par in range(2):
                pS_, b_pS_ = pSpar[par]
                for hh in range(4):
                    OP("pe", "matmul", pS_[:, hh * 64:(hh + 1) * 64], lhsT=KTn[par * 64:par * 64 + 64, hh, :],
                       rhs=QTn[par * 64:par * 64 + 64, hh, :], start=True, stop=True,
                       reads=[b_KTn, b_QTn], writes=[b_pS_], inc=(hh == 3))
            CK("t2")
            scn, b_scn = SB(S4, "scn", [64, 8, 64], F32)
            ptn, b_ptn = SB(S4, "ptn", [64, 8, 64], BF16)
            for par in range(2):
                pS_, b_pS_ = pSpar[par]
                OP("dve", "tensor_tensor", out=scn[:].rearrange("p (hh two) q -> p hh two q", two=2)[:, :, par, :],
                   in0=pS_[0:64, 0:256].rearrange("p (hh q) -> p hh q", q=64),
                   in1=csn[:].rearrange("p (hh two) -> p hh two", two=2)[:, :, par].unsqueeze(2).broadcast_to([64, 4, 64]),
                   op=ALU.add, reads=[b_pS_, b_csn], writes=[b_scn])
            OP("act", "activation", out=ptn[:], in_=scn[:], func=AF.Exp, reads=[b_scn], writes=[b_ptn])
            OP("dve", "tensor_tensor", out=ptn[:], in0=ptn[:], in1=bdb[:].unsqueeze(1).broadcast_to([64, 8, 64]), op=ALU.mult,
               reads=[b_ptn, b_bdb], writes=[b_ptn])
            CK("t3")
            for h in range(H):
                OP("pe", "matmul", pAn[0:65, h, :], lhsT=Vn[:, h, :], rhs=ptn[:, h, :], start=True, stop=True,
                   reads=[b_Vn, b_ptn], writes=[b_pAn], inc=(h == 7))
            CK("t4")
            asum, b_asum = SB(S4, "asum", [128, 8, 64], F32)
            OP("dve", "tensor_copy", out=asum[0:65], in_=pAp[0:65], reads=[b_pAp], writes=[b_asum])
            OP("dve", "tensor_tensor", out=asum[0:65], in0=asum[0:65], in1=pAn[0:65], op=ALU.add, reads=[b_asum, b_pAn], writes=[b_asum])
            rs2, b_rs2 = SB(S4, "rs2", [128, 512], F32)
            OP("dve", "reciprocal", out=rs2[64:65, :], in_=asum[64:65].rearrange("p h q -> p (h q)"), reads=[b_asum], writes=[b_rs2])
            OP("pe", "matmul", pSs[0:64, :], lhsT=onesf[64:65, 0:64], rhs=rs2[64:65, :], start=True, stop=True,
               reads=[b_onesf, b_rs2], writes=[b_pSs])
            CK("t5")
            ysn, b_ysn = SB(S4, "ysn", [64, 8, 64], BF16, dma=True)
            OP("dve", "tensor_tensor", out=ysn[:].rearrange("p h q -> p (h q)"), in0=asum[0:64].rearrange("p h q -> p (h q)"),
               in1=pSs[0:64, :], op=ALU.mult, reads=[b_asum, b_pSs], writes=[b_ysn])
            DMA("sp", yat[:, :, 2048:2112].rearrange("h d q -> d h q"), ysn[:], reads=[b_ysn, b_yat], owner=b_ysn)
            zpad, b_zpad = SB(S4, "zpad", [64, 8, 64], BF16, dma=True)
            OP("pool", "memset", zpad[:], 0.0, writes=[b_zpad])
            DMA("sp", yat[:, :, 2112:2176].rearrange("h d q -> d h q"), zpad[:], reads=[b_zpad, b_yat], owner=b_zpad)
            P.barrier()
            P.release(b_ptb, b_bdt, b_ysn, b_zpad, *[b for _, b in kst], *[b for _, b in vst], *[b for _, b in fst])

        with contextlib.ExitStack() as SBk:
            P.enabled = "B" in PHASES
            wcv, b_wcv = SB(SBk, "wcv", [128, 8, 1536], BF16, dma="sw")
            wgc, b_wgc = SB(SBk, "wgc", [128, 8, 1024], BF16, dma="sw")
            wga, b_wga = SB(SBk, "wga", [128, 8, 1024], BF16, dma="sw")
            wbc, b_wbc = SB(SBk, "wbc", [128, 4, 1024], BF16, dma="sw")
            wba, b_wba = SB(SBk, "wba", [64, 8, 1024], BF16, dma="sw")
            wo, b_wo = SB(SBk, "wo", [128, 8, 1024], BF16, dma="sw")
            wr, b_wr = SB(SBk, "wr", [128, 8, 32], F32, dma=True)
            DMA("pool", wcv[:], wview(w_in[:, 0:1536]), writes=[b_wcv])
            DMA("pool", wgc[:], wview(w_in[:, C_GC:C_GC + 1024]), writes=[b_wgc])
            DMA("pool", wga[:], wview(w_in[:, C_GA:C_GA + 1024]), writes=[b_wga])
            DMA("pool", wbc[:], wview(w_br_conv), writes=[b_wbc])
            DMA("pool", wba[:], w_br_attn.rearrange("(h d) n -> d h n", d=64), writes=[b_wba])
            DMA("pool", wo[:], wview(w_o), writes=[b_wo])
            DMA("sp", wr[:], wview(w_router), writes=[b_wr])
            cw, b_cw = SB(SBk, "cw", [128, 3, 4], F32, dma=True)
            for k_ in range(3):
                DMA("sp", cw[:, k_, :], conv_w[k_:k_ + 1, :].rearrange("o (c p) -> p (o c)", p=128), writes=[b_cw],
                    allow_slow_non_contiguous=True)
            hm, b_hm = SB(SBk, "hm", [128, 8], F32, dma=True)
            DMA("sp", hm[:], hmask, writes=[b_hm])
            gin_bc, b_gin_bc = SB(SBk, "gin_bc", [128, D], F32, dma=True)
            bin_bc, b_bin_bc = SB(SBk, "bin_bc", [128, D], F32, dma=True)
            g1_bc, b_g1_bc = SB(SBk, "g1_bc", [128, D], F32, dma=True)
            b1_bc, b_b1_bc = SB(SBk, "b1_bc", [128, D], F32, dma=True)
            brb, b_brb = SB(SBk, "brb", [128, E], F32, dma=True)
            DMA("sp", gin_bc[:], ln_in_g.broadcast_to([128, D]), writes=[b_gin_bc])
            DMA("sp", bin_bc[:], ln_in_b.broadcast_to([128, D]), writes=[b_bin_bc])
            DMA("sp", g1_bc[:], ln1_g.broadcast_to([128, D]), writes=[b_g1_bc])
            DMA("sp", b1_bc[:], ln1_b.broadcast_to([128, D]), writes=[b_b1_bc])
            DMA("sp", brb[:], b_router.broadcast_to([128, E]), writes=[b_brb])
            eoff, b_eoff = SB(SBk, "eoff", [128, E], F32)
            OP("pool", "iota", eoff[:], pattern=[[CAP, E]], base=1, channel_multiplier=0, allow_small_or_imprecise_dtypes=True,
               writes=[b_eoff])
            macc, b_macc = SB(SBk, "macc", [128, E], F32)
            OP("pool", "memset", macc[:], 0.0, writes=[b_macc])
            ztb, b_ztb = SB(SBk, "ztb", [128, 96], I32, dma=True)
            OP("pool", "memset", ztb[:], 0, writes=[b_ztb])
            DMA("sp", tbl.rearrange("(p f) o -> p (f o)", p=128), ztb[:], reads=[b_ztb], writes=[b_tbl], owner=b_ztb)
            uh, b_uh = SB(SBk, "uh", [128, 4, 8], BF16)
            scT, b_scT = SB(SBk, "scT", [32, 512], F32, dma=True)
            DMA("sp", scT[:], sconv, writes=[b_scT])
            ush, b_ush = SB(SBk, "ush", [128, 4, 32], BF16)

            NG = 256
            xt4 = [SB(SBk, "bxt%d" % i, [128, D], F32, dma=True) for i in range(3)]
            xln = [SB(SBk, "xln%d" % i, [128, D], F32) for i in range(2)]
            xnb = [SB(SBk, "bxn%d" % i, [128, D], BF16) for i in range(2)]
            xTg, b_xTg = SB(SBk, "xTg", [128, 8, NG], BF16)
            st6, b_st6 = SB(SBk, "bst6", [128, 12], F32)
            mv, b_mv = SB(SBk, "bmv", [128, 2], F32)
            rstd, b_rstd = SB(SBk, "brstd", [128, 1], F32)
            ccs, b_ccs = SB(SBk, "ccs", [128, NG], F32)
            ext, b_ext = SB(SBk, "ext", [128, 4, NG + 2], BF16)
            exs, b_exs = SB(SBk, "exs", [128, 4, 16, 6], BF16)
            yc, b_yc = SB(SBk, "yc", [128, NG], F32)
            ycT, b_ycT = SB(SBk, "ycT", [128, 4, NG], BF16)
            yaT, b_yaT = SB(SBk, "yaT", [64, 8, NG], BF16, dma=True)
            sgc, b_sgc = SB(SBk, "sgc", [128, NG], F32)
            sga, b_sga = SB(SBk, "sga", [128, NG], F32)
            t1, b_t1 = SB(SBk, "t1", [128, NG], F32)
            mT, b_mT = SB(SBk, "mT", [128, 8, NG], BF16)
            x1t = [SB(SBk, "x1t%d" % i, [128, D], F32, dma=True) for i in range(2)]
            x1bt = [SB(SBk, "x1bt%d" % i, [128, D], BF16, dma=True) for i in range(2)]
            x1T, b_x1T = SB(SBk, "x1T", [128, 8, 128], F32)
            lg, b_lg = SB(SBk, "lg", [128, E], F32)
            m8, b_m8 = SB(SBk, "m8", [128, 8], F32)
            msk, b_msk = SB(SBk, "msk", [128, E], F32)
            ex, b_ex = SB(SBk, "ex", [128, E], F32)
            sm, b_sm = SB(SBk, "sm", [128, 4], F32)
            Gt, b_Gt = SB(SBk, "Gt", [128, E], F32)
            key, b_key = SB(SBk, "key", [128, E], F32)
            k8, b_k8 = SB(SBk, "k8", [128, 8], F32)
            oh, b_oh = SB(SBk, "oh", [128, E], F32)
            sf, b_sf = SB(SBk, "sf", [128, 4], F32)
            convo, b_convo = SB(SBk, "convo", [128, 4, 32], F32, dma=True)
            convp, b_convp = SB(SBk, "convp", [128, 4, 2], F32, dma=True)

            pTr = [PS(SBk, "bpTr%d" % i, [128, 4, 128], BF16) for i in range(2)]
            pA = [PS(SBk, "bpA%d" % i, [128, 512]) for i in range(4)]
            pX, b_pX = PS(SBk, "bpX", [128, 4, 128])
            pL, b_pL = PS(SBk, "bpL", [128, 64])
            pa_i = [0]

            def nextpA():
                r = pA[pa_i[0] % 4]
                pa_i[0] += 1
                return r

            for ci in range(4):
                OP("pe", "transpose", out=pX[:, ci, 0:32], in_=scT[:, ci * 128:(ci + 1) * 128], identity=identf[0:32, 0:32],
                   reads=[b_scT, b_identf], writes=[b_pX])
            OP("dve", "tensor_copy", out=ush[:], in_=pX[:, :, 0:32], reads=[b_pX], writes=[b_ush])

            tile_ctr = [0]

            def group(rows0, n, kind):
                nt = n // 128
                lnt = []
                for ti in range(nt):
                    i = tile_ctr[0]; tile_ctr[0] += 1
                    xt, b_xt = xt4[i % 3]
                    src = xh if kind == "halo" else xo[rows0 + ti * 128: rows0 + (ti + 1) * 128, :]
                    DMA("sp", xt[:], src, writes=[b_xt])
                    ln_stats(st6, b_st6, mv, b_mv, rstd, b_rstd, xt, b_xt)
                    xn_, b_xn = xnb[i % 2]
                    OP("dve", "tensor_scalar", out=xn_[:], in0=xt[:], scalar1=mv[:, 0:1], scalar2=rstd[:, 0:1],
                       op0=ALU.subtract, op1=ALU.mult, reads=[b_xt, b_mv, b_rstd], writes=[b_xn])
                    if kind != "halo":
                        xl, b_xl = xln[ti % 2]
                        OP("pool", "tensor_tensor", out=xl[:], in0=xn_[:], in1=gin_bc[:], op=ALU.mult, reads=[b_xn, b_gin_bc], writes=[b_xl])
                        OP("pool", "tensor_tensor", out=xl[:], in0=xl[:], in1=bin_bc[:], op=ALU.add, reads=[b_xl, b_bin_bc], writes=[b_xl])
                        lnt.append((xl, b_xl))
                    for half in range(2):
                        pt_, b_pt = pTr[half]
                        for q in range(4):
                            dc = half * 4 + q
                            OP("pe", "transpose", out=pt_[:, q, :], in_=xn_[:, dc * 128:(dc + 1) * 128], identity=ident[:],
                               reads=[b_xn, b_ident], writes=[b_pt])
                        for q in range(4):
                            dc = half * 4 + q
                            OP("act", "activation", out=xTg[:, dc, ti * 128:(ti + 1) * 128], in_=pt_[:, q, :], func=AF.Identity,
                               bias=binb[:, dc:dc + 1], scale=ging[:, dc:dc + 1], reads=[b_pt, b_ging, b_binb], writes=[b_xTg])
                return lnt

            def proj_fm(wt, b_wt, col0, ps_ap, b_ps, n, last=True):
                for dc in range(8):
                    OP("pe", "matmul", ps_ap, lhsT=wt[:, dc, col0:col0 + 128], rhs=xTg[:, dc, 0:n], start=(dc == 0), stop=(dc == 7),
                       reads=[b_xTg, b_wt], writes=[b_ps], inc=(dc == 7))

            def conv_u(n, ci, dst_ap):
                pc, b_pc = nextpA()
                proj_fm(wcv, b_wcv, 512 + ci * 128, pc[:, 0:n], b_pc, n)
                ph, b_ph = nextpA()
                proj_fm(wcv, b_wcv, 1024 + ci * 128, ph[:, 0:n], b_ph, n)
                OP("act", "copy", out=ccs[:, 0:n], in_=pc[:, 0:n], reads=[b_pc], writes=[b_ccs])
                return ph, b_ph

            group(0, 128, "halo")
            for ci in range(4):
                ph, b_ph = conv_u(128, ci, None)
                OP("dve", "tensor_tensor", out=yc[:, 0:8], in0=ph[:, 0:8], in1=ccs[:, 0:8], op=ALU.mult, reads=[b_ph, b_ccs], writes=[b_yc])
                OP("dve", "tensor_tensor", out=uh[:, ci, :], in0=yc[:, 0:8], in1=hm[:], op=ALU.mult, reads=[b_yc, b_hm], writes=[b_uh])

            def token_groups():
                for gi in range(8):
                    yield gi * 256, 256, "prompt", gi
                yield 2048, 128, "sample", 8

            for rows0, n, kind, gi in token_groups():
                lnt = group(rows0, n, kind)
                DMA("sp", yaT[:, :, 0:n], yat[:, :, rows0:rows0 + n].rearrange("h d t -> d h t"), reads=[b_yat], writes=[b_yaT])
                for ci in range(4):
                    ph, b_ph = conv_u(n, ci, None)
                    if kind == "prompt":
                        m, half = gi // 2, gi % 2
                        if half == 0:
                            OP("pool", "tensor_copy", out=ext[:, ci, 0:2], in_=uh[:, ci, 2 * m:2 * m + 2], reads=[b_uh], writes=[b_ext])
                        else:
                            OP("pool", "tensor_copy", out=ext[:, ci, 0:2], in_=ext[:, ci, n:n + 2], reads=[b_ext], writes=[b_ext])
                        OP("dve", "tensor_tensor", out=ext[:, ci, 2:n + 2], in0=ph[:, 0:n], in1=ccs[:, 0:n], op=ALU.mult,
                           reads=[b_ph, b_ccs], writes=[b_ext])
                        e0, e1, e2 = ext[:, ci, 0:n], ext[:, ci, 1:n + 1], ext[:, ci, 2:n + 2]
                        ycv = yc[:, 0:n]
                        if gi == 7:
                            OP("dve", "tensor_tensor", out=convp[:, ci, :], in0=ph[:, n - 2:n], in1=ccs[:, n - 2:n], op=ALU.mult,
                               reads=[b_ph, b_ccs], writes=[b_convp])
                    else:
                        OP("pool", "tensor_copy", out=exs[:, ci, :, 0:2], in_=ush[:, ci, :].rearrange("p (s r) -> p s r", r=2),
                           reads=[b_ush], writes=[b_exs])
                        OP("dve", "tensor_tensor", out=exs[:, ci, :, 2:6], in0=ph[:, 0:64].rearrange("p (s i) -> p s i", i=4),
                           in1=ccs[:, 0:64].rearrange("p (s i) -> p s i", i=4), op=ALU.mult, reads=[b_ph, b_ccs], writes=[b_exs])
                        e0, e1, e2 = exs[:, ci, :, 0:4], exs[:, ci, :, 1:5], exs[:, ci, :, 2:6]
                        ycv = yc[:, 0:64].rearrange("p (s i) -> p s i", i=4)
                        OP("dve", "tensor_tensor", out=convo[:, ci, :].rearrange("p (s r) -> p s r", r=2),
                           in0=ph[:, 0:64].rearrange("p (s i) -> p s i", i=4)[:, :, 2:4],
                           in1=ccs[:, 0:64].rearrange("p (s i) -> p s i", i=4)[:, :, 2:4], op=ALU.mult,
                           reads=[b_ph, b_ccs], writes=[b_convo])
                        OP("pool", "memset", yc[:, 64:128], 0.0, writes=[b_yc])
                    b_e = b_ext if kind == "prompt" else b_exs
                    OP("dve", "tensor_scalar_mul", out=ycv, in0=e0, scalar1=cw[:, 0, ci:ci + 1], reads=[b_e, b_cw], writes=[b_yc])
                    OP("dve", "scalar_tensor_tensor", out=ycv, in0=e1, scalar=cw[:, 1, ci:ci + 1], in1=ycv, op0=ALU.mult, op1=ALU.add,
                       reads=[b_e, b_cw, b_yc], writes=[b_yc])
                    OP("dve", "scalar_tensor_tensor", out=ycv, in0=e2, scalar=cw[:, 2, ci:ci + 1], in1=ycv, op0=ALU.mult, op1=ALU.add,
                       reads=[b_e, b_cw, b_yc], writes=[b_yc])
                    pb, b_pb = nextpA()
                    proj_fm(wcv, b_wcv, ci * 128, pb[:, 0:n], b_pb, n)
                    OP("dve", "tensor_tensor", out=ycT[:, ci, 0:n], in0=pb[:, 0:n], in1=yc[:, 0:n], op=ALU.mult,
                       reads=[b_pb, b_yc], writes=[b_ycT])
                for nc_ in range(8):
                    pg_, b_pg = nextpA()
                    proj_fm(wgc, b_wgc, nc_ * 128, pg_[:, 0:n], b_pg, n)
                    OP("act", "activation", out=sgc[:, 0:n], in_=pg_[:, 0:n], func=AF.Sigmoid, reads=[b_pg], writes=[b_sgc])
                    pg2, b_pg2 = nextpA()
                    proj_fm(wga, b_wga, nc_ * 128, pg2[:, 0:n], b_pg2, n)
                    OP("act", "activation", out=sga[:, 0:n], in_=pg2[:, 0:n], func=AF.Sigmoid, reads=[b_pg2], writes=[b_sga])
                    pbc, b_pbc = nextpA()
                    for ci in range(4):
                        OP("pe", "matmul", pbc[:, 0:n], lhsT=wbc[:, ci, nc_ * 128:(nc_ + 1) * 128], rhs=ycT[:, ci, 0:n],
                           start=(ci == 0), stop=(ci == 3), reads=[b_wbc, b_ycT], writes=[b_pbc], inc=(ci == 3))
                    pba, b_pba = nextpA()
                    for h in range(H):
                        OP("pe", "matmul", pba[:, 0:n], lhsT=wba[:, h, nc_ * 128:(nc_ + 1) * 128], rhs=yaT[:, h, 0:n],
                           start=(h == 0), stop=(h == 7), reads=[b_wba, b_yaT], writes=[b_pba], inc=(h == 7))
                    OP("dve", "tensor_tensor", out=t1[:, 0:n], in0=pbc[:, 0:n], in1=sgc[:, 0:n], op=ALU.mult, reads=[b_pbc, b_sgc], writes=[b_t1])
                    OP("dve", "tensor_tensor", out=sga[:, 0:n], in0=pba[:, 0:n], in1=sga[:, 0:n], op=ALU.mult, reads=[b_pba, b_sga], writes=[b_sga])
                    OP("pool", "tensor_tensor", out=mT[:, nc_, 0:n], in0=t1[:, 0:n], in1=sga[:, 0:n], op=ALU.add, reads=[b_t1, b_sga], writes=[b_mT])
                for ti in range(n // 128):
                    t = (rows0 // 128) + ti
                    xl, b_xl = lnt[ti]
                    x1_, b_x1 = x1t[t % 2]
                    for nh in range(2):
                        po_, b_po = nextpA()
                        for dc in range(8):
                            OP("pe", "matmul", po_[:], lhsT=mT[:, dc, ti * 128:(ti + 1) * 128], rhs=wo[:, dc, nh * 512:(nh + 1) * 512],
                               start=(dc == 0), stop=(dc == 7), reads=[b_mT, b_wo], writes=[b_po], inc=(dc == 7))
                        OP("dve", "scalar_tensor_tensor", out=x1_[:, nh * 512:(nh + 1) * 512], in0=xl[:, nh * 512:(nh + 1) * 512],
                           scalar=ALPHA, in1=po_[:], op0=ALU.mult, op1=ALU.add, reads=[b_xl, b_po], writes=[b_x1])
                    ln_stats(st6, b_st6, mv, b_mv, rstd, b_rstd, x1_, b_x1)
                    OP("dve", "tensor_scalar", out=x1_[:], in0=x1_[:], scalar1=mv[:, 0:1], scalar2=rstd[:, 0:1],
                       op0=ALU.subtract, op1=ALU.mult, reads=[b_x1, b_mv, b_rstd], writes=[b_x1])
                    OP("pool", "tensor_tensor", out=x1_[:], in0=x1_[:], in1=g1_bc[:], op=ALU.mult, reads=[b_x1, b_g1_bc], writes=[b_x1])
                    OP("pool", "tensor_tensor", out=x1_[:], in0=x1_[:], in1=b1_bc[:], op=ALU.add, reads=[b_x1, b_b1_bc], writes=[b_x1])
                    x1b_, b_x1b_ = x1bt[t % 2]
                    OP("pool", "tensor_copy", out=x1b_[:], in_=x1_[:], reads=[b_x1], writes=[b_x1b_])
                    DMA("sp", x1s[t * 128:(t + 1) * 128, :], x1_[:], reads=[b_x1, b_x1s], owner=b_x1)
                    DMA("sp", x1b[t * 128:(t + 1) * 128, :], x1b_[:], reads=[b_x1b_, b_x1b], owner=b_x1b_)
                    for half in range(2):
                        for q in range(4):
                            dc = half * 4 + q
                            OP("pe", "transpose", out=pX[:, q, :], in_=x1_[:, dc * 128:(dc + 1) * 128], identity=identf[:],
                               reads=[b_x1, b_identf], writes=[b_pX])
                        OP("act", "copy", out=x1T[:, half * 4:half * 4 + 4, :], in_=pX[:], reads=[b_pX], writes=[b_x1T])
                    for dc in range(8):
                        OP("pe", "matmul", pL[:, 0:32], lhsT=x1T[:, dc, :], rhs=wr[:, dc, :], start=(dc == 0), stop=(dc == 7),
                           reads=[b_x1T, b_wr], writes=[b_pL], inc=(dc == 7))
                    OP("dve", "tensor_tensor", out=lg[:], in0=pL[:, 0:32], in1=brb[:], op=ALU.add, reads=[b_pL, b_brb], writes=[b_lg])
                    OP("dve", "max", out=m8[:], in_=lg[:], reads=[b_lg], writes=[b_m8])
                    OP("dve", "tensor_scalar", out=msk[:], in0=lg[:], scalar1=m8[:, 3:4], scalar2=None, op0=ALU.is_ge,
                       reads=[b_lg, b_m8], writes=[b_msk])
                    OP("dve", "tensor_scalar_mul", out=sm[:, 0:1], in0=m8[:, 0:1], scalar1=-1.0, reads=[b_m8], writes=[b_sm])
                    OP("act", "activation", out=ex[:], in_=lg[:], func=AF.Exp, bias=sm[:, 0:1], scale=1.0, reads=[b_lg, b_sm], writes=[b_ex])
                    OP("dve", "tensor_tensor", out=ex[:], in0=ex[:], in1=msk[:], op=ALU.mult, reads=[b_ex, b_msk], writes=[b_ex])
                    OP("dve", "tensor_reduce", out=sm[:, 1:2], in_=ex[:], axis=AX.X, op=ALU.add, reads=[b_ex], writes=[b_sm])
                    OP("dve", "reciprocal", out=sm[:, 2:3], in_=sm[:, 1:2], reads=[b_sm], writes=[b_sm])
                    OP("dve", "tensor_scalar_mul", out=Gt[:], in0=ex[:], scalar1=sm[:, 2:3], reads=[b_ex, b_sm], writes=[b_Gt])
                    OP("pe", "matmul", pL[:, 32:64], lhsT=Lsf[:], rhs=msk[:], start=True, stop=False, reads=[b_Lsf, b_msk], writes=[b_pL], inc=False)
                    OP("pe", "matmul", pL[:, 32:64], lhsT=onesf[:], rhs=macc[:], start=False, stop=True, reads=[b_onesf, b_macc], writes=[b_pL])
                    OP("dve", "tensor_tensor", out=key[:], in0=pL[:, 32:64], in1=eoff[:], op=ALU.add, reads=[b_pL, b_eoff], writes=[b_key])
                    OP("dve", "tensor_tensor", out=key[:], in0=key[:], in1=msk[:], op=ALU.mult, reads=[b_key, b_msk], writes=[b_key])
                    OP("dve", "tensor_tensor", out=macc[:], in0=macc[:], in1=msk[:], op=ALU.add, reads=[b_macc, b_msk], writes=[b_macc])
                    OP("dve", "max", out=k8[:], in_=key[:], reads=[b_key], writes=[b_k8])
                    OP("dve", "tensor_scalar_add", out=sf[:], in0=k8[:, 0:4], scalar1=-1.0, reads=[b_k8], writes=[b_sf])
                    OP("dve", "tensor_copy", out=slot_i[:, t, :], in_=sf[:], reads=[b_sf], writes=[b_slot])
                    for k in range(4):
                        OP("dve", "tensor_scalar", out=oh[:], in0=key[:], scalar1=k8[:, k:k + 1], scalar2=None, op0=ALU.is_equal,
                           reads=[b_key, b_k8], writes=[b_oh])
                        OP("dve", "tensor_tensor", out=oh[:], in0=oh[:], in1=Gt[:], op=ALU.mult, reads=[b_oh, b_Gt], writes=[b_oh])
                        OP("dve", "tensor_reduce", out=gk[:, t, k:k + 1], in_=oh[:], axis=AX.X, op=ALU.add, reads=[b_oh], writes=[b_gk])
                        P.dma("pool", lambda e, t=t, k=k: e.indirect_dma_start(
                            out=tbl, out_offset=bass.IndirectOffsetOnAxis(ap=slot_i[:, t, k:k + 1], axis=0),
                            in_=tokid[:, t:t + 1], in_offset=None),
                            reads=[b_slot, b_tokid, b_tbl], owner=b_tbl)
            DMA("sp", o_convs.rearrange("(c p) s r -> p c (s r)", p=128), convo[:], reads=[b_convo])
            DMA("sp", o_convp.rearrange("(c p) r -> p c r", p=128), convp[:], reads=[b_convp])
            P.barrier()
            P.release(b_wcv, b_wgc, b_wga, b_wbc, b_wba, b_wo, b_wr, b_cw, b_hm, b_gin_bc, b_bin_bc, b_g1_bc, b_b1_bc, b_brb,
                      b_ztb, b_scT, b_yaT, b_convo, b_convp, *[b for _, b in xt4], *[b for _, b in x1t], *[b for _, b in x1bt])

        with contextlib.ExitStack() as SC:
            P.enabled = "C" in PHASES
            wgs = [SB(SC, "wg%d" % i, [128, 8, D], BF16) for i in range(2)]
            wus = [SB(SC, "wu%d" % i, [128, 8, D], BF16) for i in range(2)]
            wds = [SB(SC, "wd%d" % i, [128, 8, D], BF16) for i in range(2)]
            wcb = [[[P.buf("wc%d_%d_%d" % (mi, i, dc)) for dc in range(8)] for i in range(2)] for mi in range(3)]
            NSTG = 6
            stg = [SB(SC, "stg%d" % i, [128, D], F32, dma=True) for i in range(NSTG)]
            stg_i = [0]

            def load_chunk(e, c):
                mi, dc = divmod(c, 8)
                i = e % 2
                wt = (wgs, wus, wds)[mi][i][0]
                src = (w_gate, w_up, w_down)[mi][min(e, ne_ - 1)]
                k = stg_i[0]; stg_i[0] += 1
                st_, b_st = stg[k % NSTG]
                DMA("sp", st_[:], src[dc * 128:(dc + 1) * 128, :], writes=[b_st])
                if k % 2 == 0:
                    OP("act", "copy", out=wt[:, dc, :], in_=st_[:], reads=[b_st], writes=[wcb[mi][i][dc]])
                else:
                    OP("dve", "tensor_copy", out=wt[:, dc, :], in_=st_[:], reads=[b_st], writes=[wcb[mi][i][dc]])
            bgu = [SB(SC, "bgu%d" % i, [128, 2, 8], F32, dma=True) for i in range(2)]
            bdn = [SB(SC, "bdn%d" % i, [128, D], F32, dma=True) for i in range(2)]
            idx = [SB(SC, "idx%d" % i, [128, 3], I32, dma=True) for i in range(2)]
            xg = [SB(SC, "xg%d" % i, [128, 3, D], BF16, dma="sw") for i in range(2)]
            xgT, b_xgT = SB(SC, "xgT", [128, 8, CAP], BF16)
            hT, b_hT = SB(SC, "hT", [128, 8, CAP], BF16)
            gs_, b_gs = SB(SC, "gs", [128, CAP], F32)
            us_, b_us = SB(SC, "us", [128, CAP], F32)
            sg_, b_sg = SB(SC, "sg", [128, CAP], F32)
            yo = [SB(SC, "yo%d" % i, [128, D], BF16, dma=True) for i in range(2)]
            pTr = [PS(SC, "cpTr%d" % i, [128, 4, 128], BF16) for i in range(2)]
            pG = [PS(SC, "cpG%d" % i, [128, 512]) for i in range(2)]
            pU = [PS(SC, "cpU%d" % i, [128, 512]) for i in range(2)]
            pY = [PS(SC, "cpY%d" % i, [128, 512]) for i in range(2)]

            def load_expert(e):
                i = e % 2
                DMA("sp", bgu[i][0][:, 0, :], b_gate[e:e + 1, :].rearrange("o (c p) -> p (o c)", p=128), writes=[bgu[i][1]],
                    allow_slow_non_contiguous=True)
                DMA("sp", bgu[i][0][:, 1, :], b_up[e:e + 1, :].rearrange("o (c p) -> p (o c)", p=128), writes=[bgu[i][1]],
                    allow_slow_non_contiguous=True)
                DMA("sp", bdn[i][0][:], b_down[e:e + 1, :].broadcast_to([128, D]), writes=[bdn[i][1]])
                DMA("sp", idx[i][0][:], tbl[e * CAP:(e + 1) * CAP, :].rearrange("(j p) o -> p (j o)", p=128), reads=[b_tbl],
                    writes=[idx[i][1]], allow_slow_non_contiguous=True)
                for j in range(3):
                    P.dma("pool", lambda en, i=i, j=j: en.indirect_dma_start(
                        out=xg[i][0][:, j, :], out_offset=None, in_=x1b,
                        in_offset=bass.IndirectOffsetOnAxis(ap=idx[i][0][:, j:j + 1], axis=0)),
                        reads=[idx[i][1], b_x1b], writes=[xg[i][1]])

            load_expert(0)
            for c_ in range(24):
                load_chunk(0, c_)
            yo_i = 0
            for e in range(E):
                i = e % 2
                if e + 1 < E:
                    load_expert(e + 1)
                wg_ = wgs[i][0]; wu_ = wus[i][0]; wd_ = wds[i][0]
                xg_, b_xg = xg[i]
                for j in range(3):
                    for half in range(2):
                        pt_, b_pt = pTr[half]
                        for q in range(4):
                            dc = half * 4 + q
                            OP("pe", "transpose", out=pt_[:, q, :], in_=xg_[:, j, dc * 128:(dc + 1) * 128], identity=ident[:],
                               reads=[b_xg, b_ident], writes=[b_pt])
                        OP("act", "copy", out=xgT[:, half * 4:half * 4 + 4, j * 128:(j + 1) * 128], in_=pt_[:], reads=[b_pt], writes=[b_xgT])
                for fo in range(8):
                    pg_, b_pg = pG[fo % 2]
                    pu_, b_pu = pU[fo % 2]
                    for dc in range(8):
                        OP("pe", "matmul", pg_[:, 0:CAP], lhsT=wg_[:, dc, fo * 128:(fo + 1) * 128], rhs=xgT[:, dc, :],
                           start=(dc == 0), stop=(dc == 7), reads=[wcb[0][i][dc], b_xgT], writes=[b_pg], inc=(dc == 7))
                    for dc in range(8):
                        OP("pe", "matmul", pu_[:, 0:CAP], lhsT=wu_[:, dc, fo * 128:(fo + 1) * 128], rhs=xgT[:, dc, :],
                           start=(dc == 0), stop=(dc == 7), reads=[wcb[1][i][dc], b_xgT], writes=[b_pu], inc=(dc == 7))
                    OP("dve", "tensor_scalar", out=gs_[:], in0=pg_[:, 0:CAP], scalar1=bgu[i][0][:, 0, fo:fo + 1], scalar2=7.0,
                       op0=ALU.add, op1=ALU.min, reads=[b_pg, bgu[i][1]], writes=[b_gs])
                    OP("act", "activation", out=sg_[:], in_=gs_[:], func=AF.Sigmoid, scale=1.702, reads=[b_gs], writes=[b_sg])
                    OP("dve", "tensor_scalar", out=us_[:], in0=pu_[:, 0:CAP], scalar1=bgu[i][0][:, 1, fo:fo + 1], scalar2=7.0,
                       op0=ALU.add, op1=ALU.min, reads=[b_pu, bgu[i][1]], writes=[b_us])
                    OP("pool", "tensor_scalar", out=us_[:], in0=us_[:], scalar1=-7.0, scalar2=1.0, op0=ALU.max, op1=ALU.add,
                       reads=[b_us], writes=[b_us])
                    OP("pool", "tensor_tensor", out=gs_[:], in0=gs_[:], in1=sg_[:], op=ALU.mult, reads=[b_gs, b_sg], writes=[b_gs])
                    OP("pool", "tensor_tensor", out=hT[:, fo, :], in0=gs_[:], in1=us_[:], op=ALU.mult, reads=[b_gs, b_us], writes=[b_hT])
                    if e + 1 < E:
                        for r_ in range(3):
                            load_chunk(e + 1, fo * 3 + r_)
                for j in range(3):
                    yo_, b_yo = yo[yo_i % 2]; yo_i += 1
                    for nh in range(2):
                        py_, b_py = pY[nh]
                        for fo in range(8):
                            OP("pe", "matmul", py_[:], lhsT=hT[:, fo, j * 128:(j + 1) * 128], rhs=wd_[:, fo, nh * 512:(nh + 1) * 512],
                               start=(fo == 0), stop=(fo == 7), reads=[b_hT, wcb[2][i][fo]], writes=[b_py], inc=(fo == 7))
                        OP("dve", "tensor_tensor", out=yo_[:, nh * 512:(nh + 1) * 512], in0=py_[:], in1=bdn[i][0][:, nh * 512:(nh + 1) * 512],
                           op=ALU.add, reads=[b_py, bdn[i][1]], writes=[b_yo])
                    r0 = e * CAP + j * 128
                    DMA("sp", ybuf[r0:r0 + 128, :], yo_[:], reads=[b_yo, b_ybuf], owner=b_yo)
            P.barrier()
            P.release(*[b for _, b in stg], *[b for _, b in bgu], *[b for _, b in bdn],
                      *[b for _, b in idx], *[b for _, b in xg], *[b for _, b in yo])

        with contextlib.ExitStack() as SD:
            P.enabled = "D" in PHASES
            wpg, b_wpg = SB(SD, "wpg", [128, 8, D], BF16, dma="sw")
            wpp, b_wpp = SB(SD, "wpp", [128, 2, D], BF16, dma="sw")
            DMA("pool", wpg[:], wview(w_ple_gate), writes=[b_wpg])
            DMA("pool", wpp[:], wview(w_ple_proj), writes=[b_wpp])
            g2_bc, b_g2_bc = SB(SD, "g2_bc", [128, D], F32, dma=True)
            b2_bc, b_b2_bc = SB(SD, "b2_bc", [128, D], F32, dma=True)
            DMA("sp", g2_bc[:], ln2_g.broadcast_to([128, D]), writes=[b_g2_bc])
            DMA("sp", b2_bc[:], ln2_b.broadcast_to([128, D]), writes=[b_b2_bc])
            yk = [SB(SD, "yk%d" % i, [128, 4, D], BF16, dma="sw") for i in range(2)]
            x1r = [SB(SD, "x1r%d" % i, [128, D], F32, dma=True) for i in range(2)]
            pr = [SB(SD, "pr%d" % i, [128, 256], F32, dma=True) for i in range(2)]
            prb, b_prb = SB(SD, "prb", [128, 256], BF16)
            pT_, b_pT = SB(SD, "pTd", [128, 2, 128], BF16)
            acc_, b_acc = SB(SD, "accd", [128, D], F32)
            x2b, b_x2b = SB(SD, "x2b", [128, D], BF16)
            x2T, b_x2T = SB(SD, "x2T", [128, 8, 128], BF16)
            sgp, b_sgp = SB(SD, "sgp", [128, 512], F32)
            outs = [SB(SD, "outd%d" % i, [128, D], F32, dma=True) for i in range(2)]
            st6, b_st6 = SB(SD, "dst6", [128, 12], F32)
            mv, b_mv = SB(SD, "dmv", [128, 2], F32)
            rstd, b_rstd = SB(SD, "drstd", [128, 1], F32)
            pTr = [PS(SD, "dpTr%d" % i, [128, 4, 128], BF16) for i in range(2)]
            pGa = [PS(SD, "dpG%d" % i, [128, 512]) for i in range(2)]
            pPr = [PS(SD, "dpP%d" % i, [128, 512]) for i in range(2)]
            for t in range(NT):
                yk_, b_yk = yk[t % 2]
                for k in range(4):
                    P.dma("pool", lambda en, yk_=yk_, t=t, k=k: en.indirect_dma_start(
                        out=yk_[:, k, :], out_offset=None, in_=ybuf,
                        in_offset=bass.IndirectOffsetOnAxis(ap=slot_i[:, t, k:k + 1], axis=0)),
                        reads=[b_slot, b_ybuf], writes=[b_yk])
                x1_, b_x1 = x1r[t % 2]
                DMA("sp", x1_[:], x1s[t * 128:(t + 1) * 128, :], reads=[b_x1s], writes=[b_x1])
                p_, b_p = pr[t % 2]
                DMA("sp", p_[:], po[t * 128:(t + 1) * 128, :], writes=[b_p])
                OP("dve", "tensor_scalar_mul", out=acc_[:], in0=x1_[:], scalar1=ALPHA, reads=[b_x1], writes=[b_acc])
                for k in range(4):
                    OP("dve", "scalar_tensor_tensor", out=acc_[:], in0=yk_[:, k, :], scalar=gk[:, t, k:k + 1], in1=acc_[:],
                       op0=ALU.mult, op1=ALU.add, reads=[b_yk, b_gk, b_acc], writes=[b_acc])
                ln_stats(st6, b_st6, mv, b_mv, rstd, b_rstd, acc_, b_acc)
                OP("dve", "tensor_scalar", out=acc_[:], in0=acc_[:], scalar1=mv[:, 0:1], scalar2=rstd[:, 0:1],
                   op0=ALU.subtract, op1=ALU.mult, reads=[b_acc, b_mv, b_rstd], writes=[b_acc])
                OP("pool", "tensor_tensor", out=acc_[:], in0=acc_[:], in1=g2_bc[:], op=ALU.mult, reads=[b_acc, b_g2_bc], writes=[b_acc])
                OP("pool", "tensor_tensor", out=acc_[:], in0=acc_[:], in1=b2_bc[:], op=ALU.add, reads=[b_acc, b_b2_bc], writes=[b_acc])
                OP("pool", "tensor_copy", out=x2b[:], in_=acc_[:], reads=[b_acc], writes=[b_x2b])
                OP("pool", "tensor_copy", out=prb[:], in_=p_[:], reads=[b_p], writes=[b_prb])
                for half in range(2):
                    pt_, b_pt = pTr[half]
                    for q in range(4):
                        dc = half * 4 + q
                        OP("pe", "transpose", out=pt_[:, q, :], in_=x2b[:, dc * 128:(dc + 1) * 128], identity=ident[:],
                           reads=[b_x2b, b_ident], writes=[b_pt])
                    OP("act", "copy", out=x2T[:, half * 4:half * 4 + 4, :], in_=pt_[:], reads=[b_pt], writes=[b_x2T])
                pt_, b_pt = pTr[0]
                for q in range(2):
                    OP("pe", "transpose", out=pt_[:, q, :], in_=prb[:, q * 128:(q + 1) * 128], identity=ident[:],
                       reads=[b_prb, b_ident], writes=[b_pt])
                OP("act", "copy", out=pT_[:], in_=pt_[:, 0:2, :], reads=[b_pt], writes=[b_pT])
                o_, b_o = outs[t % 2]
                for nh in range(2):
                    pg_, b_pg = pGa[nh]
                    for dc in range(8):
                        OP("pe", "matmul", pg_[:], lhsT=x2T[:, dc, :], rhs=wpg[:, dc, nh * 512:(nh + 1) * 512], start=(dc == 0), stop=(dc == 7),
                           reads=[b_x2T, b_wpg], writes=[b_pg], inc=(dc == 7))
                    pp_, b_pp = pPr[nh]
                    for dc in range(2):
                        OP("pe", "matmul", pp_[:], lhsT=pT_[:, dc, :], rhs=wpp[:, dc, nh * 512:(nh + 1) * 512], start=(dc == 0), stop=(dc == 1),
                           reads=[b_pT, b_wpp], writes=[b_pp], inc=(dc == 1))
                    OP("act", "activation", out=sgp[:], in_=pg_[:], func=AF.Sigmoid, reads=[b_pg], writes=[b_sgp])
                    OP("dve", "tensor_tensor", out=sgp[:], in0=pp_[:], in1=sgp[:], op=ALU.mult, reads=[b_pp, b_sgp], writes=[b_sgp])
                    OP("dve", "tensor_tensor", out=o_[:, nh * 512:(nh + 1) * 512], in0=sgp[:], in1=acc_[:, nh * 512:(nh + 1) * 512], op=ALU.add,
                       reads=[b_sgp, b_acc], writes=[b_o])
                DMA("sp", o_y[t * 128:(t + 1) * 128, :], o_[:], reads=[b_o])
            P.barrier()

        P.enabled = True
        P.barrier()
        with nc.Block() as block:
            P.emit(block)
    P.close()
    return nc


_NC_CACHE = {}


def _prep_core(c, I):
    b, j = c // 4, c % 4
    f32 = np.float32
    xp = I["x_prompt"]; xs = I["x_sample"]
    Gs = [j + 4 * m for m in range(4)]
    xo = np.zeros((TOWN, D), f32)
    po = np.zeros((TOWN, 256), f32)
    xh = np.zeros((128, D), f32)
    hmask = np.zeros((128, 8), f32)
    sel = np.zeros((128, 4, 64), f32)
    kvis = np.zeros((128, 4, 64), f32)
    for m, G in enumerate(Gs):
        xo[m * 512:(m + 1) * 512] = xp[b, G * 512:(G + 1) * 512]
        po[m * 512:(m + 1) * 512] = I["p_prompt"][0, b, G * 512:(G + 1) * 512]
        if G > 0:
            xh[2 * m:2 * m + 2] = xp[b, G * 512 - 2:G * 512]
            hmask[:, 2 * m:2 * m + 2] = 1.0
        sel[127, m, 4 * G + 3] = 1.0
        kvis[:, m, 4 * G + 4:] = -30000.0
    xo[2048:2112] = xs[16 * c:16 * c + 16].reshape(64, D)
    po[2048:2112] = I["p_sample"][0, 16 * c:16 * c + 16].reshape(64, 256)
    dm = np.zeros((128, 16, 512), f32)
    kp = np.arange(128)[:, None]
    qi = np.arange(128)[None, :]
    tri = (kp <= qi).astype(f32)
    for r in range(16):
        rel = r - 4 * j
        if rel < 0:
            dm[:, r, :] = 1.0
        elif rel < 4:
            for qs in range(4):
                if qs > rel:
                    dm[:, r, qs * 128:(qs + 1) * 128] = 1.0
                elif qs == rel:
                    dm[:, r, qs * 128:(qs + 1) * 128] = tri
    bdm = np.zeros((64, 64), f32)
    for s in range(16):
        for i2 in range(4):
            for i1 in range(i2 + 1):
                bdm[4 * s + i1, 4 * s + i2] = 1.0
    return dict(
        xb=np.ascontiguousarray(xp[b]), xo=xo, po=po, xh=xh, hmask=hmask,
        dmask=dm.astype(ml_dtypes.bfloat16), sel=sel, kvis=kvis, bd=bdm,
        pt=np.ascontiguousarray(I["page_table"][16 * c:16 * c + 16]).reshape(1, 256).astype(np.int32),
        sconv=np.ascontiguousarray(I["state_conv"][0, 16 * c:16 * c + 16]).reshape(32, 512),
    )


def kernel(**I):
    I = {k: np.asarray(v) for k, v in I.items()}
    if "nc" not in _NC_CACHE:
        _NC_CACHE["nc"] = build()
    nc = _NC_CACHE["nc"]
    shared = dict(
        cache_k=I["cache_k"].reshape(NPOOLROWS, 512), cache_v=I["cache_v"].reshape(NPOOLROWS, 512),
        cache_f=I["cache_logf"].reshape(NPOOLROWS, 8),
        ln_in_g=I["ln_in_g"].reshape(1, D), ln_in_b=I["ln_in_b"].reshape(1, D),
        w_in=I["w_in"][0], b_f=I["b_f"].reshape(1, 8), conv_w=I["conv_w"][0],
        w_br_conv=I["w_br_conv"][0], w_br_attn=I["w_br_attn"][0], w_o=I["w_o"][0],
        ln1_g=I["ln1_g"].reshape(1, D), ln1_b=I["ln1_b"].reshape(1, D),
        w_router=I["w_router"][0], b_router=I["b_router"].reshape(1, E),
        w_gate=I["w_gate"][0], b_gate=I["b_gate"][0], w_up=I["w_up"][0], b_up=I["b_up"][0],
        w_down=I["w_down"][0], b_down=I["b_down"][0],
        ln2_g=I["ln2_g"].reshape(1, D), ln2_b=I["ln2_b"].reshape(1, D),
        w_ple_gate=I["w_ple_gate"][0], w_ple_proj=I["w_ple_proj"][0],
    )
    shared = {k: np.ascontiguousarray(v) for k, v in shared.items()}
    in_maps = []
    for c in range(8):
        d = dict(shared)
        d.update(_prep_core(c, I))
        in_maps.append(d)
    res = run_bass_kernel_spmd(nc, in_maps, core_ids=list(range(8)))
    R = res.results
    f32 = np.float32
    y_prompt = np.zeros((2, S, D), f32); y_sample = np.zeros((128, 4, D), f32)
    k_prompt = np.zeros((1, 2, S, H, HD), f32); v_prompt = np.zeros((1, 2, S, H, HD), f32)
    logf_prompt = np.zeros((1, 2, S, H), f32); conv_prompt = np.zeros((1, 2, 2, 512), f32)
    k_sample = np.zeros((1, 128, 4, H, HD), f32); v_sample = np.zeros((1, 128, 4, H, HD), f32)
    logf_sample = np.zeros((1, 128, 4, H), f32); conv_sample = np.zeros((1, 128, 2, 512), f32)
    for c in range(8):
        b, j = c // 4, c % 4
        r = R[c]
        oy = np.asarray(r["o_y"])
        for m in range(4):
            G = j + 4 * m
            y_prompt[b, G * 512:(G + 1) * 512] = oy[m * 512:(m + 1) * 512]
        y_sample[16 * c:16 * c + 16] = oy[2048:2112].reshape(16, 4, D)
        kT = np.asarray(r["o_kT"]); ov = np.asarray(r["o_v"]); olf = np.asarray(r["o_logf"])
        if j == 0:
            k_prompt[0, b] = kT[:, :S].T.reshape(S, H, HD)
            v_prompt[0, b] = ov[:S].reshape(S, H, HD)
            logf_prompt[0, b] = olf[:S]
        if j == 3:
            conv_prompt[0, b] = np.asarray(r["o_convp"]).T
        k_sample[0, 16 * c:16 * c + 16] = kT[:, S:S + 64].T.reshape(16, 4, H, HD)
        v_sample[0, 16 * c:16 * c + 16] = ov[S:S + 64].reshape(16, 4, H, HD)
        logf_sample[0, 16 * c:16 * c + 16] = olf[S:S + 64].reshape(16, 4, H)
        conv_sample[0, 16 * c:16 * c + 16] = np.transpose(np.asarray(r["o_convs"]), (1, 2, 0))
    return (y_prompt, y_sample, k_prompt, v_prompt, logf_prompt, conv_prompt,
            k_sample, v_sample, logf_sample, conv_sample)
```
